# Optimizing a Trainium2 kernel written in Bass

```python
import math
import jax, jax.numpy as jnp
from jax import lax
import numpy as np

D_MODEL = 1024
BATCH = 8
SEQ = 4096
DEPTH = 1

D_MIX = D_MODEL
ATTN_WIDTH = D_MIX // 2
N_DIFF_HEADS = 4
DIFF_HEAD_DIM = ATTN_WIDTH // (2 * N_DIFF_HEADS)
DIFF_V_DIM = 2 * DIFF_HEAD_DIM
ATTN_QK = N_DIFF_HEADS * 2 * DIFF_HEAD_DIM
POOL_WIDTH = D_MIX - ATTN_WIDTH
POOL_WINDOWS = (2, 4, 8, 16)
N_POOL_GROUPS = len(POOL_WINDOWS)
POOL_GROUP_DIM = POOL_WIDTH // N_POOL_GROUPS
D_IN_PROJ = 2 * ATTN_QK + ATTN_WIDTH + POOL_WIDTH

ROPE_THETA = 500000.0
ROPE_DIM = DIFF_HEAD_DIM // 4
Q_BLOCK = 128

N_EXPERTS = 32
TOP_K = 4
D_EXPERT = D_MODEL
SWIGLU_LIMIT = 7.0
SWIGLU_ALPHA = 1.702
MOE_BLOCK = 128

PLE_DIM = 256
EPS = 1e-6

kernel_name = "hybrid_diffattn_pool_moe_ple"


def rms_norm(t, g):
    tf = t.astype(jnp.float32)
    y = tf * lax.rsqrt(jnp.mean(tf * tf, axis=-1, keepdims=True) + EPS)
    return (y * g.astype(jnp.float32)).astype(t.dtype)


def apply_partial_rope(t, cos, sin):
    tf = t.astype(jnp.float32)
    half = ROPE_DIM // 2
    t1 = tf[..., :half]
    t2 = tf[..., half:ROPE_DIM]
    out = jnp.concatenate([t1 * cos - t2 * sin, t2 * cos + t1 * sin, tf[..., ROPE_DIM:]], axis=-1)
    return out.astype(t.dtype)


def diff_attention(q, k, v, positions, q_norm, k_norm, lam_q1, lam_k1, lam_q2, lam_k2, subln, lambda_init):
    B, S, _ = q.shape
    H, d = N_DIFF_HEADS, DIFF_HEAD_DIM
    q = rms_norm(q.reshape(B, S, H, 2, d), q_norm)
    k = rms_norm(k.reshape(B, S, H, 2, d), k_norm)
    v = v.reshape(B, S, H, DIFF_V_DIM)
    freqs = ROPE_THETA ** (-jnp.arange(0, ROPE_DIM, 2, dtype=jnp.float32) / ROPE_DIM)
    ang = positions.astype(jnp.float32)[..., None] * freqs
    cos = jnp.cos(ang)[:, :, None, None, :]
    sin = jnp.sin(ang)[:, :, None, None, :]
    q = apply_partial_rope(q, cos, sin)
    k = apply_partial_rope(k, cos, sin)
    lam = (jnp.exp(jnp.sum(lam_q1.astype(jnp.float32) * lam_k1.astype(jnp.float32)))
           - jnp.exp(jnp.sum(lam_q2.astype(jnp.float32) * lam_k2.astype(jnp.float32)))
           + lambda_init)
    scale = 1.0 / math.sqrt(d)
    nb = S // Q_BLOCK
    qb = q.transpose(0, 2, 3, 1, 4).reshape(B, H, 2, nb, Q_BLOCK, d)
    qb = jnp.moveaxis(qb, 3, 0)
    kt = k.transpose(0, 2, 3, 1, 4)
    vt = v.transpose(0, 2, 1, 3)
    key_idx = jnp.arange(S)

    def block(args):
        qblk, start = args
        s = jnp.einsum('bhcqd,bhckd->bhcqk', qblk, kt).astype(jnp.float32) * scale
        q_idx = start + jnp.arange(Q_BLOCK)
        mask = key_idx[None, :] <= q_idx[:, None]
        s = jnp.where(mask, s, -jnp.inf)
        a = jax.nn.softmax(s, axis=-1)
        w = a[:, :, 0] - lam * a[:, :, 1]
        return jnp.einsum('bhqk,bhkv->bhqv', w.astype(vt.dtype), vt)

    starts = jnp.arange(nb) * Q_BLOCK
    o = lax.map(block, (qb, starts))
    o = o.transpose(1, 0, 3, 2, 4).reshape(B, S, H, DIFF_V_DIM)
    o = rms_norm(o, subln) * (1.0 - lambda_init)
    return o.reshape(B, S, H * DIFF_V_DIM)


def pool_mixer(u, w_pool, pool_scale):
    B, S, _ = u.shape
    ug = u.reshape(B, S, N_POOL_GROUPS, POOL_GROUP_DIM)
    t_idx = jnp.arange(S)
    outs = []
    for g, w in enumerate(POOL_WINDOWS):
        uf = ug[:, :, g].astype(jnp.float32)
        cs = jnp.cumsum(uf, axis=1)
        shifted = jnp.pad(cs, ((0, 0), (w, 0), (0, 0)))[:, :S]
        count = jnp.minimum(t_idx + 1, w).astype(jnp.float32)
        outs.append((cs - shifted) / count[None, :, None] - uf)
    pooled = jnp.stack(outs, axis=2).astype(u.dtype)
    y = jnp.einsum('bsgc,gcd->bsgd', pooled, w_pool)
    return y.reshape(B, S, POOL_WIDTH) * pool_scale


def moe(xn, w_router, b_router, w_gate, b_gate, w_up, b_up, w_down, b_down):
    B, S, D = xn.shape
    N = B * S
    xt = xn.reshape(N, D)
    logits = (xt @ w_router).astype(jnp.float32) + b_router.astype(jnp.float32)
    top_vals, top_idx = lax.top_k(logits, TOP_K)
    gates = jax.nn.softmax(top_vals, axis=-1).astype(xn.dtype)
    NK = N * TOP_K
    expert_flat = top_idx.reshape(NK)
    token_flat = jnp.repeat(jnp.arange(N), TOP_K)
    gate_flat = gates.reshape(NK)
    order = jnp.argsort(expert_flat, stable=True)
    se = expert_flat[order]
    st = token_flat[order]
    sg = gate_flat[order]
    counts = jnp.bincount(expert_flat, length=N_EXPERTS)
    padded = ((counts + MOE_BLOCK - 1) // MOE_BLOCK) * MOE_BLOCK
    pend = jnp.cumsum(padded)
    pstart = pend - padded
    start = jnp.cumsum(counts) - counts
    dest = pstart[se] + (jnp.arange(NK) - start[se])
    n_blocks = -(-NK // MOE_BLOCK) + N_EXPERTS
    P = n_blocks * MOE_BLOCK
    xbuf = jnp.zeros((P, D), xn.dtype).at[dest].set(xt[st])
    block_expert = jnp.clip(jnp.searchsorted(pend, jnp.arange(n_blocks) * MOE_BLOCK, side='right'),
                            0, N_EXPERTS - 1)

    def expert_block(args):
        xb, e = args
        gt = xb @ w_gate[e] + b_gate[e]
        up = xb @ w_up[e] + b_up[e]
        gt = jnp.minimum(gt, SWIGLU_LIMIT)
        up = jnp.clip(up, -SWIGLU_LIMIT, SWIGLU_LIMIT)
        hdn = (up + 1.0) * (gt * jax.nn.sigmoid(SWIGLU_ALPHA * gt))
        return hdn @ w_down[e] + b_down[e]

    ybuf = lax.map(expert_block, (xbuf.reshape(n_blocks, MOE_BLOCK, D), block_expert)).reshape(P, D)
    y = ybuf[dest] * sg[:, None]
    out = jnp.zeros((N, D), xn.dtype).at[st].add(y)
    return out.reshape(B, S, D)


def setup_inputs(seed: int = 0) -> dict:
    key = jax.random.key(seed)
    ks = jax.random.split(key, 32)
    f32 = jnp.float32
    L = DEPTH

    def nrm(k, shape, scale):
        return jax.random.normal(k, shape, f32) * scale

    def gain(k, shape):
        return 1.0 + 0.02 * jax.random.normal(k, shape, f32)

    x = jax.random.normal(ks[0], (BATCH, SEQ, D_MODEL), f32)
    p = jax.random.normal(ks[1], (DEPTH, BATCH, SEQ, PLE_DIM), f32)
    offsets = jax.random.randint(ks[2], (BATCH, 1), 0, SEQ, dtype=jnp.int32)
    positions = (offsets + jnp.arange(SEQ, dtype=jnp.int32)[None, :]).astype(jnp.int32)
    return {
        "x": x,
        "p": p,
        "positions": positions,
        "attn_norm": gain(ks[3], (L, D_MODEL)),
        "w_in": nrm(ks[4], (L, D_MODEL, D_IN_PROJ), D_MODEL ** -0.5),
        "q_norm": gain(ks[5], (L, DIFF_HEAD_DIM)),
        "k_norm": gain(ks[6], (L, DIFF_HEAD_DIM)),
        "lam_q1": nrm(ks[7], (L, DIFF_HEAD_DIM), 0.1),
        "lam_k1": nrm(ks[8], (L, DIFF_HEAD_DIM), 0.1),
        "lam_q2": nrm(ks[9], (L, DIFF_HEAD_DIM), 0.1),
        "lam_k2": nrm(ks[10], (L, DIFF_HEAD_DIM), 0.1),
        "subln": gain(ks[11], (L, DIFF_V_DIM)),
        "w_pool": nrm(ks[12], (L, N_POOL_GROUPS, POOL_GROUP_DIM, POOL_GROUP_DIM), POOL_GROUP_DIM ** -0.5),
        "pool_scale": gain(ks[13], (L, POOL_WIDTH)),
        "w_out": nrm(ks[14], (L, D_MIX, D_MODEL), D_MIX ** -0.5),
        "ffn_norm": gain(ks[15], (L, D_MODEL)),
        "w_router": nrm(ks[16], (L, D_MODEL, N_EXPERTS), D_MODEL ** -0.5),
        "b_router": nrm(ks[17], (L, N_EXPERTS), 0.01),
        "w_gate": nrm(ks[18], (L, N_EXPERTS, D_MODEL, D_EXPERT), D_MODEL ** -0.5),
        "b_gate": nrm(ks[19], (L, N_EXPERTS, D_EXPERT), 0.02),
        "w_up": nrm(ks[20], (L, N_EXPERTS, D_MODEL, D_EXPERT), D_MODEL ** -0.5),
        "b_up": nrm(ks[21], (L, N_EXPERTS, D_EXPERT), 0.02),
        "w_down": nrm(ks[22], (L, N_EXPERTS, D_EXPERT, D_MODEL), D_EXPERT ** -0.5),
        "b_down": nrm(ks[23], (L, N_EXPERTS, D_MODEL), 0.02),
        "ple_gate_norm": gain(ks[24], (L, D_MODEL)),
        "w_ple_gate": nrm(ks[25], (L, D_MODEL, D_MODEL), D_MODEL ** -0.5),
        "w_ple_proj": nrm(ks[26], (L, PLE_DIM, D_MODEL), PLE_DIM ** -0.5),
        "ple_post_norm": gain(ks[27], (L, D_MODEL)),
    }


def reference(x, p, positions, attn_norm, w_in, q_norm, k_norm, lam_q1, lam_k1, lam_q2, lam_k2,
              subln, w_pool, pool_scale, w_out, ffn_norm, w_router, b_router, w_gate, b_gate,
              w_up, b_up, w_down, b_down, ple_gate_norm, w_ple_gate, w_ple_proj, ple_post_norm):
    h = x
    for i in range(DEPTH):
        lambda_init = 0.8 - 0.6 * math.exp(-0.3 * i)
        hn = rms_norm(h, attn_norm[i])
        z = hn @ w_in[i]
        q = z[..., :ATTN_QK]
        k = z[..., ATTN_QK:2 * ATTN_QK]
        v = z[..., 2 * ATTN_QK:2 * ATTN_QK + ATTN_WIDTH]
        u = z[..., 2 * ATTN_QK + ATTN_WIDTH:]
        a_out = diff_attention(q, k, v, positions, q_norm[i], k_norm[i], lam_q1[i], lam_k1[i],
                               lam_q2[i], lam_k2[i], subln[i], lambda_init)
        p_out = pool_mixer(u, w_pool[i], pool_scale[i])
        h = h + jnp.concatenate([a_out, p_out], axis=-1) @ w_out[i]
        h = h + moe(rms_norm(h, ffn_norm[i]), w_router[i], b_router[i], w_gate[i], b_gate[i],
                    w_up[i], b_up[i], w_down[i], b_down[i])
        gate = jax.nn.sigmoid(rms_norm(h, ple_gate_norm[i]) @ w_ple_gate[i])
        e = rms_norm(p[i].astype(h.dtype) @ w_ple_proj[i], ple_post_norm[i])
        h = h + gate * e
    return h
```

```python
import numpy as np
import concourse.bass as bass
import concourse.mybir as mybir

F32 = mybir.dt.float32
BF16 = mybir.dt.bfloat16
I32 = mybir.dt.int32
U32 = mybir.dt.uint32
ALU = mybir.AluOpType
AF = mybir.ActivationFunctionType
AX = mybir.AxisListType

PE, ACT, DVE, POOL, SP = "tensor", "scalar", "vector", "gpsimd", "sync"


class Tok:
    __slots__ = ("sem", "val", "eng")

    def __init__(self, sem, val, eng):
        self.sem, self.val, self.eng = sem, val, eng


class Rec:
    __slots__ = ("fn", "waits", "inc", "incv")

    def __init__(self, fn):
        self.fn, self.waits, self.inc, self.incv = fn, [], None, 0


class Sched:
    def __init__(self, nc, stack, n_dma_sems=96):
        self.nc = nc
        self.stack = stack
        self.ops = {e: [] for e in (PE, ACT, DVE, POOL, SP)}
        self.esem = {e: stack.enter_context(nc.semaphore("s_" + e)) for e in self.ops}
        self.ecount = {e: 0 for e in self.ops}
        self.pending = {e: False for e in self.ops}
        self.waited = {e: {} for e in self.ops}
        self.dsems = {}
        self.dcount = {}
        self.free_dsems = [stack.enter_context(nc.semaphore("d%d" % i)) for i in range(n_dma_sems)]
        self.writer = {}
        self.readers = {}

    def _need(self, eng, tok, same_ok):
        if tok is None:
            return None
        if tok.eng is not None:
            if self.ecount[tok.eng] < tok.val:
                last = self.ops[tok.eng][-1]
                assert last.inc is None
                last.inc, last.incv = self.esem[tok.eng], 1
                self.ecount[tok.eng] += 1
                self.pending[tok.eng] = False
                assert self.ecount[tok.eng] == tok.val
            val = tok.val
        else:
            val = self.dcount[id(tok.sem)]
        w = self.waited[eng]
        if w.get(id(tok.sem), 0) >= val:
            return None
        w[id(tok.sem)] = val
        return (tok.sem, val)

    def op(self, eng, fn, reads=(), writes=(), inc=None, dma=None, extra=()):
        raw, other = [], []
        for k in reads:
            t = self.writer.get(k)
            if t is not None:
                raw.append(t)
        for k in writes:
            t = self.writer.get(k)
            if t is not None:
                other.append(t)
            other.extend(self.readers.get(k, ()))
        raw.extend(extra)
        rec = Rec(fn)
        is_dma = dma is not None
        for t in raw:
            if t.eng == eng and not is_dma and eng == PE:
                continue
            wt = self._need(eng, t, False)
            if wt:
                rec.waits.append(wt)
        for t in other:
            if t.eng == eng and not is_dma and eng == PE:
                continue
            if is_dma and t.eng is None and dma in self.dsems and t.sem is self.dsems[dma]:
                continue
            wt = self._need(eng, t, False)
            if wt:
                rec.waits.append(wt)
        self.ops[eng].append(rec)
        if is_dma:
            if dma not in self.dsems:
                self.dsems[dma] = self.free_dsems.pop()
                self.dcount[id(self.dsems[dma])] = 0
            s = self.dsems[dma]
            self.dcount[id(s)] += 16
            rec.inc, rec.incv = s, 16
            tok = Tok(s, self.dcount[id(s)], None)
        else:
            if inc is None:
                inc = eng != PE
            if inc:
                self.ecount[eng] += 1
                rec.inc, rec.incv = self.esem[eng], 1
                self.pending[eng] = False
                tok = Tok(self.esem[eng], self.ecount[eng], eng)
            else:
                self.pending[eng] = True
                tok = Tok(self.esem[eng], self.ecount[eng] + 1, eng)
        for k in reads:
            self.readers.setdefault(k, []).append(tok)
        for k in writes:
            self.writer[k] = tok
            self.readers[k] = []
        return tok

    def finish(self, final_toks):
        rec = Rec(lambda e: e.nop())
        for t in final_toks:
            wt = self._need(SP, t, False)
            if wt:
                rec.waits.append(wt)
        self.ops[SP].append(rec)
        nc = self.nc
        with nc.Block() as block:
            def emit(name):
                def run(e):
                    for r in self.ops[name]:
                        for (s, v) in r.waits:
                            e.wait_ge(s, v)
                        ins = r.fn(e)
                        if r.inc is not None:
                            ins.then_inc(r.inc, r.incv)
                return run
            block.tensor(emit(PE))
            block.scalar(emit(ACT))
            block.vector(emit(DVE))
            block.gpsimd(emit(POOL))
            block.sync(emit(SP))
def dispatch(nc, S, sb, stC, pb, lgall, top8, idx8, maskall, Gall, onesf, tri, iota, bstart, eb_i, dest_i, gk, xn2_d, xbuf_d, pidx, OHall, ridx_i):
    NT, NBLK = 32, 160
    allk = [("lg", t) for t in range(NT)]
    exl = sb("exl", [128, NT, 32], F32, stC); sums = sb("sums", [128, NT], F32, stC)
    negmax = sb("negmax", [128, NT, 1], F32, stC)
    S.op(DVE, lambda e: e.tensor_tensor(out=maskall[:], in0=lgall[:], in1=top8[:, :, 3:4].to_broadcast([128, NT, 32]), op=ALU.is_ge),
         reads=allk + [("top8", t) for t in range(NT)], writes=["maskall"])
    S.op(DVE, lambda e: e.tensor_tensor(out=exl[:], in0=lgall[:], in1=top8[:, :, 0:1].to_broadcast([128, NT, 32]), op=ALU.subtract),
         reads=allk + [("top8", t) for t in range(NT)], writes=["exl"])
    S.op(ACT, lambda e: e.activation(out=exl[:], in_=exl[:], func=AF.Exp), reads=["exl"], writes=["exl"])
    S.op(DVE, lambda e: e.tensor_tensor(out=exl[:], in0=exl[:], in1=maskall[:], op=ALU.mult), reads=["exl", "maskall"], writes=["exl"])
    S.op(DVE, lambda e: e.tensor_reduce(out=sums[:], in_=exl[:], axis=AX.X, op=ALU.add), reads=["exl"], writes=["sums"])
    S.op(DVE, lambda e: e.reciprocal(out=sums[:], in_=sums[:]), reads=["sums"], writes=["sums"])
    S.op(DVE, lambda e: e.tensor_tensor(out=Gall[:], in0=exl[:], in1=sums[:].unsqueeze(2).to_broadcast([128, NT, 32]), op=ALU.mult), reads=["exl", "sums"], writes=["Gall"])
    mflat = maskall[:].rearrange("p t e -> p (t e)")
    S.op(PE, lambda e: e.matmul(pb[5][:], lhsT=onesf[:], rhs=mflat[:, 0:512], start=True, stop=True), reads=["maskall"], writes=["pb5"], inc=True)
    S.op(PE, lambda e: e.matmul(pb[6][:], lhsT=onesf[:], rhs=mflat[:, 512:1024], start=True, stop=True), reads=["maskall"], writes=["pb6"], inc=True)
    csA = sb("csA", [128, 48, 32], F32, stC); csB = sb("csB", [128, 48, 32], F32, stC); cs0 = sb("cs0", [128, NT, 32], F32, stC)
    S.op(DVE, lambda e: e.memset(csA[:, 0:16, :], 0.0), writes=["csA"])
    S.op(DVE, lambda e: e.memset(csB[:, 0:16, :], 0.0), writes=["csB"])
    S.op(DVE, lambda e: e.tensor_copy(out=csA[:, 16:32, :], in_=pb[5][:].rearrange("p (t e) -> p t e", e=32)), reads=["pb5"], writes=["csA"])
    S.op(DVE, lambda e: e.tensor_copy(out=csA[:, 32:48, :], in_=pb[6][:].rearrange("p (t e) -> p t e", e=32)), reads=["pb6"], writes=["csA"])
    S.op(DVE, lambda e: e.tensor_copy(out=cs0[:], in_=csA[:, 16:48, :]), reads=["csA"], writes=["cs0"])
    cur, curk, oth, othk = csA, "csA", csB, "csB"
    for j in range(5):
        sh = 1 << j
        S.op(DVE, lambda e, cur=cur, oth=oth, sh=sh: e.tensor_tensor(out=oth[:, 16:48, :], in0=cur[:, 16:48, :], in1=cur[:, 16 - sh:48 - sh, :], op=ALU.add), reads=[curk], writes=[othk])
        cur, curk, oth, othk = oth, othk, cur, curk
    incl = cur; inclk = curk
    cnt = sb("cnt", [128, 32], F32, stC); padd = sb("padd", [128, 32], F32, stC)
    scA = sb("scA", [128, 64], F32, stC); scB = sb("scB", [128, 64], F32, stC); pstart = sb("pstart", [128, 32], F32, stC)
    S.op(DVE, lambda e: e.tensor_copy(out=cnt[:], in_=incl[:, 47, :]), reads=[inclk], writes=["cnt"])
    cmpc = sb("cmpc", [128, 32, 32], F32, stC)
    S.op(DVE, lambda e: e.tensor_tensor(out=cmpc[:], in0=cnt[:].unsqueeze(2).to_broadcast([128, 32, 32]), in1=bstart[:, 0:32].unsqueeze(1).to_broadcast([128, 32, 32]), op=ALU.is_gt),
         reads=["cnt"], writes=["cmpc"])
    S.op(DVE, lambda e: e.tensor_reduce(out=padd[:], in_=cmpc[:], axis=AX.X, op=ALU.add), reads=["cmpc"], writes=["padd"])
    S.op(DVE, lambda e: e.tensor_scalar(out=padd[:], in0=padd[:], scalar1=128.0, scalar2=None, op0=ALU.mult), reads=["padd"], writes=["padd"])
    S.op(DVE, lambda e: e.memset(scA[:, 0:32], 0.0), writes=["scA"])
    S.op(DVE, lambda e: e.memset(scB[:, 0:32], 0.0), writes=["scB"])
    S.op(DVE, lambda e: e.tensor_copy(out=scA[:, 32:64], in_=padd[:]), reads=["padd"], writes=["scA"])
    cur, curk, oth, othk = scA, "scA", scB, "scB"
    for j in range(5):
        sh = 1 << j
        S.op(DVE, lambda e, cur=cur, oth=oth, sh=sh: e.tensor_tensor(out=oth[:, 32:64], in0=cur[:, 32:64], in1=cur[:, 32 - sh:64 - sh], op=ALU.add), reads=[curk], writes=[othk])
        cur, curk, oth, othk = oth, othk, cur, curk
    pend = cur; pendk = curk
    S.op(DVE, lambda e: e.tensor_tensor(out=pstart[:], in0=pend[:, 32:64], in1=padd[:], op=ALU.subtract), reads=[pendk, "padd"], writes=["pstart"])
    base = sb("base", [128, NT, 32], F32, stC)
    S.op(DVE, lambda e: e.tensor_tensor(out=base[:], in0=incl[:, 16:48, :], in1=cs0[:], op=ALU.subtract), reads=[inclk, "cs0"], writes=["base"])
    S.op(DVE, lambda e: e.tensor_tensor(out=base[:], in0=base[:], in1=pstart[:].unsqueeze(1).to_broadcast([128, NT, 32]), op=ALU.add), reads=["base", "pstart"], writes=["base"])
    for t in range(NT):
        bank = 5 + t // 16
        S.op(PE, lambda e, t=t, bank=bank: e.matmul(pb[bank][:, (t % 16) * 32:(t % 16 + 1) * 32], lhsT=tri[:], rhs=maskall[:, t, :], start=True, stop=True),
             reads=["maskall"], writes=["pb%d" % bank], inc=(t % 16 == 15))
    slot = sb("slot", [128, NT, 32], F32, stC)
    S.op(DVE, lambda e: e.tensor_tensor(out=slot[:, 0:16, :], in0=pb[5][:].rearrange("p (t e) -> p t e", e=32), in1=base[:, 0:16, :], op=ALU.add), reads=["pb5", "base"], writes=["slot"])
    S.op(DVE, lambda e: e.tensor_tensor(out=slot[:, 16:32, :], in0=pb[6][:].rearrange("p (t e) -> p t e", e=32), in1=base[:, 16:32, :], op=ALU.add), reads=["pb6", "base"], writes=["slot"])
    ebf = sb("ebf", [128, NBLK], F32, stC)
    S.op(DVE, lambda e: e.memset(ebf[:], 0.0), writes=["ebf"])
    for ee in range(32):
        S.op(DVE, lambda e, ee=ee: e.scalar_tensor_tensor(out=ebf[:], in0=bstart[:], scalar=pend[:, 32 + ee:33 + ee], in1=ebf[:], op0=ALU.is_ge, op1=ALU.add),
             reads=[pendk, "ebf"], writes=["ebf"])
    S.op(DVE, lambda e: e.tensor_scalar(out=ebf[:], in0=ebf[:], scalar1=31.0, scalar2=None, op0=ALU.min), reads=["ebf"], writes=["ebf"])
    S.op(DVE, lambda e: e.tensor_copy(out=eb_i[:], in_=ebf[:]), reads=["ebf"], writes=["eb_i"])
    S.op(DVE, lambda e: e.tensor_scalar(out=OHall[:], in0=ebf[:], scalar1=pidx[:, 1:2], scalar2=None, op0=ALU.is_equal), reads=["ebf"], writes=["OHall"])
    neq = sb("neq", [128, NBLK], F32, stC); ridxf = sb("ridxf", [128, NBLK], F32, stC)
    import os as _os2
    S.op(DVE, lambda e: e.memset(neq[:, 0:2], 0.0 if _os2.environ.get('KNOLOAD') else 1.0), writes=["neq"])
    S.op(DVE, lambda e: e.tensor_tensor(out=neq[:, 2:NBLK], in0=ebf[:, 2:NBLK], in1=ebf[:, 0:NBLK - 2], op=(ALU.is_lt if _os2.environ.get('KNOLOAD') else ALU.not_equal)), reads=["ebf"], writes=["neq"])
    S.op(DVE, lambda e: e.tensor_scalar(out=ridxf[:], in0=ebf[:], scalar1=128.0, scalar2=pidx[:, 0:1], op0=ALU.mult, op1=ALU.add), reads=["ebf"], writes=["ridxf"])
    S.op(DVE, lambda e: e.scalar_tensor_tensor(out=ridxf[:], in0=ridxf[:], scalar=-1.0e6, in1=neq[:], op0=ALU.add, op1=ALU.mult), reads=["ridxf", "neq"], writes=["ridxf"])
    S.op(DVE, lambda e: e.tensor_scalar(out=ridxf[:], in0=ridxf[:], scalar1=1.0e6, scalar2=None, op0=ALU.add), reads=["ridxf"], writes=["ridxf"])
    S.op(DVE, lambda e: e.tensor_copy(out=ridx_i[:], in_=ridxf[:]), reads=["ridxf"], writes=["ridx"])
    idxf = sb("idxf", [128, NT, 4], F32, stC); oh = sb("oh", [128, NT, 32], F32, stC); oh2 = sb("oh2", [128, NT, 32], F32, stC)
    destf = sb("destf", [128, NT, 4], F32, stC)
    S.op(DVE, lambda e: e.tensor_copy(out=idxf[:], in_=idx8[:, :, 0:4]), reads=[("idx8", t) for t in range(NT)], writes=["idxf"])
    for k in range(4):
        S.op(DVE, lambda e, k=k: e.tensor_tensor(out=oh[:], in0=iota[:].unsqueeze(1).to_broadcast([128, NT, 32]), in1=idxf[:, :, k:k + 1].to_broadcast([128, NT, 32]), op=ALU.is_equal),
             reads=["idxf"], writes=["oh"])
        S.op(DVE, lambda e: e.tensor_tensor(out=oh2[:], in0=oh[:], in1=slot[:], op=ALU.mult), reads=["oh", "slot"], writes=["oh2"])
        S.op(DVE, lambda e, k=k: e.tensor_reduce(out=destf[:, :, k], in_=oh2[:], axis=AX.X, op=ALU.add), reads=["oh2"], writes=["destf"])
        S.op(DVE, lambda e: e.tensor_tensor(out=oh2[:], in0=oh[:], in1=Gall[:], op=ALU.mult), reads=["oh", "Gall"], writes=["oh2"])
        S.op(DVE, lambda e, k=k: e.tensor_reduce(out=gk[:, :, k], in_=oh2[:], axis=AX.X, op=ALU.add), reads=["oh2"], writes=["gk"])
    S.op(DVE, lambda e: e.tensor_copy(out=dest_i[:], in_=destf[:]), reads=["destf"], writes=["dest_i"])
    xs = [sb("xs%d" % i, [128, 1024], BF16, stC) for i in range(2)]
    for t in range(NT):
        S.op(SP, lambda e, t=t: e.dma_start(out=xs[t % 2][:], in_=xn2_d[t * 128:(t + 1) * 128, :]), reads=["xn2_d"], writes=[("xs", t % 2)], dma="xs%d" % (t % 2))
        for k in range(4):
            S.op(POOL, lambda e, t=t, k=k: e.indirect_dma_start(out=xbuf_d, out_offset=bass.IndirectOffsetOnAxis(ap=dest_i[:, t, k:k + 1], axis=0), in_=xs[t % 2][:], in_offset=None),
                 reads=[("xs", t % 2), "dest_i"], writes=["xbuf"], dma="scat")


def moe_blocks(nc, S, sb, st, pb, pbb, OHall, ridx_i, wbf_d, btab_d, xbuf_d, ybuf_d, identb, nblk=160):
    import os as _o3
    NOW = bool(_o3.environ.get('KNOW'))
    with ExitStack() as stD:
        W = {nm: [sb("%s%d" % (nm, i), [128, 8, 1024], BF16, stD) for i in range(2)] for nm in ("wg", "wu", "wd")}
        Wd_ = {nm: wbf_d[m_].rearrange("(r c2) x -> r (c2 x)", c2=4) for m_, nm in enumerate(("wg", "wu", "wd"))}
        btab = sb("btab", [64, 3072], F32, stD); bt2 = sb("bt2", [64, 3072], BF16, stD); btl = sb("btl", [64, 3072], F32, stD)
        S.op(SP, lambda e: e.dma_start(out=btab[0:32, :], in_=btab_d), writes=["btab"], dma="btab")
        S.op(SP, lambda e: e.dma_start(out=btab[32:64, :], in_=btab_d), writes=["btab"], dma="btab")
        S.op(DVE, lambda e: e.tensor_copy(out=bt2[:], in_=btab[:]), reads=["btab"], writes=["bt2"])
        S.op(DVE, lambda e: e.tensor_copy(out=btl[:], in_=bt2[:]), reads=["bt2"], writes=["btl"])
        S.op(DVE, lambda e: e.tensor_tensor(out=btl[:], in0=btab[:], in1=btl[:], op=ALU.subtract), reads=["btab", "btl"], writes=["btl"])
        S.op(DVE, lambda e: e.tensor_copy(out=bt2[32:64, :], in_=btl[32:64, :]), reads=["btl", "bt2"], writes=["bt2"])
        ohb = [sb("ohb%d" % i, [64, 128], BF16, stD) for i in range(2)]
        xb = [sb("xb%d" % i, [128, 1024], BF16, stD) for i in range(2)]
        xT = [sb("xT%d" % i, [128, 8, 128], BF16, stD) for i in range(2)]
        gtb = [sb("gtb%d" % i, [128, 512], F32, stD) for i in range(2)]; sgb = [sb("sgb%d" % i, [128, 512], F32, stD) for i in range(2)]
        upb = [sb("upb%d" % i, [128, 512], F32, stD) for i in range(2)]
        hdn = [sb("hdn%d" % i, [128, 1024], BF16, stD) for i in range(2)]
        hT = [sb("hT%d" % i, [128, 8, 128], BF16, stD) for i in range(2)]
        yb = [sb("yb%d" % i, [128, 1024], F32, stD) for i in range(2)]
        breg = stD.enter_context(nc.gpsimd.register("bnd_reg"))
        S.op(POOL, lambda e: e.reg_mov(breg, 32 * 128 - 1))

        def gath(nm, b):
            par = b % 2
            S.op(POOL, lambda e: e.indirect_dma_start(out=W[nm][par][:].rearrange("p c f -> p (c f)"), out_offset=None, in_=Wd_[nm],
                 in_offset=bass.IndirectOffsetOnAxis(ap=ridx_i[:, b:b + 1], axis=0), bounds_check=breg, oob_is_err=False),
                 reads=["ridx", "wbf"], writes=[(nm, par, c) for c in range(8)], dma="%s%d" % (nm, par))

        def P1(b):
            par = b % 2
            gath("wg", b); gath("wu", b)
            if b >= 1:
                gath("wd", b - 1)
            if b == 0:
                S.op(SP, lambda e: e.dma_start(out=xb[0][:], in_=xbuf_d[0:128, :]), writes=[("xb", 0)], dma="xb0")
            if b + 1 < nblk:
                S.op(SP, lambda e, b=b: e.dma_start(out=xb[(b + 1) % 2][:], in_=xbuf_d[(b + 1) * 128:(b + 2) * 128, :]), writes=[("xb", (b + 1) % 2)], dma="xb%d" % ((b + 1) % 2))
            S.op(DVE, lambda e, b=b, par=par: e.tensor_copy(out=ohb[par][:], in_=OHall[0:64, b:b + 1].to_broadcast([64, 128])), reads=["OHall"], writes=[("ohb", par)])
            for c in range(8):
                S.op(PE, lambda e, c=c, par=par: e.transpose(out=pbb[6][:, c * 128:(c + 1) * 128], in_=xb[par][:, c * 128:(c + 1) * 128], identity=identb[:]),
                     reads=[("xb", par)], writes=["pb6"], inc=(c == 7))
            S.op(ACT, lambda e, par=par: e.activation(out=xT[par][:], in_=pbb[6].rearrange("p (c t) -> p c t", c=8), func=AF.Copy), reads=["pb6"], writes=[("xT", par)])

        def P2(b):
            par = b % 2
            for hf in range(2):
                accs = [("wg", 2 * hf, hf * 512), ("wu", 2 * hf + 1, 1024 + hf * 512)]
                for (nm, a, boff) in accs:
                    S.op(PE, lambda e, a=a, boff=boff, par=par: e.matmul(pb[a][:], lhsT=ohb[par][:], rhs=bt2[:, boff:boff + 512], start=True, stop=False),
                         reads=[("ohb", par), "bt2"], writes=["pb%d" % a])
                for c in range(8):
                    for (nm, a, boff) in accs:
                        S.op(PE, lambda e, a=a, nm=nm, hf=hf, c=c, par=par: e.matmul(pb[a][:], lhsT=xT[par][:, c, :], rhs=W[nm][par][:, c, hf * 512:(hf + 1) * 512], start=False, stop=(c == 7)),
                             reads=[("xT", par)] + ([] if NOW else [(nm, par, c)]), writes=["pb%d" % a], inc=(c == 7))
                G, U = pb[2 * hf], pb[2 * hf + 1]; gk_, uk_ = "pb%d" % (2 * hf), "pb%d" % (2 * hf + 1)
                S.op(DVE, lambda e, hf=hf, G=G: e.tensor_scalar(out=gtb[hf][:], in0=G[:], scalar1=7.0, scalar2=None, op0=ALU.min), reads=[gk_], writes=[("gtb", hf)])
                S.op(ACT, lambda e, hf=hf: e.activation(out=sgb[hf][:], in_=gtb[hf][:], func=AF.Sigmoid, scale=1.702), reads=[("gtb", hf)], writes=[("sgb", hf)])
                S.op(DVE, lambda e, hf=hf, U=U: e.tensor_scalar(out=upb[hf][:], in0=U[:], scalar1=-7.0, scalar2=7.0, op0=ALU.max, op1=ALU.min), reads=[uk_], writes=[("upb", hf)])
                S.op(DVE, lambda e, hf=hf: e.scalar_tensor_tensor(out=upb[hf][:], in0=upb[hf][:], scalar=1.0, in1=gtb[hf][:], op0=ALU.add, op1=ALU.mult), reads=[("upb", hf), ("gtb", hf)], writes=[("upb", hf)])
                S.op(DVE, lambda e, hf=hf, par=par: e.tensor_tensor(out=hdn[par][:].rearrange("s (c p) -> s p c", p=128)[:, hf * 64:(hf + 1) * 64, :], in0=upb[hf][:].rearrange("s (p c) -> s p c", c=8),
                     in1=sgb[hf][:].rearrange("s (p c) -> s p c", c=8), op=ALU.mult), reads=[("upb", hf), ("sgb", hf)], writes=[("hdn", par)])

        def P3(b):
            par = b % 2
            for c in range(8):
                S.op(PE, lambda e, c=c, par=par: e.transpose(out=pbb[7][:, c * 128:(c + 1) * 128], in_=hdn[par][:, c * 128:(c + 1) * 128], identity=identb[:]),
                     reads=[("hdn", par)], writes=["pb7"], inc=(c == 7))
            S.op(ACT, lambda e, par=par: e.activation(out=hT[par][:], in_=pbb[7].rearrange("p (c t) -> p c t", c=8), func=AF.Copy), reads=["pb7"], writes=[("hT", par)])
            for hf in range(2):
                S.op(PE, lambda e, hf=hf, par=par: e.matmul(pb[4 + hf][:], lhsT=ohb[par][:], rhs=bt2[:, 2048 + hf * 512:2048 + (hf + 1) * 512], start=True, stop=False),
                     reads=[("ohb", par), "bt2"], writes=["pb%d" % (4 + hf)])
                for c in range(8):
                    S.op(PE, lambda e, hf=hf, c=c, par=par: e.matmul(pb[4 + hf][:], lhsT=hT[par][:, c, :], rhs=W["wd"][par][:, c, hf * 512:(hf + 1) * 512], start=False, stop=(c == 7)),
                         reads=[("hT", par)] + ([] if NOW else [("wd", par, c)]), writes=["pb%d" % (4 + hf)], inc=(c == 7))
                if hf == 0:
                    S.op(ACT, lambda e, hf=hf, par=par: e.activation(out=yb[par][:, hf * 512:(hf + 1) * 512], in_=pb[4 + hf][:], func=AF.Copy), reads=["pb%d" % (4 + hf)], writes=[("yb", par)])
                else:
                    S.op(DVE, lambda e, hf=hf, par=par: e.tensor_copy(out=yb[par][:, hf * 512:(hf + 1) * 512], in_=pb[4 + hf][:]), reads=["pb%d" % (4 + hf)], writes=[("yb", par)])
            S.op(SP, lambda e, b=b, par=par: e.dma_start(out=ybuf_d[b * 128:(b + 1) * 128, :], in_=yb[par][:]), reads=[("yb", par)], writes=["ybuf"], dma="yst%d" % par)

        P1(0); P2(0)
        for b in range(1, nblk):
            P1(b); P2(b); P3(b - 1)
        gath("wd", nblk - 1)
        P3(nblk - 1)


def run_pipelined(tile_ops, ntiles, skew):
    lists = {}
    nops = None
    s = 0
    done = 0
    while done < ntiles:
        for t in range(ntiles):
            st_ = s - t * skew
            if st_ < 0:
                break
            if t not in lists:
                lists[t] = tile_ops(t)
                nops = len(lists[t])
            if st_ < len(lists[t]):
                lists[t][st_]()
                if st_ == len(lists[t]) - 1:
                    done += 1
        s += 1


def ple_phase(nc, S, sb, st, pb, pbb, dest_i, gk, h1_d, ybuf_d, p_d, w_pg_d, w_pp_d, post_b_d, gcol_ple, identb, out_d, epsc):
    NT = 32
    with ExitStack() as stE:
        w_pg = sb("w_pg", [128, 8, 1024], BF16, stE); w_pp = sb("w_pp", [128, 2, 1024], BF16, stE); post_b = sb("post_b", [128, 1024], F32, stE)
        for c in range(8):
            S.op(POOL, lambda e, c=c: e.dma_start(out=w_pg[:, c, :], in_=w_pg_d[c * 128:(c + 1) * 128, :]), writes=[("w_pg", c)], dma="w_pg")
        for c in range(8):
            S.op(DVE, lambda e, c=c: e.tensor_scalar(out=w_pg[:, c, :], in0=w_pg[:, c, :], scalar1=gcol_ple[:, c:c + 1], scalar2=None, op0=ALU.mult), reads=[("w_pg", c)], writes=[("w_pg", c)])
        for c in range(2):
            S.op(POOL, lambda e, c=c: e.dma_start(out=w_pp[:, c, :], in_=w_pp_d[c * 128:(c + 1) * 128, :]), writes=["w_pp"], dma="w_pp")
        S.op(SP, lambda e: e.dma_start(out=post_b[:], in_=post_b_d), writes=["post_b"], dma="cE")
        D = lambda nm, shape, dt=F32: [sb("%s_%d" % (nm, i), shape, dt, stE) for i in range(3)]
        yk = [[sb("yk%d_%d" % (i, k), [128, 1024], F32, stE) for k in range(4)] for i in range(3)]
        h2 = D("h2", [128, 1024]); pt = D("pt", [128, 256]); ptb = D("ptb", [128, 256], BF16); pT = D("pT", [128, 2, 128], BF16)
        hn3 = D("hn3", [128, 1024], BF16); hn3T = D("hn3T", [128, 8, 128], BF16); ejunk = D("ejunk", [128, 1024], BF16)
        gate = D("gate", [128, 1024]); et = D("et", [128, 1024]); st5 = D("st5", [128, 8])

        def tile_ops(t):
            par = t % 3
            ops = []
            A = ops.append
            P = lambda nm: (nm, par)
            A(lambda: S.op(SP, lambda e: e.dma_start(out=h2[par][:], in_=h1_d[t * 128:(t + 1) * 128, :]), reads=["h1_d"], writes=[P("h2")], dma="h2_%d" % par))
            A(lambda: S.op(SP, lambda e: e.dma_start(out=pt[par][:], in_=p_d[t * 128:(t + 1) * 128, :]), writes=[P("pt")], dma="pt%d" % par))
            for k in range(4):
                A(lambda k=k: S.op(POOL, lambda e: e.indirect_dma_start(out=yk[par][k][:], out_offset=None, in_=ybuf_d, in_offset=bass.IndirectOffsetOnAxis(ap=dest_i[:, t, k:k + 1], axis=0)),
                                   reads=["ybuf", "dest_i"], writes=[("yk", par, k)], dma="yk%d" % par))
            A(lambda: S.op(ACT, lambda e: e.activation(out=ptb[par][:], in_=pt[par][:], func=AF.Copy), reads=[P("pt")], writes=[P("ptb")]))
            for c in range(2):
                A(lambda c=c: S.op(PE, lambda e: e.transpose(out=pbb[3][:, c * 128:(c + 1) * 128], in_=ptb[par][:, c * 128:(c + 1) * 128], identity=identb[:]), reads=[P("ptb")], writes=["pb3"], inc=(c == 1)))
            A(lambda: S.op(DVE, lambda e: e.tensor_copy(out=pT[par][:], in_=pbb[3][:, 0:256].rearrange("p (c t) -> p c t", c=2)), reads=["pb3"], writes=[P("pT")]))
            A(lambda: S.op(DVE, lambda e: e.memset(st5[par][:, 0:3], 0.0), writes=[P("st5")]))
            for hf in range(2):
                for c in range(2):
                    A(lambda c=c, hf=hf: S.op(PE, lambda e: e.matmul(pb[4 + hf][:], lhsT=pT[par][:, c, :], rhs=w_pp[:, c, hf * 512:(hf + 1) * 512], start=(c == 0), stop=(c == 1)),
                                              reads=[P("pT"), "w_pp"], writes=["pb%d" % (4 + hf)], inc=(c == 1)))
                A(lambda hf=hf: S.op(ACT, lambda e: e.activation(out=ejunk[par][:, hf * 512:(hf + 1) * 512], in_=pb[4 + hf][:], func=AF.Square, accum_out=st5[par][:, 1 + hf:2 + hf]),
                                     reads=["pb%d" % (4 + hf), P("st5")], writes=[P("ejunk"), ("st5e", par, hf)]))
            A(lambda: S.op(DVE, lambda e: e.tensor_tensor(out=st5[par][:, 4:5], in0=st5[par][:, 1:2], in1=st5[par][:, 2:3], op=ALU.add), reads=[("st5e", par, 0), ("st5e", par, 1)], writes=[P("st5s")]))
            A(lambda: S.op(ACT, lambda e: e.activation(out=st5[par][:, 4:5], in_=st5[par][:, 4:5], func=AF.Ln, scale=1.0 / 1024, bias=epsc[:, 0:1]), reads=[P("st5s")], writes=[P("st5s")]))
            A(lambda: S.op(ACT, lambda e: e.activation(out=st5[par][:, 4:5], in_=st5[par][:, 4:5], func=AF.Exp, scale=-0.5), reads=[P("st5s")], writes=[P("st5s")]))
            for hf in range(2):
                A(lambda hf=hf: S.op(DVE, lambda e: e.scalar_tensor_tensor(out=et[par][:, hf * 512:(hf + 1) * 512], in0=pb[4 + hf][:], scalar=st5[par][:, 4:5], in1=post_b[:, hf * 512:(hf + 1) * 512], op0=ALU.mult, op1=ALU.mult),
                                     reads=["pb%d" % (4 + hf), P("st5s"), "post_b"], writes=[P("et")]))
            A('STAGE')
            for k in range(4):
                A(lambda k=k: S.op(DVE, lambda e: e.scalar_tensor_tensor(out=h2[par][:], in0=yk[par][k][:], scalar=gk[:, t, k:k + 1], in1=h2[par][:], op0=ALU.mult, op1=ALU.add),
                                   reads=[("yk", par, k), P("h2"), "gk"], writes=[P("h2")]))
            A(lambda: S.op(ACT, lambda e: e.activation(out=ejunk[par][:], in_=h2[par][:], func=AF.Square, accum_out=st5[par][:, 0:1]), reads=[P("h2"), P("st5")], writes=[P("ejunk"), P("st5a")]))
            A(lambda: S.op(ACT, lambda e: e.activation(out=st5[par][:, 3:4], in_=st5[par][:, 0:1], func=AF.Ln, scale=1.0 / 1024, bias=epsc[:, 0:1]), reads=[P("st5a")], writes=[P("st5r")]))
            A(lambda: S.op(ACT, lambda e: e.activation(out=st5[par][:, 3:4], in_=st5[par][:, 3:4], func=AF.Exp, scale=-0.5), reads=[P("st5r")], writes=[P("st5r")]))
            A(lambda: S.op(ACT, lambda e: e.activation(out=hn3[par][:], in_=h2[par][:], func=AF.Copy, scale=st5[par][:, 3:4]), reads=[P("h2"), P("st5r")], writes=[P("hn3")]))
            for c in range(8):
                A(lambda c=c: S.op(PE, lambda e: e.transpose(out=pbb[0][:, c * 128:(c + 1) * 128], in_=hn3[par][:, c * 128:(c + 1) * 128], identity=identb[:]), reads=[P("hn3")], writes=["pb0"], inc=(c == 7)))
            A(lambda: S.op(DVE, lambda e: e.tensor_copy(out=hn3T[par][:], in_=pbb[0].rearrange("p (c t) -> p c t", c=8)), reads=["pb0"], writes=[P("hn3T")]))
            A('STAGE')
            for hf in range(2):
                for c in range(8):
                    A(lambda c=c, hf=hf: S.op(PE, lambda e: e.matmul(pb[1 + hf][:], lhsT=hn3T[par][:, c, :], rhs=w_pg[:, c, hf * 512:(hf + 1) * 512], start=(c == 0), stop=(c == 7)),
                                              reads=[P("hn3T"), ("w_pg", c)], writes=["pb%d" % (1 + hf)], inc=(c == 7)))
                A(lambda hf=hf: S.op(ACT, lambda e: e.activation(out=gate[par][:, hf * 512:(hf + 1) * 512], in_=pb[1 + hf][:], func=AF.Sigmoid), reads=["pb%d" % (1 + hf)], writes=[P("gate")]))
            A(lambda: S.op(DVE, lambda e: e.tensor_tensor(out=et[par][:], in0=et[par][:], in1=gate[par][:], op=ALU.mult), reads=[P("et"), P("gate")], writes=[P("et")]))
            A(lambda: S.op(DVE, lambda e: e.tensor_tensor(out=et[par][:], in0=et[par][:], in1=h2[par][:], op=ALU.add), reads=[P("et"), P("h2")], writes=[P("et")]))
            A(lambda: S.op(SP, lambda e: e.dma_start(out=out_d[t * 128:(t + 1) * 128, :], in_=et[par][:]), reads=[P("et")], writes=["out_d"], dma="out%d" % par))
            stages = [[]]
            for o in ops:
                if o == 'STAGE':
                    stages.append([])
                else:
                    stages[-1].append(o)
            return stages
        cache = {}

        def get(t):
            if t not in cache:
                cache[t] = tile_ops(t)
            return cache[t]
        for it in range(NT + 2):
            for stg, t in ((0, it), (1, it - 1), (2, it - 2)):
                if 0 <= t < NT:
                    for o in get(t)[stg]:
                        o()
import math
import os as _os
from contextlib import ExitStack
import ml_dtypes
from concourse.bass_utils import run_bass_kernel_spmd

S_TOK = 4096
NT = 32
NG = 8
NBLK = 160
NSLOT = NBLK * 128
LAMBDA_INIT = 0.2
EPS = 1e-6
DEBUG = False


def build(stage=99):
    nc = bass.Bass("TRN2", target_bir_lowering=False)

    def din(name, shape, dt=F32):
        return nc.dram_tensor(name, list(shape), dt, kind="ExternalInput").ap()

    x_d = din("x", [S_TOK, 1024]); p_d = din("p", [S_TOK, 256]); pos_d = din("posb", [128, S_TOK], I32)
    w_in_d = din("w_in", [1024, 2048]); gcol_attn_d = din("gcol_attn", [128, 8])
    qn_d = din("qn_col", [128, 1]); kn_d = din("kn_col", [128, 1]); lamv_d = din("lamv", [128, 4, 64])
    subln_d = din("subln_b", [128, 128]); w_pool_d = din("w_pool", [4, 128, 128]); pscale_d = din("pscale_col", [128, 4])
    w_out_d = din("w_out", [1024, 1024]); ffn_b_d = din("ffn_b", [128, 1024]); w_r_d = din("w_router", [128, 8, 32])
    b_r_d = din("b_router_b", [128, 32])
    wg_d = din("w_gate", [32, 1024, 1024]); wu_d = din("w_up", [32, 1024, 1024]); wd_d = din("w_down", [32, 1024, 1024])
    btab_d = din("btab", [32, 3072])
    gcol_ple_d = din("gcol_ple", [128, 8]); w_pg_d = din("w_ple_gate", [1024, 1024]); w_pp_d = din("w_ple_proj", [256, 1024])
    post_b_d = din("post_b", [128, 1024])
    identb_d = din("ident_bf", [128, 128], BF16); identf_d = din("ident_f", [128, 128]); onesf_d = din("ones_f", [128, 128])
    tri_d = din("tri_strict", [128, 128]); blk_d = din("blkdiag", [128, 128], BF16); rmat_d = din("rmat", [128, 128], BF16)
    cmask_d = din("cmask", [128, 128], BF16); freq_d = din("freq_col", [128, 1]); invc_d = din("invcnt", [128, 16])
    iota_d = din("iota_row", [128, 32]); bstart_d = din("bstart_row", [128, NBLK]); pidx_d = din("pidx_col", [128, 2])
    out_d = nc.dram_tensor("out", [S_TOK, 1024], F32, kind="ExternalOutput").ap()
    KS = "ExternalOutput" if DEBUG else "Internal"
    catp_d = nc.dram_tensor("catp_s", [4, 128, S_TOK], BF16, kind=KS).ap()
    if DEBUG:
        dbg_q = nc.dram_tensor("dbg_q", [128, 4, S_TOK], BF16, kind=KS).ap(); dbg_k = nc.dram_tensor("dbg_k", [128, 4, S_TOK], BF16, kind=KS).ap()
        dbg_v = nc.dram_tensor("dbg_v", [128, NT, 4, 130], BF16, kind=KS).ap(); dbg_g = nc.dram_tensor("dbg_g", [128, 1024], BF16, kind=KS).ap(); dbg_v2 = nc.dram_tensor("dbg_v2", [128, NT, 4, 130], BF16, kind=KS).ap()
        dbg_r = nc.dram_tensor("dbg_r", [128, NT * 4 * 2 + NBLK], F32, kind=KS).ap()
    cata_d = nc.dram_tensor("cata_s", [128, 4, S_TOK], BF16, kind=KS).ap()
    wbf_d = [nc.dram_tensor("wbf_s%d" % i, [32 * 512, 2048], BF16, kind="Internal").ap() for i in range(3)]
    h1_d = nc.dram_tensor("h1_s", [S_TOK, 1024], F32, kind=KS).ap()
    xn2_d = nc.dram_tensor("xn2_s", [S_TOK, 1024], BF16, kind=KS).ap()
    xbuf_d = nc.dram_tensor("xbuf_s", [NSLOT, 1024], BF16, kind=KS).ap()
    ybuf_d = nc.dram_tensor("ybuf_s", [NSLOT, 1024], F32, kind=KS).ap()

    with ExitStack() as st:
        S = Sched(nc, st)

        def sb(name, shape, dt=F32, stack=st):
            return stack.enter_context(nc.sbuf_tensor("s_" + name, list(shape), dt))

        pb = [st.enter_context(nc.psum_tensor("pb%d" % i, [128, 512], F32)) for i in range(8)]
        pbb = [pb[i][:].bitcast(BF16) for i in range(8)]

        def barrier():
            toks = []
            for e in (PE, ACT, DVE, POOL, SP):
                if S.pending[e]:
                    last = S.ops[e][-1]
                    last.inc, last.incv = S.esem[e], 1
                    S.ecount[e] += 1
                    S.pending[e] = False
                if S.ecount[e] > 0:
                    toks.append(Tok(S.esem[e], S.ecount[e], e))
            for name, s in S.dsems.items():
                toks.append(Tok(s, S.dcount[id(s)], None))
            for e in (PE, ACT, DVE, POOL, SP):
                S.op(e, lambda h: h.nop(), extra=toks, inc=False if e == PE else None)
            S.writer.clear(); S.readers.clear()

        identb = sb("identb", [128, 128], BF16); identf = sb("identf", [128, 128]); onesf = sb("onesf", [128, 128])
        tri = sb("tri", [128, 128]); blk = sb("blk", [128, 128], BF16); rmat = sb("rmat", [128, 128], BF16)
        cmask = sb("cmask", [128, 128], BF16); freq = sb("freq", [128, 1]); invc = sb("invc", [128, 16])
        iota = sb("iota", [128, 32]); bstart = sb("bstart", [128, NBLK])
        qn = sb("qn", [128, 1]); kn = sb("kn", [128, 1]); lamv = sb("lamv", [128, 4, 64]); subln = sb("subln", [128, 128])
        pscale = sb("pscale", [128, 4]); gcol_attn = sb("gcol_attn", [128, 8]); gcol_ple = sb("gcol_ple", [128, 8])
        lam_c = sb("lam_c", [128, 4]); epsc = sb("epsc", [128, 1])
        OHall = sb("OHall", [128, NBLK], F32); ridx_i = sb("ridx_i", [128, NBLK], I32); pidx = sb("pidx", [128, 2], F32)
        S.op(DVE, lambda e: e.memset(epsc[:], EPS), writes=["epsc"])
        for i, (t_, d_) in enumerate([(identb, identb_d), (identf, identf_d), (onesf, onesf_d), (tri, tri_d), (blk, blk_d), (rmat, rmat_d),
                       (cmask, cmask_d), (freq, freq_d), (invc, invc_d), (iota, iota_d), (bstart, bstart_d), (qn, qn_d),
                       (kn, kn_d), (lamv, lamv_d), (subln, subln_d), (pscale, pscale_d), (gcol_attn, gcol_attn_d),
                       (gcol_ple, gcol_ple_d), (pidx, pidx_d)]):
            S.op(SP, lambda e, t_=t_, d_=d_: e.dma_start(out=t_[:], in_=d_), writes=["const"], dma="const")
        lamt = sb("lamt", [128, 2, 64])
        S.op(DVE, lambda e: e.tensor_tensor(out=lamt[:, 0, :], in0=lamv[:, 0, :], in1=lamv[:, 1, :], op=ALU.mult), reads=["const"], writes=["lamt"])
        S.op(DVE, lambda e: e.tensor_tensor(out=lamt[:, 1, :], in0=lamv[:, 2, :], in1=lamv[:, 3, :], op=ALU.mult), reads=["const"], writes=["lamt"])
        S.op(DVE, lambda e: e.tensor_reduce(out=lam_c[:, 0:2], in_=lamt[:], axis=AX.X, op=ALU.add), reads=["lamt"], writes=["lam_c"])
        S.op(ACT, lambda e: e.activation(out=lam_c[:, 0:2], in_=lam_c[:, 0:2], func=AF.Exp), reads=["lam_c"], writes=["lam_c"])
        S.op(DVE, lambda e: e.tensor_tensor(out=lam_c[:, 2:3], in0=lam_c[:, 1:2], in1=lam_c[:, 0:1], op=ALU.subtract), reads=["lam_c"], writes=["lam_c2"])
        S.op(DVE, lambda e: e.tensor_scalar(out=lam_c[:, 3:4], in0=lam_c[:, 2:3], scalar1=-LAMBDA_INIT, scalar2=None, op0=ALU.add), reads=["lam_c2"], writes=["nlam"])
        nlam = lam_c[:, 3:4]
        S.op(DVE, lambda e: e.tensor_scalar(out=subln[:], in0=subln[:], scalar1=1.0 - LAMBDA_INIT, scalar2=None, op0=ALU.mult), reads=["const"], writes=["const"])

        eb_i = sb("eb_i", [128, NBLK], I32); dest_i = sb("dest_i", [128, NT, 4], I32); gk = sb("gk", [128, NT, 4], F32)
        def _precast_gen():
            for e_ in range(32):
                for m_, wsrc in enumerate((wg_d, wu_d, wd_d)):
                    S.op(POOL, lambda e, m_=m_, e_=e_, wsrc=wsrc: e.dma_start(out=wbf_d[m_][e_ * 512:(e_ + 1) * 512, :], in_=wsrc[e_].rearrange("(r two) f -> r (two f)", two=2)),
                         writes=["wbf"], dma="precast")
                    yield
        _pc = _precast_gen()

        def precast(k_):
            for _ in range(k_):
                try:
                    next(_pc)
                except StopIteration:
                    return
        with ExitStack() as stAB:
            qT = sb("qT", [128, 4, S_TOK], BF16, stAB); kT = sb("kT", [128, 4, S_TOK], BF16, stAB)
            Vsb = sb("Vsb", [128, NT, 4, 130], BF16, stAB)
            S.op(POOL, lambda e: e.memset(Vsb[:, :, :, 128:130], 1.0), writes=["Vones"])
            with ExitStack() as stA:
                w_in = sb("w_in", [128, 8, 2048], BF16, stA)
                for c in range(8):
                    S.op(POOL, lambda e, c=c: e.dma_start(out=w_in[:, c, :], in_=w_in_d[c * 128:(c + 1) * 128, :]), writes=[("w_in", c)], dma="w_in")
                for c in range(8):
                    S.op(DVE, lambda e, c=c: e.tensor_scalar(out=w_in[:, c, :], in0=w_in[:, c, :], scalar1=gcol_attn[:, c:c + 1], scalar2=None, op0=ALU.mult),
                         reads=[("w_in", c), "const"], writes=[("w_in", c)])
                wpool = sb("wpool", [128, 4, 128], BF16, stA)
                for g in range(4):
                    S.op(POOL, lambda e, g=g: e.dma_start(out=wpool[:, g, :], in_=w_pool_d[g]), writes=["wpool"], dma="wpool")
                xt = [sb("xt%d" % i, [128, 1024], F32, stA) for i in range(2)]
                xjunk = sb("xjunk", [128, 1024], BF16, stA)
                xn = [sb("xn%d" % i, [128, 1024], BF16, stA) for i in range(2)]
                st1 = sb("st1", [128, 64], F32, stA)
                hnT = [sb("hnT0", [128, 8, 512], BF16, stA)] * 2
                posi = sb("posi", [128, 512], I32, stA)
                cosn = sb("cosn", [128, 512], F32, stA); sinn = sb("sinn", [128, 512], F32, stA)
                sq = [sb("sq%d" % i, [128, 512], BF16, stA) for i in range(2)]
                qg = [sb("qg%d" % i, [128, 512], BF16, stA) for i in range(2)]
                rstd_t = sb("rstd_t", [128, 512], F32, stA); t1 = sb("t1", [128, 512], F32, stA); t2 = sb("t2", [128, 512], F32, stA); ang = t1; ang2 = t2
                uT = [sb("uT0", [128, 4, 528], F32, stA)] * 2
                sA = sb("sA", [128, 528], F32, stA); sB = sb("sB", [128, 528], F32, stA)
                pooled = sb("pooled", [128, 4, 512], BF16, stA); catp = [sb("catp0", [128, 4, 512], BF16, stA)] * 2
                S.op(DVE, lambda e: e.memset(st1[:], 0.0), writes=["st1"])
                S.op(POOL, lambda e: e.memset(uT[0][:, :, 0:16], 0.0), writes=[("uT", 0)])
                for n in range(NG):
                    par = n % 2
                    hk = ("hnT", 0)
                    for j in range(4):
                        t = n * 4 + j
                        xk = ("xt", t % 2)
                        S.op(SP, lambda e, t=t: e.dma_start(out=xt[t % 2][:], in_=x_d[t * 128:(t + 1) * 128, :]), writes=[xk], dma="xt%d" % (t % 2))
                        S.op(ACT, lambda e, t=t: e.activation(out=xjunk[:], in_=xt[t % 2][:], func=AF.Square, accum_out=st1[:, t:t + 1]),
                             reads=[xk, "st1"], writes=["xjunk", ("ss", t)])
                        S.op(ACT, lambda e, t=t: e.activation(out=st1[:, 32 + t:33 + t], in_=st1[:, t:t + 1], func=AF.Ln, scale=1.0 / 1024, bias=epsc[:, 0:1]),
                             reads=[("ss", t)], writes=[("rs", t)])
                        S.op(ACT, lambda e, t=t: e.activation(out=st1[:, 32 + t:33 + t], in_=st1[:, 32 + t:33 + t], func=AF.Exp, scale=-0.5),
                             reads=[("rs", t)], writes=[("rs", t)])
                        S.op(ACT, lambda e, t=t: e.activation(out=xn[t % 2][:], in_=xt[t % 2][:], func=AF.Copy, scale=st1[:, 32 + t:33 + t]),
                             reads=[xk, ("rs", t)], writes=[("xn", t % 2)])
                        for c in range(8):
                            S.op(PE, lambda e, t=t, c=c: e.transpose(out=pbb[7][:, c * 128:(c + 1) * 128], in_=xn[t % 2][:, c * 128:(c + 1) * 128], identity=identb[:]),
                                 reads=[("xn", t % 2), "const"], writes=["pb7"], inc=(c == 7))
                        S.op(DVE, lambda e, j=j, par=par: e.tensor_copy(out=hnT[par][:, :, j * 128:(j + 1) * 128], in_=pbb[7].rearrange("p (c t) -> p c t", c=8)),
                             reads=["pb7"], writes=[hk])
                    precast(4)
                    S.op(SP, lambda e, n=n: e.dma_start(out=posi[:], in_=pos_d[:, n * 512:(n + 1) * 512]), writes=["posi"], dma="posi")
                    S.op(DVE, lambda e: e.tensor_copy(out=ang[:], in_=posi[:]), reads=["posi"], writes=["t1"])
                    S.op(DVE, lambda e: e.tensor_scalar(out=ang[:], in0=ang[:], scalar1=freq[:, 0:1], scalar2=None, op0=ALU.mult), reads=["t1", "const"], writes=["t1"])
                    S.op(DVE, lambda e: e.tensor_scalar(out=ang2[:], in0=ang[:], scalar1=math.pi / 2, scalar2=None, op0=ALU.add), reads=["t1"], writes=["t2"])
                    for (aa, ak) in ((ang, "t1"), (ang2, "t2")):
                        S.op(DVE, lambda e, aa=aa: e.tensor_scalar(out=rstd_t[:], in0=aa[:], scalar1=1.0 / (2 * math.pi), scalar2=None, op0=ALU.mult), reads=[ak], writes=["rstd_t"])
                        S.op(DVE, lambda e: e.tensor_copy(out=posi[:], in_=rstd_t[:]), reads=["rstd_t"], writes=["posi"])
                        S.op(DVE, lambda e: e.tensor_copy(out=rstd_t[:], in_=posi[:]), reads=["posi"], writes=["rstd_t"])
                        S.op(DVE, lambda e, aa=aa: e.scalar_tensor_tensor(out=aa[:], in0=rstd_t[:], scalar=-2 * math.pi, in1=aa[:], op0=ALU.mult, op1=ALU.add), reads=["rstd_t", ak], writes=[ak])
                        S.op(DVE, lambda e, aa=aa: e.tensor_scalar(out=rstd_t[:], in0=aa[:], scalar1=math.pi, scalar2=-2 * math.pi, op0=ALU.is_gt, op1=ALU.mult), reads=[ak], writes=["rstd_t"])
                        S.op(DVE, lambda e, aa=aa: e.tensor_tensor(out=aa[:], in0=aa[:], in1=rstd_t[:], op=ALU.add), reads=[ak, "rstd_t"], writes=[ak])
                        S.op(DVE, lambda e, aa=aa: e.tensor_scalar(out=rstd_t[:], in0=aa[:], scalar1=-math.pi, scalar2=2 * math.pi, op0=ALU.is_lt, op1=ALU.mult), reads=[ak], writes=["rstd_t"])
                        S.op(DVE, lambda e, aa=aa: e.tensor_tensor(out=aa[:], in0=aa[:], in1=rstd_t[:], op=ALU.add), reads=[ak, "rstd_t"], writes=[ak])
                    S.op(ACT, lambda e: e.activation(out=sinn[:], in_=ang[:], func=AF.Sin), reads=["t1"], writes=["sinn"])
                    S.op(ACT, lambda e: e.activation(out=cosn[:], in_=ang2[:], func=AF.Sin), reads=["t2"], writes=["cosn"])
                    for ch in range(8):
                        pa = pb[ch % 2]; pak = "pb%d" % (ch % 2)
                        sp_ = ch % 2
                        for c in range(8):
                            S.op(PE, lambda e, c=c, ch=ch, pa=pa, par=par: e.matmul(pa[:], lhsT=w_in[:, c, ch * 128:(ch + 1) * 128], rhs=hnT[par][:, c, :], start=(c == 0), stop=(c == 7)),
                                 reads=[hk, ("w_in", c)], writes=[pak], inc=(c == 7))
                        S.op(ACT, lambda e, pa=pa, sp_=sp_: e.activation(out=sq[sp_][:], in_=pa[:], func=AF.Square), reads=[pak], writes=[("sq", sp_)])
                        gc = qn if ch < 4 else kn
                        S.op(ACT, lambda e, pa=pa, sp_=sp_, gc=gc: e.activation(out=qg[sp_][:], in_=pa[:], func=AF.Copy, scale=gc[:, 0:1]), reads=[pak, "const"], writes=[("qg", sp_)])
                        pm = pb[2 + (ch % 2)]; pmk = "pb%d" % (2 + ch % 2)
                        pr = pb[4 + (ch % 2)]; prk = "pb%d" % (4 + ch % 2)
                        S.op(PE, lambda e, pm=pm, sp_=sp_: e.matmul(pm[:], lhsT=blk[:], rhs=sq[sp_][:], start=True, stop=True), reads=[("sq", sp_), "const"], writes=[pmk], inc=True)
                        S.op(PE, lambda e, pr=pr, sp_=sp_: e.matmul(pr[:], lhsT=rmat[:], rhs=qg[sp_][:], start=True, stop=True), reads=[("qg", sp_), "const"], writes=[prk], inc=True)
                        S.op(ACT, lambda e, pm=pm: e.activation(out=rstd_t[:], in_=pm[:], func=AF.Ln, bias=epsc[:, 0:1]), reads=[pmk], writes=["rstd_t"])
                        S.op(ACT, lambda e: e.activation(out=rstd_t[:], in_=rstd_t[:], func=AF.Exp, scale=-0.5), reads=["rstd_t"], writes=["rstd_t"])
                        S.op(POOL, lambda e, sp_=sp_: e.tensor_tensor(out=t1[:], in0=qg[sp_][:], in1=cosn[:], op=ALU.mult), reads=[("qg", sp_), "cosn"], writes=["t1"])
                        S.op(DVE, lambda e, pr=pr: e.tensor_tensor(out=t2[:], in0=pr[:], in1=sinn[:], op=ALU.mult), reads=[prk, "sinn"], writes=["t2"])
                        S.op(DVE, lambda e: e.tensor_tensor(out=t1[:], in0=t1[:], in1=t2[:], op=ALU.add), reads=["t1", "t2"], writes=["t1"])
                        dst = (qT if ch < 4 else kT)
                        S.op(DVE, lambda e, dst=dst, ch=ch, n=n: e.tensor_tensor(out=dst[:, ch % 4, n * 512:(n + 1) * 512], in0=t1[:], in1=rstd_t[:], op=ALU.mult),
                             reads=["t1", "rstd_t"], writes=[("qk", ch, n)])
                    for j in range(4):
                        t = n * 4 + j
                        for c in range(8):
                            S.op(PE, lambda e, c=c, j=j, par=par: e.matmul(pb[6][:], lhsT=hnT[par][:, c, j * 128:(j + 1) * 128], rhs=w_in[:, c, 1024:1536], start=(c == 0), stop=(c == 7)),
                                 reads=[hk, ("w_in", c)], writes=["pb6"], inc=(c == 7))
                        S.op(ACT, lambda e, t=t: e.activation(out=Vsb[:, t, :, 0:128], in_=pb[6][:].rearrange("p (h v) -> p h v", h=4), func=AF.Copy),
                             reads=["pb6"], writes=[("V", t)])
                    uk = ("uT", 0)
                    if n > 0:
                        S.op(POOL, lambda e, par=par: e.tensor_copy(out=uT[par][:, :, 0:16], in_=uT[1 - par][:, :, 512:528]), reads=[("uT", 0)], writes=[uk])
                    for g in range(4):
                        for c in range(8):
                            S.op(PE, lambda e, c=c, g=g, par=par: e.matmul(pb[6][:], lhsT=w_in[:, c, 1536 + g * 128:1536 + (g + 1) * 128], rhs=hnT[par][:, c, :], start=(c == 0), stop=(c == 7)),
                                 reads=[hk, ("w_in", c)], writes=["pb6"], inc=(c == 7))
                        S.op(ACT, lambda e, g=g, par=par: e.activation(out=uT[par][:, g, 16:528], in_=pb[6][:], func=AF.Copy), reads=["pb6"], writes=[uk])
                        src = uT[par][:, g, :]
                        bufs = [sA, sB]
                        cur, curk = src, uk
                        for jj in range(g + 1):
                            sh = 1 << jj
                            lo = (1 << (jj + 1)) - 1
                            o = bufs[jj % 2]; ok_ = "sA" if jj % 2 == 0 else "sB"
                            S.op(POOL, lambda e, o=o, cur=cur, lo=lo, sh=sh: e.tensor_tensor(out=o[:, lo:528], in0=cur[:, lo:528], in1=cur[:, lo - sh:528 - sh], op=ALU.add),
                                 reads=[curk], writes=[ok_])
                            cur, curk = o, ok_
                        w = 1 << (g + 1)
                        S.op(DVE, lambda e, g=g, cur=cur, src=src, w=w: e.scalar_tensor_tensor(out=pooled[:, g, :], in0=cur[:, 16:528], scalar=1.0 / w, in1=src[:, 16:528], op0=ALU.mult, op1=ALU.subtract),
                             reads=[curk, uk], writes=["pooled"])
                        if n == 0:
                            S.op(DVE, lambda e, cur=cur, w=w: e.tensor_tensor(out=cur[:, 16:16 + w - 1], in0=cur[:, 16:16 + w - 1], in1=invc[:, 0:w - 1], op=ALU.mult),
                                 reads=[curk, "const"], writes=[curk])
                            S.op(DVE, lambda e, g=g, cur=cur, src=src, w=w: e.tensor_tensor(out=pooled[:, g, 0:w - 1], in0=cur[:, 16:16 + w - 1], in1=src[:, 16:16 + w - 1], op=ALU.subtract),
                                 reads=[curk, uk], writes=["pooled"])
                        S.op(PE, lambda e, g=g: e.matmul(pb[6][:], lhsT=wpool[:, g, :], rhs=pooled[:, g, :], start=True, stop=True), reads=["pooled", "wpool"], writes=["pb6"], inc=True)
                        S.op(ACT, lambda e, g=g, par=par: e.activation(out=catp[par][:, g, :], in_=pb[6][:], func=AF.Copy, scale=pscale[:, g:g + 1]), reads=["pb6", "const"], writes=[("catp", 0)])
                    for g in range(4):
                        S.op(POOL, lambda e, g=g, n=n, par=par: e.dma_start(out=catp_d[g, :, n * 512:(n + 1) * 512], in_=catp[par][:, g, :]), reads=[("catp", 0)], writes=["catp_d"], dma="catp_st")

            barrier()
            if DEBUG and _os.environ.get("KEARLY", "1") == "1":
                S.op(SP, lambda e: e.dma_start(out=dbg_q, in_=qT[:]), dma="dbg")
                S.op(SP, lambda e: e.dma_start(out=dbg_k, in_=kT[:]), dma="dbg")
                S.op(SP, lambda e: e.dma_start(out=dbg_v, in_=Vsb[:]), dma="dbg")
            with ExitStack() as stB:
                catA = sb("catA", [128, 4, S_TOK], BF16, stB)
                PT = [[sb("PT%d_%d" % (i, c), [128, 512], BF16, stB) for c in range(2)] for i in range(2)]
                otmp = sb("otmp", [128, 128], F32, stB); ojunk = sb("ojunk", [128, 128], BF16, stB)
                ob = sb("ob", [128, 4, 128], BF16, stB); st2 = sb("st2", [128, 8], F32, stB)

                def Oap(i, lo, hi):
                    return pb[4 + i // 3][:, (i % 3) * 160 + lo:(i % 3) * 160 + hi]
                osb = [sb("osb%d" % i, [128, 512], F32, stB) for i in range(3)]

                def Osb(i, lo, hi):
                    return osb[i // 3][:, (i % 3) * 160 + lo:(i % 3) * 160 + hi]

                def SE(h, n, kt):
                    bufi = kt % 2
                    j = kt - 4 * n
                    qlo = max(j, 0) * 128
                    for c in range(2):
                        ps = pb[bufi * 2 + c]; psk = "pb%d" % (bufi * 2 + c)
                        S.op(PE, lambda e, ps=ps, c=c: e.matmul(ps[:, qlo:512], lhsT=kT[c * 64:(c + 1) * 64, h, kt * 128:(kt + 1) * 128],
                             rhs=qT[c * 64:(c + 1) * 64, h, n * 512 + qlo:(n + 1) * 512], start=True, stop=True), writes=[psk], inc=True)
                        S.op(ACT, lambda e, ps=ps, c=c: e.activation(out=PT[bufi][c][:, qlo:512], in_=ps[:, qlo:512], func=AF.Exp, scale=0.125),
                             reads=[psk], writes=[("PT", bufi, c)])
                        if j >= 0:
                            S.op(POOL, lambda e, c=c: e.tensor_tensor(out=PT[bufi][c][:, qlo:qlo + 128], in0=PT[bufi][c][:, qlo:qlo + 128], in1=cmask[:], op=ALU.mult),
                                 reads=[("PT", bufi, c)], writes=[("PT", bufi, c)])

                def AVm(h, n, kt):
                    bufi = kt % 2
                    j = kt - 4 * n
                    for qi in range(max(j, 0), 4):
                        for c in range(2):
                            i = qi * 2 + c
                            S.op(PE, lambda e, i=i, c=c, qi=qi: e.matmul(Oap(i, 0, 129), lhsT=PT[bufi][c][:, qi * 128:(qi + 1) * 128],
                                 rhs=Vsb[:, kt, h, 0:129], start=(kt == 0 and i % 3 == 0), stop=((i, kt - 4 * n) in ((2, 1), (5, 2), (7, 3)))), reads=[("PT", bufi, c)], writes=[("O", i // 3)],
                                 inc=(qi == 3 and c == 1))

                def post(h, n):
                    S.op(ACT, lambda e: e.activation(out=osb[0][:], in_=pb[4][:], func=AF.Copy), reads=[("O", 0)], writes=[("osb", 0)])
                    S.op(DVE, lambda e: e.tensor_copy(out=osb[1][:], in_=pb[5][:]), reads=[("O", 1)], writes=[("osb", 1)])
                    S.op(ACT, lambda e: e.activation(out=osb[2][:, 0:320], in_=pb[6][:, 0:320], func=AF.Copy), reads=[("O", 2)], writes=[("osb", 2)])
                    for qi in range(4):
                        i1, i2 = qi * 2, qi * 2 + 1
                        k1, k2 = ("osb", i1 // 3), ("osb", i2 // 3)
                        S.op(DVE, lambda e, i1=i1: e.reciprocal(out=st2[:, 0:1], in_=Osb(i1, 128, 129)), reads=[k1], writes=["st2a"])
                        S.op(DVE, lambda e, i2=i2: e.reciprocal(out=st2[:, 1:2], in_=Osb(i2, 128, 129)), reads=[k2], writes=["st2b"])
                        S.op(DVE, lambda e: e.tensor_tensor(out=st2[:, 1:2], in0=st2[:, 1:2], in1=nlam, op=ALU.mult), reads=["st2b"], writes=["st2b"])
                        S.op(DVE, lambda e, i1=i1: e.tensor_scalar(out=otmp[:], in0=Osb(i1, 0, 128), scalar1=st2[:, 0:1], scalar2=None, op0=ALU.mult), reads=[k1, "st2a"], writes=["otmp"])
                        S.op(DVE, lambda e, i2=i2: e.scalar_tensor_tensor(out=otmp[:], in0=Osb(i2, 0, 128), scalar=st2[:, 1:2], in1=otmp[:], op0=ALU.mult, op1=ALU.add),
                             reads=[k2, "st2b", "otmp"], writes=["otmp"])
                        S.op(DVE, lambda e: e.memset(st2[:, 2:3], 0.0), writes=["st2c"])
                        S.op(ACT, lambda e: e.activation(out=ojunk[:], in_=otmp[:], func=AF.Square, accum_out=st2[:, 2:3]), reads=["otmp", "st2c"], writes=["ojunk", "st2c"])
                        S.op(ACT, lambda e: e.activation(out=st2[:, 3:4], in_=st2[:, 2:3], func=AF.Ln, scale=1.0 / 128, bias=epsc[:, 0:1]), reads=["st2c"], writes=["st2d"])
                        S.op(ACT, lambda e: e.activation(out=st2[:, 3:4], in_=st2[:, 3:4], func=AF.Exp, scale=-0.5), reads=["st2d"], writes=["st2d"])
                        S.op(DVE, lambda e, qi=qi: e.scalar_tensor_tensor(out=ob[:, qi, :], in0=otmp[:], scalar=st2[:, 3:4], in1=subln[:], op0=ALU.mult, op1=ALU.mult),
                             reads=["otmp", "st2d"], writes=["ob"])

                def fin(h, n):
                    for qi in range(4):
                        S.op(PE, lambda e, qi=qi: e.transpose(out=pbb[7][:, qi * 128:(qi + 1) * 128], in_=ob[:, qi, :], identity=identb[:]), reads=["ob"], writes=["pb7"], inc=(qi == 3))
                    S.op(DVE, lambda e: e.tensor_copy(out=catA[:, h, n * 512:(n + 1) * 512], in_=pbb[7][:, 0:512]), reads=["pb7"], writes=[("catA", h)])

                heads = ([int(c) for c in _os.environ.get('KHEADS', '0123')] if stage >= 2 else [])
                groups = [(h, n) for h in heads for n in range(NG)]
                pending_fin = None
                for gi, (h, n) in enumerate(groups):
                    nkt = 4 * n + 4
                    precast(2)
                    SE(h, n, 0)
                    for kt in range(nkt):
                        if kt + 1 < nkt:
                            SE(h, n, kt + 1)
                        AVm(h, n, kt)
                        if kt == min(2, nkt - 1) and pending_fin is not None:
                            fin(*pending_fin)
                            ph = pending_fin[0]
                            if pending_fin[1] == NG - 1:
                                S.op(SP, lambda e, ph=ph: e.dma_start(out=cata_d[:, ph, :], in_=catA[:, ph, :]), reads=[("catA", ph)], writes=["cata_d"], dma="cata_st")
                            pending_fin = None
                    post(h, n)
                    pending_fin = (h, n)
                if pending_fin is not None:
                    fin(*pending_fin)
                    ph = pending_fin[0]
                    S.op(SP, lambda e, ph=ph: e.dma_start(out=cata_d[:, ph, :], in_=catA[:, ph, :]), reads=[("catA", ph)], writes=["cata_d"], dma="cata_st")
            if DEBUG:
                barrier()
                S.op(SP, lambda e: e.dma_start(out=dbg_v2, in_=Vsb[:]), dma="dbg")
        barrier()
        with ExitStack() as stC:
            precast(96)
            w_out = sb("w_out", [128, 8, 1024], BF16, stC)
            for c in range(8):
                S.op(POOL, lambda e, c=c: e.dma_start(out=w_out[:, c, :], in_=w_out_d[c * 128:(c + 1) * 128, :]), writes=["w_out"], dma="w_out")
            catP = sb("catP", [128, 4, S_TOK], BF16, stC); catA2 = sb("catA2", [128, 4, S_TOK], BF16, stC)
            for g in range(4):
                S.op(SP, lambda e, g=g: e.dma_start(out=catA2[:, g, :], in_=cata_d[:, g, :]), writes=["catP"], dma="catP")
            for g in range(4):
                S.op(SP, lambda e, g=g: e.dma_start(out=catP[:, g, :], in_=catp_d[g]), writes=["catP"], dma="catP")
            ffn_b = sb("ffn_b", [128, 1024], F32, stC); w_r = sb("w_r", [128, 8, 32], F32, stC); b_r = sb("b_r", [128, 32], F32, stC)
            S.op(SP, lambda e: e.dma_start(out=ffn_b[:], in_=ffn_b_d), writes=["cC"], dma="cC")
            S.op(SP, lambda e: e.dma_start(out=w_r[:], in_=w_r_d), writes=["cC"], dma="cC")
            S.op(SP, lambda e: e.dma_start(out=b_r[:], in_=b_r_d), writes=["cC"], dma="cC")
            xt2 = [sb("xt2_%d" % i, [128, 1024], F32, stC) for i in range(2)]
            h1t = [sb("h1t%d" % i, [128, 1024], F32, stC) for i in range(2)]
            xn2f = sb("xn2f", [128, 1024], F32, stC); xn2b = [sb("xn2b%d" % i, [128, 1024], BF16, stC) for i in range(2)]
            cjunk = sb("cjunk", [128, 1024], BF16, stC)
            xn2T = sb("xn2T", [128, 1024], F32, stC)
            st3 = sb("st3", [128, 64], F32, stC)
            lgall = sb("lgall", [128, NT, 32], F32, stC); top8 = sb("top8", [128, NT, 8], F32, stC); idx8 = sb("idx8", [128, NT, 8], U32, stC)
            maskall = sb("maskall", [128, NT, 32], F32, stC); Gall = sb("Gall", [128, NT, 32], F32, stC)
            S.op(DVE, lambda e: e.memset(st3[:], 0.0), writes=["st3"])
            NTC = NT if stage >= 3 else 0
            for t in range(NTC):
                xk = ("xt2", t % 2); hk2 = ("h1t", t % 2)
                S.op(SP, lambda e, t=t: e.dma_start(out=xt2[t % 2][:], in_=x_d[t * 128:(t + 1) * 128, :]), writes=[xk], dma="xt2_%d" % (t % 2))
                for hf in range(2):
                    for c in range(8):
                        cat_c = catA2[:, c, t * 128:(t + 1) * 128] if c < 4 else catP[:, c - 4, t * 128:(t + 1) * 128]
                        S.op(PE, lambda e, cat_c=cat_c, c=c, hf=hf: e.matmul(pb[hf][:], lhsT=cat_c, rhs=w_out[:, c, hf * 512:(hf + 1) * 512], start=(c == 0), stop=(c == 7)),
                             reads=["w_out", "catP"], writes=["pb%d" % hf], inc=(c == 7))
                    S.op(DVE, lambda e, t=t, hf=hf: e.tensor_tensor(out=h1t[t % 2][:, hf * 512:(hf + 1) * 512], in0=pb[hf][:], in1=xt2[t % 2][:, hf * 512:(hf + 1) * 512], op=ALU.add),
                         reads=["pb%d" % hf, xk], writes=[hk2])
                S.op(POOL, lambda e, t=t: e.dma_start(out=h1_d[t * 128:(t + 1) * 128, :], in_=h1t[t % 2][:]), reads=[hk2], writes=["h1_d"], dma="h1st%d" % (t % 2))
                S.op(ACT, lambda e, t=t: e.activation(out=cjunk[:], in_=h1t[t % 2][:], func=AF.Square, accum_out=st3[:, t:t + 1]), reads=[hk2, "st3"], writes=["cjunk", ("ss3", t)])
                S.op(ACT, lambda e, t=t: e.activation(out=st3[:, 32 + t:33 + t], in_=st3[:, t:t + 1], func=AF.Ln, scale=1.0 / 1024, bias=epsc[:, 0:1]), reads=[("ss3", t)], writes=[("rs3", t)])
                S.op(ACT, lambda e, t=t: e.activation(out=st3[:, 32 + t:33 + t], in_=st3[:, 32 + t:33 + t], func=AF.Exp, scale=-0.5), reads=[("rs3", t)], writes=[("rs3", t)])
                S.op(DVE, lambda e, t=t: e.scalar_tensor_tensor(out=xn2f[:], in0=h1t[t % 2][:], scalar=st3[:, 32 + t:33 + t], in1=ffn_b[:], op0=ALU.mult, op1=ALU.mult),
                     reads=[hk2, ("rs3", t), "cC"], writes=["xn2f"])
                S.op(ACT, lambda e, t=t: e.activation(out=xn2b[t % 2][:].rearrange("s (c p) -> s c p", p=128), in_=xn2f[:].rearrange("s (p c) -> s c p", c=8), func=AF.Copy), reads=["xn2f"], writes=[("xn2b", t % 2)])
                S.op(POOL, lambda e, t=t: e.dma_start(out=xn2_d[t * 128:(t + 1) * 128, :], in_=xn2b[t % 2][:]), reads=[("xn2b", t % 2)], writes=["xn2_d"], dma="xn2st%d" % (t % 2))
                for c in range(8):
                    bank = 2 + c // 4
                    S.op(PE, lambda e, c=c, bank=bank: e.transpose(out=pb[bank][:, (c % 4) * 128:(c % 4 + 1) * 128], in_=xn2f[:, c * 128:(c + 1) * 128], identity=identf[:]),
                         reads=["xn2f"], writes=["pb%d" % bank], inc=(c % 4 == 3))
                S.op(ACT, lambda e: e.activation(out=xn2T[:, 0:512], in_=pb[2][:], func=AF.Copy), reads=["pb2"], writes=["xn2Ta"])
                S.op(DVE, lambda e: e.tensor_copy(out=xn2T[:, 512:1024], in_=pb[3][:]), reads=["pb3"], writes=["xn2Tb"])
                for c in range(8):
                    S.op(PE, lambda e, c=c: e.matmul(pb[4][:, 0:32], lhsT=xn2T[:, c * 128:(c + 1) * 128], rhs=w_r[:, c, :], start=(c == 0), stop=(c == 7)),
                         reads=["xn2Ta", "xn2Tb", "cC"], writes=["pb4"], inc=(c == 7))
                S.op(DVE, lambda e, t=t: e.tensor_tensor(out=lgall[:, t, :], in0=pb[4][:, 0:32], in1=b_r[:], op=ALU.add), reads=["pb4", "cC"], writes=[("lg", t)])
                S.op(DVE, lambda e, t=t: e.max(out=top8[:, t, :], in_=lgall[:, t, :]), reads=[("lg", t)], writes=[("top8", t)])
                S.op(DVE, lambda e, t=t: e.max_index(out=idx8[:, t, :], in_max=top8[:, t, :], in_values=lgall[:, t, :]), reads=[("lg", t), ("top8", t)], writes=[("idx8", t)])
            if stage >= 3:
                dispatch(nc, S, sb, stC, pb, lgall, top8, idx8, maskall, Gall, onesf, tri, iota, bstart, eb_i, dest_i, gk, xn2_d, xbuf_d, pidx, OHall, ridx_i)
        barrier()
        if stage >= 4:
            moe_blocks(nc, S, sb, st, pb, pbb, OHall, ridx_i, wbf_d, btab_d, xbuf_d, ybuf_d, identb)
            barrier()
        if stage >= 5:
            ple_phase(nc, S, sb, st, pb, pbb, dest_i, gk, h1_d, ybuf_d, p_d, w_pg_d, w_pp_d, post_b_d, gcol_ple, identb, out_d, epsc)
        fin = [Tok(s, S.dcount[id(s)], None) for s in S.dsems.values()]
        S.finish(fin)
    return nc

_NC_CACHE = {}


def _consts():
    bf = ml_dtypes.bfloat16
    c = {}
    c["ident_bf"] = np.eye(128, dtype=np.float32).astype(bf)
    c["ident_f"] = np.eye(128, dtype=np.float32)
    c["ones_f"] = np.ones((128, 128), np.float32)
    k = np.arange(128)
    c["tri_strict"] = (k[:, None] < k[None, :]).astype(np.float32)
    c["cmask"] = (k[:, None] <= k[None, :]).astype(np.float32).astype(bf)
    blk = np.zeros((128, 128), np.float32)
    blk[:64, :64] = 1.0 / 64; blk[64:, 64:] = 1.0 / 64
    c["blkdiag"] = blk.astype(bf)
    r = np.zeros((128, 128), np.float32)
    for base in (0, 64):
        for m in range(8):
            r[base + m + 8, base + m] = -1.0
            r[base + m, base + m + 8] = 1.0
    c["rmat"] = r.astype(bf)
    freqs = (np.float32(500000.0) ** (-np.arange(0, 16, 2, dtype=np.float32) / np.float32(16))).astype(np.float32)
    f = np.zeros((128, 1), np.float32)
    for base in (0, 64):
        for i in range(8):
            f[base + i, 0] = freqs[i]; f[base + 8 + i, 0] = freqs[i]
    c["freq_col"] = f
    c["invcnt"] = np.broadcast_to((1.0 / np.arange(1, 17, dtype=np.float32))[None, :], (128, 16)).copy()
    c["iota_row"] = np.broadcast_to(np.arange(32, dtype=np.float32)[None, :], (128, 32)).copy()
    c["pidx_col"] = np.stack([np.arange(128, dtype=np.float32), (np.arange(128) % 32).astype(np.float32)], axis=1)
    c["bstart_row"] = np.broadcast_to((128.0 * np.arange(NBLK, dtype=np.float32))[None, :], (128, NBLK)).copy()
    return c


def _rep(v, n=128):
    return np.ascontiguousarray(np.broadcast_to(np.asarray(v)[None, ...], (n,) + tuple(np.asarray(v).shape)))


def make_in_maps(inputs):
    f32 = np.float32
    g = {k: np.asarray(v) for k, v in inputs.items()}
    shared = dict(_consts())
    shared["w_in"] = np.ascontiguousarray(g["w_in"][0], f32)
    shared["gcol_attn"] = np.ascontiguousarray(g["attn_norm"][0].reshape(8, 128).T)
    shared["qn_col"] = np.ascontiguousarray(np.tile(g["q_norm"][0], 2).reshape(128, 1))
    shared["kn_col"] = np.ascontiguousarray(np.tile(g["k_norm"][0], 2).reshape(128, 1))
    shared["lamv"] = _rep(np.stack([g["lam_q1"][0], g["lam_k1"][0], g["lam_q2"][0], g["lam_k2"][0]], 0))
    shared["subln_b"] = _rep(g["subln"][0])
    shared["w_pool"] = np.ascontiguousarray(g["w_pool"][0])
    shared["pscale_col"] = np.ascontiguousarray(g["pool_scale"][0].reshape(4, 128).T)
    shared["w_out"] = np.ascontiguousarray(g["w_out"][0])
    shared["ffn_b"] = _rep(g["ffn_norm"][0])
    shared["w_router"] = np.ascontiguousarray(g["w_router"][0].reshape(8, 128, 32).transpose(1, 0, 2))
    shared["b_router_b"] = _rep(g["b_router"][0])
    shared["w_gate"] = np.ascontiguousarray(g["w_gate"][0]); shared["w_up"] = np.ascontiguousarray(g["w_up"][0]); shared["w_down"] = np.ascontiguousarray(g["w_down"][0])
    shared["btab"] = np.ascontiguousarray(np.concatenate([g["b_gate"][0], g["b_up"][0], g["b_down"][0]], axis=1))
    shared["gcol_ple"] = np.ascontiguousarray(g["ple_gate_norm"][0].reshape(8, 128).T)
    shared["w_ple_gate"] = np.ascontiguousarray(g["w_ple_gate"][0]); shared["w_ple_proj"] = np.ascontiguousarray(g["w_ple_proj"][0])
    shared["post_b"] = _rep(g["ple_post_norm"][0])
    maps = []
    for b in range(8):
        m = dict(shared)
        m["x"] = np.ascontiguousarray(g["x"][b]); m["p"] = np.ascontiguousarray(g["p"][0, b])
        m["posb"] = _rep(g["positions"][b].astype(np.int32))
        maps.append(m)
    return maps


def kernel(**inputs):
    if "nc" not in _NC_CACHE:
        _NC_CACHE["nc"] = build()
    nc = _NC_CACHE["nc"]
    maps = make_in_maps(inputs)
    res = run_bass_kernel_spmd(nc, maps, core_ids=list(range(8)))
    return np.stack([np.asarray(r["out"]) for r in res.results], axis=0).astype(np.float32)
```

```python
import numpy as np
import concourse.bass as bass
import concourse.mybir as mybir

F32 = mybir.dt.float32
BF16 = mybir.dt.bfloat16
I32 = mybir.dt.int32
U32 = mybir.dt.uint32
ALU = mybir.AluOpType
AF = mybir.ActivationFunctionType
AX = mybir.AxisListType

PE, ACT, DVE, POOL, SP = "tensor", "scalar", "vector", "gpsimd", "sync"


class Tok:
    __slots__ = ("sem", "val", "eng")

    def __init__(self, sem, val, eng):
        self.sem, self.val, self.eng = sem, val, eng


class Rec:
    __slots__ = ("fn", "waits", "inc", "incv")

    def __init__(self, fn):
        self.fn, self.waits, self.inc, self.incv = fn, [], None, 0


class Sched:
    def __init__(self, nc, stack, n_dma_sems=96):
        self.nc = nc
        self.stack = stack
        self.ops = {e: [] for e in (PE, ACT, DVE, POOL, SP)}
        self.esem = {e: stack.enter_context(nc.semaphore("s_" + e)) for e in self.ops}
        self.ecount = {e: 0 for e in self.ops}
        self.pending = {e: False for e in self.ops}
        self.waited = {e: {} for e in self.ops}
        self.dsems = {}
        self.dcount = {}
        self.free_dsems = [stack.enter_context(nc.semaphore("d%d" % i)) for i in range(n_dma_sems)]
        self.writer = {}
        self.readers = {}

    def _need(self, eng, tok, same_ok):
        if tok is None:
            return None
        if tok.eng is not None:
            if self.ecount[tok.eng] < tok.val:
                last = self.ops[tok.eng][-1]
                assert last.inc is None
                last.inc, last.incv = self.esem[tok.eng], 1
                self.ecount[tok.eng] += 1
                self.pending[tok.eng] = False
                assert self.ecount[tok.eng] == tok.val
            val = tok.val
        else:
            val = self.dcount[id(tok.sem)]
        w = self.waited[eng]
        if w.get(id(tok.sem), 0) >= val:
            return None
        w[id(tok.sem)] = val
        return (tok.sem, val)

    _cap = None

    def begin_capture(self):
        self._cap = []

    def end_capture(self):
        c, self._cap = self._cap, None
        return c

    def replay_interleaved(self, lists):
        lists = [x for x in lists if x]
        if not lists:
            return
        L = max(len(x) for x in lists)
        for i in range(L):
            for x in lists:
                for j in range((i * len(x)) // L, ((i + 1) * len(x)) // L):
                    self.op(*x[j])

    def op(self, eng, fn, reads=(), writes=(), inc=None, dma=None, extra=()):
        if self._cap is not None:
            self._cap.append((eng, fn, tuple(reads), tuple(writes), inc, dma, tuple(extra)))
            return None
        raw, other = [], []
        for k in reads:
            t = self.writer.get(k)
            if t is not None:
                raw.append(t)
        for k in writes:
            t = self.writer.get(k)
            if t is not None:
                other.append(t)
            other.extend(self.readers.get(k, ()))
        raw.extend(extra)
        rec = Rec(fn)
        is_dma = dma is not None
        for t in raw:
            if t.eng == eng and not is_dma and eng == PE:
                continue
            wt = self._need(eng, t, False)
            if wt:
                rec.waits.append(wt)
        for t in other:
            if t.eng == eng and not is_dma and eng == PE:
                continue
            if is_dma and t.eng is None and dma in self.dsems and t.sem is self.dsems[dma]:
                continue
            wt = self._need(eng, t, False)
            if wt:
                rec.waits.append(wt)
        self.ops[eng].append(rec)
        if is_dma:
            if dma not in self.dsems:
                self.dsems[dma] = self.free_dsems.pop()
                self.dcount[id(self.dsems[dma])] = 0
            s = self.dsems[dma]
            self.dcount[id(s)] += 16
            rec.inc, rec.incv = s, 16
            tok = Tok(s, self.dcount[id(s)], None)
        else:
            if inc is None:
                inc = eng != PE
            if inc:
                self.ecount[eng] += 1
                rec.inc, rec.incv = self.esem[eng], 1
                self.pending[eng] = False
                tok = Tok(self.esem[eng], self.ecount[eng], eng)
            else:
                self.pending[eng] = True
                tok = Tok(self.esem[eng], self.ecount[eng] + 1, eng)
        for k in reads:
            self.readers.setdefault(k, []).append(tok)
        for k in writes:
            self.writer[k] = tok
            self.readers[k] = []
        return tok

    def finish(self, final_toks):
        rec = Rec(lambda e: e.nop())
        for t in final_toks:
            wt = self._need(SP, t, False)
            if wt:
                rec.waits.append(wt)
        self.ops[SP].append(rec)
        nc = self.nc
        with nc.Block() as block:
            def emit(name):
                def run(e):
                    for r in self.ops[name]:
                        for (s, v) in r.waits:
                            e.wait_ge(s, v)
                        ins = r.fn(e)
                        if r.inc is not None:
                            ins.then_inc(r.inc, r.incv)
                return run
            block.tensor(emit(PE))
            block.scalar(emit(ACT))
            block.vector(emit(DVE))
            block.gpsimd(emit(POOL))
            block.sync(emit(SP))
def dispatch(nc, S, sb, stC, pb, lgall, top8, idx8, maskall, Gall, onesf, tri, iota, bstart, eb_i, dest_i, gk, xn2_d, xbuf_d, pidx, OHall, ridx_i):
    NT, NBLK = 32, 160
    allk = [("lg", t) for t in range(NT)]
    exl = sb("exl", [128, NT, 32], F32, stC); sums = sb("sums", [128, NT], F32, stC)
    negmax = sb("negmax", [128, NT, 1], F32, stC)
    S.op(DVE, lambda e: e.tensor_tensor(out=maskall[:], in0=lgall[:], in1=top8[:, :, 3:4].to_broadcast([128, NT, 32]), op=ALU.is_ge),
         reads=allk + [("top8", t) for t in range(NT)], writes=["maskall"])
    S.op(DVE, lambda e: e.tensor_tensor(out=exl[:], in0=lgall[:], in1=top8[:, :, 0:1].to_broadcast([128, NT, 32]), op=ALU.subtract),
         reads=allk + [("top8", t) for t in range(NT)], writes=["exl"])
    S.op(ACT, lambda e: e.activation(out=exl[:], in_=exl[:], func=AF.Exp), reads=["exl"], writes=["exl"])
    S.op(DVE, lambda e: e.tensor_tensor(out=exl[:], in0=exl[:], in1=maskall[:], op=ALU.mult), reads=["exl", "maskall"], writes=["exl"])
    S.op(DVE, lambda e: e.tensor_reduce(out=sums[:], in_=exl[:], axis=AX.X, op=ALU.add), reads=["exl"], writes=["sums"])
    S.op(DVE, lambda e: e.reciprocal(out=sums[:], in_=sums[:]), reads=["sums"], writes=["sums"])
    S.op(DVE, lambda e: e.tensor_tensor(out=Gall[:], in0=exl[:], in1=sums[:].unsqueeze(2).to_broadcast([128, NT, 32]), op=ALU.mult), reads=["exl", "sums"], writes=["Gall"])
    mflat = maskall[:].rearrange("p t e -> p (t e)")
    S.op(PE, lambda e: e.matmul(pb[5][:], lhsT=onesf[:], rhs=mflat[:, 0:512], start=True, stop=True), reads=["maskall"], writes=["pb5"], inc=True)
    S.op(PE, lambda e: e.matmul(pb[6][:], lhsT=onesf[:], rhs=mflat[:, 512:1024], start=True, stop=True), reads=["maskall"], writes=["pb6"], inc=True)
    csA = sb("csA", [128, 48, 32], F32, stC); csB = sb("csB", [128, 48, 32], F32, stC); cs0 = sb("cs0", [128, NT, 32], F32, stC)
    S.op(DVE, lambda e: e.memset(csA[:, 0:16, :], 0.0), writes=["csA"])
    S.op(DVE, lambda e: e.memset(csB[:, 0:16, :], 0.0), writes=["csB"])
    S.op(DVE, lambda e: e.tensor_copy(out=csA[:, 16:32, :], in_=pb[5][:].rearrange("p (t e) -> p t e", e=32)), reads=["pb5"], writes=["csA"])
    S.op(DVE, lambda e: e.tensor_copy(out=csA[:, 32:48, :], in_=pb[6][:].rearrange("p (t e) -> p t e", e=32)), reads=["pb6"], writes=["csA"])
    S.op(DVE, lambda e: e.tensor_copy(out=cs0[:], in_=csA[:, 16:48, :]), reads=["csA"], writes=["cs0"])
    cur, curk, oth, othk = csA, "csA", csB, "csB"
    for j in range(5):
        sh = 1 << j
        S.op(DVE, lambda e, cur=cur, oth=oth, sh=sh: e.tensor_tensor(out=oth[:, 16:48, :], in0=cur[:, 16:48, :], in1=cur[:, 16 - sh:48 - sh, :], op=ALU.add), reads=[curk], writes=[othk])
        cur, curk, oth, othk = oth, othk, cur, curk
    incl = cur; inclk = curk
    cnt = sb("cnt", [128, 32], F32, stC); padd = sb("padd", [128, 32], F32, stC)
    scA = sb("scA", [128, 64], F32, stC); scB = sb("scB", [128, 64], F32, stC); pstart = sb("pstart", [128, 32], F32, stC)
    S.op(DVE, lambda e: e.tensor_copy(out=cnt[:], in_=incl[:, 47, :]), reads=[inclk], writes=["cnt"])
    cmpc = sb("cmpc", [128, 32, 32], F32, stC)
    S.op(DVE, lambda e: e.tensor_tensor(out=cmpc[:], in0=cnt[:].unsqueeze(2).to_broadcast([128, 32, 32]), in1=bstart[:, 0:32].unsqueeze(1).to_broadcast([128, 32, 32]), op=ALU.is_gt),
         reads=["cnt"], writes=["cmpc"])
    S.op(DVE, lambda e: e.tensor_reduce(out=padd[:], in_=cmpc[:], axis=AX.X, op=ALU.add), reads=["cmpc"], writes=["padd"])
    S.op(DVE, lambda e: e.tensor_scalar(out=padd[:], in0=padd[:], scalar1=128.0, scalar2=None, op0=ALU.mult), reads=["padd"], writes=["padd"])
    S.op(DVE, lambda e: e.memset(scA[:, 0:32], 0.0), writes=["scA"])
    S.op(DVE, lambda e: e.memset(scB[:, 0:32], 0.0), writes=["scB"])
    S.op(DVE, lambda e: e.tensor_copy(out=scA[:, 32:64], in_=padd[:]), reads=["padd"], writes=["scA"])
    cur, curk, oth, othk = scA, "scA", scB, "scB"
    for j in range(5):
        sh = 1 << j
        S.op(DVE, lambda e, cur=cur, oth=oth, sh=sh: e.tensor_tensor(out=oth[:, 32:64], in0=cur[:, 32:64], in1=cur[:, 32 - sh:64 - sh], op=ALU.add), reads=[curk], writes=[othk])
        cur, curk, oth, othk = oth, othk, cur, curk
    pend = cur; pendk = curk
    S.op(DVE, lambda e: e.tensor_tensor(out=pstart[:], in0=pend[:, 32:64], in1=padd[:], op=ALU.subtract), reads=[pendk, "padd"], writes=["pstart"])
    base = sb("base", [128, NT, 32], F32, stC)
    S.op(DVE, lambda e: e.tensor_tensor(out=base[:], in0=incl[:, 16:48, :], in1=cs0[:], op=ALU.subtract), reads=[inclk, "cs0"], writes=["base"])
    S.op(DVE, lambda e: e.tensor_tensor(out=base[:], in0=base[:], in1=pstart[:].unsqueeze(1).to_broadcast([128, NT, 32]), op=ALU.add), reads=["base", "pstart"], writes=["base"])
    for t in range(NT):
        bank = 5 + t // 16
        S.op(PE, lambda e, t=t, bank=bank: e.matmul(pb[bank][:, (t % 16) * 32:(t % 16 + 1) * 32], lhsT=tri[:], rhs=maskall[:, t, :], start=True, stop=True),
             reads=["maskall"], writes=["pb%d" % bank], inc=(t % 16 == 15))
    slot = sb("slot", [128, NT, 32], F32, stC)
    S.op(DVE, lambda e: e.tensor_tensor(out=slot[:, 0:16, :], in0=pb[5][:].rearrange("p (t e) -> p t e", e=32), in1=base[:, 0:16, :], op=ALU.add), reads=["pb5", "base"], writes=["slot"])
    S.op(DVE, lambda e: e.tensor_tensor(out=slot[:, 16:32, :], in0=pb[6][:].rearrange("p (t e) -> p t e", e=32), in1=base[:, 16:32, :], op=ALU.add), reads=["pb6", "base"], writes=["slot"])
    ebf = sb("ebf", [128, NBLK], F32, stC)
    S.op(DVE, lambda e: e.memset(ebf[:], 0.0), writes=["ebf"])
    for ee in range(32):
        S.op(DVE, lambda e, ee=ee: e.scalar_tensor_tensor(out=ebf[:], in0=bstart[:], scalar=pend[:, 32 + ee:33 + ee], in1=ebf[:], op0=ALU.is_ge, op1=ALU.add),
             reads=[pendk, "ebf"], writes=["ebf"])
    S.op(DVE, lambda e: e.tensor_scalar(out=ebf[:], in0=ebf[:], scalar1=31.0, scalar2=None, op0=ALU.min), reads=["ebf"], writes=["ebf"])
    S.op(DVE, lambda e: e.tensor_copy(out=eb_i[:], in_=ebf[:]), reads=["ebf"], writes=["eb_i"])
    S.op(DVE, lambda e: e.tensor_scalar(out=OHall[:], in0=ebf[:], scalar1=pidx[:, 1:2], scalar2=None, op0=ALU.is_equal), reads=["ebf"], writes=["OHall"])
    neq = sb("neq", [128, NBLK], F32, stC); ridxf = sb("ridxf", [128, NBLK], F32, stC)
    import os as _os2
    S.op(DVE, lambda e: e.memset(neq[:, 0:2], 0.0 if _os2.environ.get('KNOLOAD') else 1.0), writes=["neq"])
    S.op(DVE, lambda e: e.tensor_tensor(out=neq[:, 2:NBLK], in0=ebf[:, 2:NBLK], in1=ebf[:, 0:NBLK - 2], op=(ALU.is_lt if _os2.environ.get('KNOLOAD') else ALU.not_equal)), reads=["ebf"], writes=["neq"])
    S.op(DVE, lambda e: e.tensor_scalar(out=ridxf[:], in0=ebf[:], scalar1=128.0, scalar2=pidx[:, 0:1], op0=ALU.mult, op1=ALU.add), reads=["ebf"], writes=["ridxf"])
    S.op(DVE, lambda e: e.scalar_tensor_tensor(out=ridxf[:], in0=ridxf[:], scalar=-1.0e6, in1=neq[:], op0=ALU.add, op1=ALU.mult), reads=["ridxf", "neq"], writes=["ridxf"])
    S.op(DVE, lambda e: e.tensor_scalar(out=ridxf[:], in0=ridxf[:], scalar1=1.0e6, scalar2=None, op0=ALU.add), reads=["ridxf"], writes=["ridxf"])
    S.op(DVE, lambda e: e.tensor_copy(out=ridx_i[:], in_=ridxf[:]), reads=["ridxf"], writes=["ridx"])
    idxf = sb("idxf", [128, NT, 4], F32, stC); oh = sb("oh", [128, NT, 32], F32, stC); oh2 = sb("oh2", [128, NT, 32], F32, stC)
    destf = sb("destf", [128, NT, 4], F32, stC)
    S.op(DVE, lambda e: e.tensor_copy(out=idxf[:], in_=idx8[:, :, 0:4]), reads=[("idx8", t) for t in range(NT)], writes=["idxf"])
    for k in range(4):
        S.op(DVE, lambda e, k=k: e.tensor_tensor(out=oh[:], in0=iota[:].unsqueeze(1).to_broadcast([128, NT, 32]), in1=idxf[:, :, k:k + 1].to_broadcast([128, NT, 32]), op=ALU.is_equal),
             reads=["idxf"], writes=["oh"])
        S.op(DVE, lambda e: e.tensor_tensor(out=oh2[:], in0=oh[:], in1=slot[:], op=ALU.mult), reads=["oh", "slot"], writes=["oh2"])
        S.op(DVE, lambda e, k=k: e.tensor_reduce(out=destf[:, :, k], in_=oh2[:], axis=AX.X, op=ALU.add), reads=["oh2"], writes=["destf"])
        S.op(DVE, lambda e: e.tensor_tensor(out=oh2[:], in0=oh[:], in1=Gall[:], op=ALU.mult), reads=["oh", "Gall"], writes=["oh2"])
        S.op(DVE, lambda e, k=k: e.tensor_reduce(out=gk[:, :, k], in_=oh2[:], axis=AX.X, op=ALU.add), reads=["oh2"], writes=["gk"])
    S.op(DVE, lambda e: e.tensor_copy(out=dest_i[:], in_=destf[:]), reads=["destf"], writes=["dest_i"])
    xs = [sb("xs%d" % i, [128, 1024], BF16, stC) for i in range(2)]
    for t in range(NT):
        S.op(SP, lambda e, t=t: e.dma_start(out=xs[t % 2][:], in_=xn2_d[t * 128:(t + 1) * 128, :]), reads=["xn2_d"], writes=[("xs", t % 2)], dma="xs%d" % (t % 2))
        for k in range(4):
            S.op(POOL, lambda e, t=t, k=k: e.indirect_dma_start(out=xbuf_d, out_offset=bass.IndirectOffsetOnAxis(ap=dest_i[:, t, k:k + 1], axis=0), in_=xs[t % 2][:], in_offset=None),
                 reads=[("xs", t % 2), "dest_i"], writes=["xbuf"], dma="scat")


def moe_blocks(nc, S, sb, st, pb, pbb, OHall, ridx_i, wbf_d, btab_d, xbuf_d, ybuf_d, identb, nblk=160):
    import os as _o3
    NOW = bool(_o3.environ.get('KNOW'))
    with ExitStack() as stD:
        W = {nm: [sb("%s%d" % (nm, i), [128, 8, 1024], BF16, stD) for i in range(2)] for nm in ("wg", "wu", "wd")}
        Wd_ = {nm: wbf_d[m_].rearrange("(r c2) x -> r (c2 x)", c2=4) for m_, nm in enumerate(("wg", "wu", "wd"))}
        btab = sb("btab", [64, 3072], F32, stD); bt2 = sb("bt2", [64, 3072], BF16, stD); btl = sb("btl", [64, 3072], F32, stD)
        S.op(SP, lambda e: e.dma_start(out=btab[0:32, :], in_=btab_d), writes=["btab"], dma="btab")
        S.op(SP, lambda e: e.dma_start(out=btab[32:64, :], in_=btab_d), writes=["btab"], dma="btab")
        S.op(DVE, lambda e: e.tensor_copy(out=bt2[:], in_=btab[:]), reads=["btab"], writes=["bt2"])
        S.op(DVE, lambda e: e.tensor_copy(out=btl[:], in_=bt2[:]), reads=["bt2"], writes=["btl"])
        S.op(DVE, lambda e: e.tensor_tensor(out=btl[:], in0=btab[:], in1=btl[:], op=ALU.subtract), reads=["btab", "btl"], writes=["btl"])
        S.op(DVE, lambda e: e.tensor_copy(out=bt2[32:64, :], in_=btl[32:64, :]), reads=["btl", "bt2"], writes=["bt2"])
        ohb = [sb("ohb%d" % i, [64, 128], BF16, stD) for i in range(2)]
        xb = [sb("xb%d" % i, [128, 1024], BF16, stD) for i in range(2)]
        xT = [sb("xT%d" % i, [128, 8, 128], BF16, stD) for i in range(2)]
        gtb = [sb("gtb%d" % i, [128, 512], F32, stD) for i in range(2)]; sgb = [sb("sgb%d" % i, [128, 512], F32, stD) for i in range(2)]
        upb = [sb("upb%d" % i, [128, 512], F32, stD) for i in range(2)]
        hdn = [sb("hdn%d" % i, [128, 1024], BF16, stD) for i in range(2)]
        hT = [sb("hT%d" % i, [128, 8, 128], BF16, stD) for i in range(2)]
        yb = [sb("yb%d" % i, [128, 1024], F32, stD) for i in range(2)]
        breg = stD.enter_context(nc.gpsimd.register("bnd_reg"))
        S.op(POOL, lambda e: e.reg_mov(breg, 32 * 128 - 1))

        def gath(nm, b):
            par = b % 2
            S.op(POOL, lambda e: e.indirect_dma_start(out=W[nm][par][:].rearrange("p c f -> p (c f)"), out_offset=None, in_=Wd_[nm],
                 in_offset=bass.IndirectOffsetOnAxis(ap=ridx_i[:, b:b + 1], axis=0), bounds_check=breg, oob_is_err=False),
                 reads=["ridx", "wbf"], writes=[(nm, par, c) for c in range(8)], dma="%s%d" % (nm, par))

        def P1(b):
            par = b % 2
            gath("wg", b); gath("wu", b)
            if b >= 1:
                gath("wd", b - 1)
            if b == 0:
                S.op(SP, lambda e: e.dma_start(out=xb[0][:], in_=xbuf_d[0:128, :]), writes=[("xb", 0)], dma="xb0")
            if b + 1 < nblk:
                S.op(SP, lambda e, b=b: e.dma_start(out=xb[(b + 1) % 2][:], in_=xbuf_d[(b + 1) * 128:(b + 2) * 128, :]), writes=[("xb", (b + 1) % 2)], dma="xb%d" % ((b + 1) % 2))
            S.op(DVE, lambda e, b=b, par=par: e.tensor_copy(out=ohb[par][:], in_=OHall[0:64, b:b + 1].to_broadcast([64, 128])), reads=["OHall"], writes=[("ohb", par)])
            for c in range(8):
                S.op(PE, lambda e, c=c, par=par: e.transpose(out=pbb[6][:, c * 128:(c + 1) * 128], in_=xb[par][:, c * 128:(c + 1) * 128], identity=identb[:]),
                     reads=[("xb", par)], writes=["pb6"], inc=(c == 7))
            S.op(ACT, lambda e, par=par: e.activation(out=xT[par][:], in_=pbb[6].rearrange("p (c t) -> p c t", c=8), func=AF.Copy), reads=["pb6"], writes=[("xT", par)])

        def P2(b):
            par = b % 2
            for hf in range(2):
                accs = [("wg", 2 * hf, hf * 512), ("wu", 2 * hf + 1, 1024 + hf * 512)]
                for (nm, a, boff) in accs:
                    S.op(PE, lambda e, a=a, boff=boff, par=par: e.matmul(pb[a][:], lhsT=ohb[par][:], rhs=bt2[:, boff:boff + 512], start=True, stop=False),
                         reads=[("ohb", par), "bt2"], writes=["pb%d" % a])
                for c in range(8):
                    for (nm, a, boff) in accs:
                        S.op(PE, lambda e, a=a, nm=nm, hf=hf, c=c, par=par: e.matmul(pb[a][:], lhsT=xT[par][:, c, :], rhs=W[nm][par][:, c, hf * 512:(hf + 1) * 512], start=False, stop=(c == 7)),
                             reads=[("xT", par)] + ([] if NOW else [(nm, par, c)]), writes=["pb%d" % a], inc=(c == 7))
                G, U = pb[2 * hf], pb[2 * hf + 1]; gk_, uk_ = "pb%d" % (2 * hf), "pb%d" % (2 * hf + 1)
                S.op(DVE, lambda e, hf=hf, G=G: e.tensor_scalar(out=gtb[hf][:], in0=G[:], scalar1=7.0, scalar2=None, op0=ALU.min), reads=[gk_], writes=[("gtb", hf)])
                S.op(ACT, lambda e, hf=hf: e.activation(out=sgb[hf][:], in_=gtb[hf][:], func=AF.Sigmoid, scale=1.702), reads=[("gtb", hf)], writes=[("sgb", hf)])
                S.op(DVE, lambda e, hf=hf, U=U: e.tensor_scalar(out=upb[hf][:], in0=U[:], scalar1=-7.0, scalar2=7.0, op0=ALU.max, op1=ALU.min), reads=[uk_], writes=[("upb", hf)])
                S.op(DVE, lambda e, hf=hf: e.scalar_tensor_tensor(out=upb[hf][:], in0=upb[hf][:], scalar=1.0, in1=gtb[hf][:], op0=ALU.add, op1=ALU.mult), reads=[("upb", hf), ("gtb", hf)], writes=[("upb", hf)])
                S.op(DVE, lambda e, hf=hf, par=par: e.tensor_tensor(out=hdn[par][:].rearrange("s (c p) -> s p c", p=128)[:, hf * 64:(hf + 1) * 64, :], in0=upb[hf][:].rearrange("s (p c) -> s p c", c=8),
                     in1=sgb[hf][:].rearrange("s (p c) -> s p c", c=8), op=ALU.mult), reads=[("upb", hf), ("sgb", hf)], writes=[("hdn", par)])

        def P3(b):
            par = b % 2
            for c in range(8):
                S.op(PE, lambda e, c=c, par=par: e.transpose(out=pbb[7][:, c * 128:(c + 1) * 128], in_=hdn[par][:, c * 128:(c + 1) * 128], identity=identb[:]),
                     reads=[("hdn", par)], writes=["pb7"], inc=(c == 7))
            S.op(ACT, lambda e, par=par: e.activation(out=hT[par][:], in_=pbb[7].rearrange("p (c t) -> p c t", c=8), func=AF.Copy), reads=["pb7"], writes=[("hT", par)])
            for hf in range(2):
                S.op(PE, lambda e, hf=hf, par=par: e.matmul(pb[4 + hf][:], lhsT=ohb[par][:], rhs=bt2[:, 2048 + hf * 512:2048 + (hf + 1) * 512], start=True, stop=False),
                     reads=[("ohb", par), "bt2"], writes=["pb%d" % (4 + hf)])
                for c in range(8):
                    S.op(PE, lambda e, hf=hf, c=c, par=par: e.matmul(pb[4 + hf][:], lhsT=hT[par][:, c, :], rhs=W["wd"][par][:, c, hf * 512:(hf + 1) * 512], start=False, stop=(c == 7)),
                         reads=[("hT", par)] + ([] if NOW else [("wd", par, c)]), writes=["pb%d" % (4 + hf)], inc=(c == 7))
                if hf == 0:
                    S.op(ACT, lambda e, hf=hf, par=par: e.activation(out=yb[par][:, hf * 512:(hf + 1) * 512], in_=pb[4 + hf][:], func=AF.Copy), reads=["pb%d" % (4 + hf)], writes=[("yb", par)])
                else:
                    S.op(DVE, lambda e, hf=hf, par=par: e.tensor_copy(out=yb[par][:, hf * 512:(hf + 1) * 512], in_=pb[4 + hf][:]), reads=["pb%d" % (4 + hf)], writes=[("yb", par)])
            S.op(SP, lambda e, b=b, par=par: e.dma_start(out=ybuf_d[b * 128:(b + 1) * 128, :], in_=yb[par][:]), reads=[("yb", par)], writes=["ybuf"], dma="yst%d" % par)

        P1(0); P2(0)
        for b in range(1, nblk):
            P1(b); P2(b); P3(b - 1)
        gath("wd", nblk - 1)
        P3(nblk - 1)


def run_pipelined(tile_ops, ntiles, skew):
    lists = {}
    nops = None
    s = 0
    done = 0
    while done < ntiles:
        for t in range(ntiles):
            st_ = s - t * skew
            if st_ < 0:
                break
            if t not in lists:
                lists[t] = tile_ops(t)
                nops = len(lists[t])
            if st_ < len(lists[t]):
                lists[t][st_]()
                if st_ == len(lists[t]) - 1:
                    done += 1
        s += 1


def ple_phase(nc, S, sb, st, pb, pbb, dest_i, gk, h1_d, ybuf_d, p_d, w_pg_d, w_pp_d, post_b_d, gcol_ple, identb, out_d, epsc):
    NT = 32
    with ExitStack() as stE:
        w_pg = sb("w_pg", [128, 8, 1024], BF16, stE); w_pp = sb("w_pp", [128, 2, 1024], BF16, stE); post_b = sb("post_b", [128, 1024], F32, stE)
        for c in range(8):
            S.op(POOL, lambda e, c=c: e.dma_start(out=w_pg[:, c, :], in_=w_pg_d[c * 128:(c + 1) * 128, :]), writes=[("w_pg", c)], dma="w_pg")
        for c in range(8):
            S.op(DVE, lambda e, c=c: e.tensor_scalar(out=w_pg[:, c, :], in0=w_pg[:, c, :], scalar1=gcol_ple[:, c:c + 1], scalar2=None, op0=ALU.mult), reads=[("w_pg", c)], writes=[("w_pg", c)])
        for c in range(2):
            S.op(POOL, lambda e, c=c: e.dma_start(out=w_pp[:, c, :], in_=w_pp_d[c * 128:(c + 1) * 128, :]), writes=["w_pp"], dma="w_pp")
        S.op(SP, lambda e: e.dma_start(out=post_b[:], in_=post_b_d), writes=["post_b"], dma="cE")
        D = lambda nm, shape, dt=F32: [sb("%s_%d" % (nm, i), shape, dt, stE) for i in range(3)]
        yk = [[sb("yk%d_%d" % (i, k), [128, 1024], F32, stE) for k in range(4)] for i in range(3)]
        h2 = D("h2", [128, 1024]); pt = D("pt", [128, 256]); ptb = D("ptb", [128, 256], BF16); pT = D("pT", [128, 2, 128], BF16)
        hn3 = D("hn3", [128, 1024], BF16); hn3T = D("hn3T", [128, 8, 128], BF16); ejunk = D("ejunk", [128, 1024], BF16)
        gate = D("gate", [128, 1024]); et = D("et", [128, 1024]); st5 = D("st5", [128, 8])

        def tile_ops(t):
            par = t % 3
            ops = []
            A = ops.append
            P = lambda nm: (nm, par)
            A(lambda: S.op(SP, lambda e: e.dma_start(out=h2[par][:], in_=h1_d[t * 128:(t + 1) * 128, :]), reads=["h1_d"], writes=[P("h2")], dma="h2_%d" % par))
            A(lambda: S.op(SP, lambda e: e.dma_start(out=pt[par][:], in_=p_d[t * 128:(t + 1) * 128, :]), writes=[P("pt")], dma="pt%d" % par))
            for k in range(4):
                A(lambda k=k: S.op(POOL, lambda e: e.indirect_dma_start(out=yk[par][k][:], out_offset=None, in_=ybuf_d, in_offset=bass.IndirectOffsetOnAxis(ap=dest_i[:, t, k:k + 1], axis=0)),
                                   reads=["ybuf", "dest_i"], writes=[("yk", par, k)], dma="yk%d" % par))
            A(lambda: S.op(ACT, lambda e: e.activation(out=ptb[par][:], in_=pt[par][:], func=AF.Copy), reads=[P("pt")], writes=[P("ptb")]))
            for c in range(2):
                A(lambda c=c: S.op(PE, lambda e: e.transpose(out=pbb[3][:, c * 128:(c + 1) * 128], in_=ptb[par][:, c * 128:(c + 1) * 128], identity=identb[:]), reads=[P("ptb")], writes=["pb3"], inc=(c == 1)))
            A(lambda: S.op(DVE, lambda e: e.tensor_copy(out=pT[par][:], in_=pbb[3][:, 0:256].rearrange("p (c t) -> p c t", c=2)), reads=["pb3"], writes=[P("pT")]))
            A(lambda: S.op(DVE, lambda e: e.memset(st5[par][:, 0:3], 0.0), writes=[P("st5")]))
            for hf in range(2):
                for c in range(2):
                    A(lambda c=c, hf=hf: S.op(PE, lambda e: e.matmul(pb[4 + hf][:], lhsT=pT[par][:, c, :], rhs=w_pp[:, c, hf * 512:(hf + 1) * 512], start=(c == 0), stop=(c == 1)),
                                              reads=[P("pT"), "w_pp"], writes=["pb%d" % (4 + hf)], inc=(c == 1)))
                A(lambda hf=hf: S.op(ACT, lambda e: e.activation(out=ejunk[par][:, hf * 512:(hf + 1) * 512], in_=pb[4 + hf][:], func=AF.Square, accum_out=st5[par][:, 1 + hf:2 + hf]),
                                     reads=["pb%d" % (4 + hf), P("st5")], writes=[P("ejunk"), ("st5e", par, hf)]))
            A(lambda: S.op(DVE, lambda e: e.tensor_tensor(out=st5[par][:, 4:5], in0=st5[par][:, 1:2], in1=st5[par][:, 2:3], op=ALU.add), reads=[("st5e", par, 0), ("st5e", par, 1)], writes=[P("st5s")]))
            A(lambda: S.op(ACT, lambda e: e.activation(out=st5[par][:, 4:5], in_=st5[par][:, 4:5], func=AF.Ln, scale=1.0 / 1024, bias=epsc[:, 0:1]), reads=[P("st5s")], writes=[P("st5s")]))
            A(lambda: S.op(ACT, lambda e: e.activation(out=st5[par][:, 4:5], in_=st5[par][:, 4:5], func=AF.Exp, scale=-0.5), reads=[P("st5s")], writes=[P("st5s")]))
            for hf in range(2):
                A(lambda hf=hf: S.op(DVE, lambda e: e.scalar_tensor_tensor(out=et[par][:, hf * 512:(hf + 1) * 512], in0=pb[4 + hf][:], scalar=st5[par][:, 4:5], in1=post_b[:, hf * 512:(hf + 1) * 512], op0=ALU.mult, op1=ALU.mult),
                                     reads=["pb%d" % (4 + hf), P("st5s"), "post_b"], writes=[P("et")]))
            A('STAGE')
            for k in range(4):
                A(lambda k=k: S.op(DVE, lambda e: e.scalar_tensor_tensor(out=h2[par][:], in0=yk[par][k][:], scalar=gk[:, t, k:k + 1], in1=h2[par][:], op0=ALU.mult, op1=ALU.add),
                                   reads=[("yk", par, k), P("h2"), "gk"], writes=[P("h2")]))
            A(lambda: S.op(ACT, lambda e: e.activation(out=ejunk[par][:], in_=h2[par][:], func=AF.Square, accum_out=st5[par][:, 0:1]), reads=[P("h2"), P("st5")], writes=[P("ejunk"), P("st5a")]))
            A(lambda: S.op(ACT, lambda e: e.activation(out=st5[par][:, 3:4], in_=st5[par][:, 0:1], func=AF.Ln, scale=1.0 / 1024, bias=epsc[:, 0:1]), reads=[P("st5a")], writes=[P("st5r")]))
            A(lambda: S.op(ACT, lambda e: e.activation(out=st5[par][:, 3:4], in_=st5[par][:, 3:4], func=AF.Exp, scale=-0.5), reads=[P("st5r")], writes=[P("st5r")]))
            A(lambda: S.op(ACT, lambda e: e.activation(out=hn3[par][:], in_=h2[par][:], func=AF.Copy, scale=st5[par][:, 3:4]), reads=[P("h2"), P("st5r")], writes=[P("hn3")]))
            for c in range(8):
                A(lambda c=c: S.op(PE, lambda e: e.transpose(out=pbb[0][:, c * 128:(c + 1) * 128], in_=hn3[par][:, c * 128:(c + 1) * 128], identity=identb[:]), reads=[P("hn3")], writes=["pb0"], inc=(c == 7)))
            A(lambda: S.op(DVE, lambda e: e.tensor_copy(out=hn3T[par][:], in_=pbb[0].rearrange("p (c t) -> p c t", c=8)), reads=["pb0"], writes=[P("hn3T")]))
            A('STAGE')
            for hf in range(2):
                for c in range(8):
                    A(lambda c=c, hf=hf: S.op(PE, lambda e: e.matmul(pb[1 + hf][:], lhsT=hn3T[par][:, c, :], rhs=w_pg[:, c, hf * 512:(hf + 1) * 512], start=(c == 0), stop=(c == 7)),
                                              reads=[P("hn3T"), ("w_pg", c)], writes=["pb%d" % (1 + hf)], inc=(c == 7)))
                A(lambda hf=hf: S.op(ACT, lambda e: e.activation(out=gate[par][:, hf * 512:(hf + 1) * 512], in_=pb[1 + hf][:], func=AF.Sigmoid), reads=["pb%d" % (1 + hf)], writes=[P("gate")]))
            A(lambda: S.op(DVE, lambda e: e.tensor_tensor(out=et[par][:], in0=et[par][:], in1=gate[par][:], op=ALU.mult), reads=[P("et"), P("gate")], writes=[P("et")]))
            A(lambda: S.op(DVE, lambda e: e.tensor_tensor(out=et[par][:], in0=et[par][:], in1=h2[par][:], op=ALU.add), reads=[P("et"), P("h2")], writes=[P("et")]))
            A(lambda: S.op(SP, lambda e: e.dma_start(out=out_d[t * 128:(t + 1) * 128, :], in_=et[par][:]), reads=[P("et")], writes=["out_d"], dma="out%d" % par))
            stages = [[]]
            for o in ops:
                if o == 'STAGE':
                    stages.append([])
                else:
                    stages[-1].append(o)
            return stages
        cache = {}

        def get(t):
            if t not in cache:
                cache[t] = tile_ops(t)
            return cache[t]
        for it in range(NT + 2):
            lists = [get(t)[stg] for stg, t in ((2, it - 2), (1, it - 1), (0, it)) if 0 <= t < NT]
            L = max(len(x) for x in lists)
            for i in range(L):
                for x in lists:
                    j0 = (i * len(x)) // L
                    j1 = ((i + 1) * len(x)) // L
                    for j in range(j0, j1):
                        x[j]()
import math
import os as _os
from contextlib import ExitStack
import ml_dtypes
from concourse.bass_utils import run_bass_kernel_spmd

S_TOK = 4096
NT = 32
NG = 8
NBLK = 160
NSLOT = NBLK * 128
LAMBDA_INIT = 0.2
EPS = 1e-6
DEBUG = False


def build(stage=99):
    nc = bass.Bass("TRN2", target_bir_lowering=False)

    def din(name, shape, dt=F32):
        return nc.dram_tensor(name, list(shape), dt, kind="ExternalInput").ap()

    x_d = din("x", [S_TOK, 1024]); p_d = din("p", [S_TOK, 256]); pos_d = din("posb", [128, S_TOK], I32)
    w_in_d = din("w_in", [1024, 2048]); gcol_attn_d = din("gcol_attn", [128, 8])
    qn_d = din("qn_col", [128, 1]); kn_d = din("kn_col", [128, 1]); lamv_d = din("lamv", [128, 4, 64])
    subln_d = din("subln_b", [128, 128]); w_pool_d = din("w_pool", [4, 128, 128]); pscale_d = din("pscale_col", [128, 4])
    w_out_d = din("w_out", [1024, 1024]); ffn_b_d = din("ffn_b", [128, 1024]); w_r_d = din("w_router", [128, 8, 32])
    b_r_d = din("b_router_b", [128, 32])
    wg_d = din("w_gate", [32, 1024, 1024]); wu_d = din("w_up", [32, 1024, 1024]); wd_d = din("w_down", [32, 1024, 1024])
    btab_d = din("btab", [32, 3072])
    gcol_ple_d = din("gcol_ple", [128, 8]); w_pg_d = din("w_ple_gate", [1024, 1024]); w_pp_d = din("w_ple_proj", [256, 1024])
    post_b_d = din("post_b", [128, 1024])
    identb_d = din("ident_bf", [128, 128], BF16); identf_d = din("ident_f", [128, 128]); onesf_d = din("ones_f", [128, 128])
    tri_d = din("tri_strict", [128, 128]); blk_d = din("blkdiag", [128, 128], BF16); rmat_d = din("rmat", [128, 128], BF16)
    cmask_d = din("cmask", [128, 128], BF16); freq_d = din("freq_col", [128, 1]); invc_d = din("invcnt", [128, 16])
    iota_d = din("iota_row", [128, 32]); bstart_d = din("bstart_row", [128, NBLK]); pidx_d = din("pidx_col", [128, 2])
    out_d = nc.dram_tensor("out", [S_TOK, 1024], F32, kind="ExternalOutput").ap()
    KS = "ExternalOutput" if DEBUG else "Internal"
    catp_d = nc.dram_tensor("catp_s", [4, 128, S_TOK], BF16, kind=KS).ap()
    if DEBUG:
        dbg_q = nc.dram_tensor("dbg_q", [128, 4, S_TOK], BF16, kind=KS).ap(); dbg_k = nc.dram_tensor("dbg_k", [128, 4, S_TOK], BF16, kind=KS).ap()
        dbg_v = nc.dram_tensor("dbg_v", [128, NT, 4, 130], BF16, kind=KS).ap(); dbg_g = nc.dram_tensor("dbg_g", [128, 1024], BF16, kind=KS).ap(); dbg_v2 = nc.dram_tensor("dbg_v2", [128, NT, 4, 130], BF16, kind=KS).ap()
        dbg_r = nc.dram_tensor("dbg_r", [128, NT * 4 * 2 + NBLK], F32, kind=KS).ap()
    cata_d = nc.dram_tensor("cata_s", [128, 4, S_TOK], BF16, kind=KS).ap()
    wbf_d = [nc.dram_tensor("wbf_s%d" % i, [32 * 512, 2048], BF16, kind="Internal").ap() for i in range(3)]
    h1_d = nc.dram_tensor("h1_s", [S_TOK, 1024], F32, kind=KS).ap()
    xn2_d = nc.dram_tensor("xn2_s", [S_TOK, 1024], BF16, kind=KS).ap()
    xbuf_d = nc.dram_tensor("xbuf_s", [NSLOT, 1024], BF16, kind=KS).ap()
    ybuf_d = nc.dram_tensor("ybuf_s", [NSLOT, 1024], F32, kind=KS).ap()

    with ExitStack() as st:
        S = Sched(nc, st)

        def sb(name, shape, dt=F32, stack=st):
            return stack.enter_context(nc.sbuf_tensor("s_" + name, list(shape), dt))

        pb = [st.enter_context(nc.psum_tensor("pb%d" % i, [128, 512], F32)) for i in range(8)]
        pbb = [pb[i][:].bitcast(BF16) for i in range(8)]

        def barrier():
            toks = []
            for e in (PE, ACT, DVE, POOL, SP):
                if S.pending[e]:
                    last = S.ops[e][-1]
                    last.inc, last.incv = S.esem[e], 1
                    S.ecount[e] += 1
                    S.pending[e] = False
                if S.ecount[e] > 0:
                    toks.append(Tok(S.esem[e], S.ecount[e], e))
            for name, s in S.dsems.items():
                toks.append(Tok(s, S.dcount[id(s)], None))
            for e in (PE, ACT, DVE, POOL, SP):
                S.op(e, lambda h: h.nop(), extra=toks, inc=False if e == PE else None)
            S.writer.clear(); S.readers.clear()

        identb = sb("identb", [128, 128], BF16); identf = sb("identf", [128, 128]); onesf = sb("onesf", [128, 128])
        tri = sb("tri", [128, 128]); blk = sb("blk", [128, 128], BF16); rmat = sb("rmat", [128, 128], BF16)
        cmask = sb("cmask", [128, 128], BF16); freq = sb("freq", [128, 1]); invc = sb("invc", [128, 16])
        iota = sb("iota", [128, 32]); bstart = sb("bstart", [128, NBLK])
        qn = sb("qn", [128, 1]); kn = sb("kn", [128, 1]); lamv = sb("lamv", [128, 4, 64]); subln = sb("subln", [128, 128])
        pscale = sb("pscale", [128, 4]); gcol_attn = sb("gcol_attn", [128, 8]); gcol_ple = sb("gcol_ple", [128, 8])
        lam_c = sb("lam_c", [128, 4]); epsc = sb("epsc", [128, 1])
        OHall = sb("OHall", [128, NBLK], F32); ridx_i = sb("ridx_i", [128, NBLK], I32); pidx = sb("pidx", [128, 2], F32)
        S.op(DVE, lambda e: e.memset(epsc[:], EPS), writes=["epsc"])
        for i, (t_, d_) in enumerate([(identb, identb_d), (identf, identf_d), (onesf, onesf_d), (tri, tri_d), (blk, blk_d), (rmat, rmat_d),
                       (cmask, cmask_d), (freq, freq_d), (invc, invc_d), (iota, iota_d), (bstart, bstart_d), (qn, qn_d),
                       (kn, kn_d), (lamv, lamv_d), (subln, subln_d), (pscale, pscale_d), (gcol_attn, gcol_attn_d),
                       (gcol_ple, gcol_ple_d), (pidx, pidx_d)]):
            S.op(SP, lambda e, t_=t_, d_=d_: e.dma_start(out=t_[:], in_=d_), writes=["const"], dma="const")
        lamt = sb("lamt", [128, 2, 64])
        S.op(DVE, lambda e: e.tensor_tensor(out=lamt[:, 0, :], in0=lamv[:, 0, :], in1=lamv[:, 1, :], op=ALU.mult), reads=["const"], writes=["lamt"])
        S.op(DVE, lambda e: e.tensor_tensor(out=lamt[:, 1, :], in0=lamv[:, 2, :], in1=lamv[:, 3, :], op=ALU.mult), reads=["const"], writes=["lamt"])
        S.op(DVE, lambda e: e.tensor_reduce(out=lam_c[:, 0:2], in_=lamt[:], axis=AX.X, op=ALU.add), reads=["lamt"], writes=["lam_c"])
        S.op(ACT, lambda e: e.activation(out=lam_c[:, 0:2], in_=lam_c[:, 0:2], func=AF.Exp), reads=["lam_c"], writes=["lam_c"])
        S.op(DVE, lambda e: e.tensor_tensor(out=lam_c[:, 2:3], in0=lam_c[:, 1:2], in1=lam_c[:, 0:1], op=ALU.subtract), reads=["lam_c"], writes=["lam_c2"])
        S.op(DVE, lambda e: e.tensor_scalar(out=lam_c[:, 3:4], in0=lam_c[:, 2:3], scalar1=-LAMBDA_INIT, scalar2=None, op0=ALU.add), reads=["lam_c2"], writes=["nlam"])
        nlam = lam_c[:, 3:4]
        S.op(DVE, lambda e: e.tensor_scalar(out=subln[:], in0=subln[:], scalar1=1.0 - LAMBDA_INIT, scalar2=None, op0=ALU.mult), reads=["const"], writes=["const"])

        eb_i = sb("eb_i", [128, NBLK], I32); dest_i = sb("dest_i", [128, NT, 4], I32); gk = sb("gk", [128, NT, 4], F32)
        def _precast_gen():
            for e_ in range(32):
                for m_, wsrc in enumerate((wg_d, wu_d, wd_d)):
                    S.op(POOL, lambda e, m_=m_, e_=e_, wsrc=wsrc: e.dma_start(out=wbf_d[m_][e_ * 512:(e_ + 1) * 512, :], in_=wsrc[e_].rearrange("(r two) f -> r (two f)", two=2)),
                         writes=["wbf"], dma="precast")
                    yield
        _pc = _precast_gen()

        def precast(k_):
            for _ in range(k_):
                try:
                    next(_pc)
                except StopIteration:
                    return
        with ExitStack() as stAB:
            qT = sb("qT", [128, 4, S_TOK], BF16, stAB); kT = sb("kT", [128, 4, S_TOK], BF16, stAB)
            Vsb = sb("Vsb", [128, NT, 4, 130], BF16, stAB)
            S.op(POOL, lambda e: e.memset(Vsb[:, :, :, 128:130], 1.0), writes=["Vones"])
            with ExitStack() as stA:
                w_in = sb("w_in", [128, 8, 2048], BF16, stA)
                for c in range(8):
                    S.op(POOL, lambda e, c=c: e.dma_start(out=w_in[:, c, :], in_=w_in_d[c * 128:(c + 1) * 128, :]), writes=[("w_in", c)], dma="w_in")
                for c in range(8):
                    S.op(DVE, lambda e, c=c: e.tensor_scalar(out=w_in[:, c, :], in0=w_in[:, c, :], scalar1=gcol_attn[:, c:c + 1], scalar2=None, op0=ALU.mult),
                         reads=[("w_in", c), "const"], writes=[("w_in", c)])
                wpool = sb("wpool", [128, 4, 128], BF16, stA)
                for g in range(4):
                    S.op(POOL, lambda e, g=g: e.dma_start(out=wpool[:, g, :], in_=w_pool_d[g]), writes=["wpool"], dma="wpool")
                xt = [sb("xt%d" % i, [128, 1024], F32, stA) for i in range(2)]
                xjunk = sb("xjunk", [128, 1024], BF16, stA)
                xn = [sb("xn%d" % i, [128, 1024], BF16, stA) for i in range(2)]
                st1 = sb("st1", [128, 64], F32, stA)
                hnT = [sb("hnT0", [128, 8, 512], BF16, stA)] * 2
                posi = sb("posi", [128, 512], I32, stA)
                cosn = sb("cosn", [128, 512], F32, stA); sinn = sb("sinn", [128, 512], F32, stA)
                sq = [sb("sq%d" % i, [128, 512], BF16, stA) for i in range(2)]
                qg = [sb("qg%d" % i, [128, 512], BF16, stA) for i in range(2)]
                rstd_t = sb("rstd_t", [128, 512], F32, stA); t1 = sb("t1", [128, 512], F32, stA); t2 = sb("t2", [128, 512], F32, stA); ang = t1; ang2 = t2
                uT = [sb("uT0", [128, 4, 528], F32, stA)] * 2
                sA = sb("sA", [128, 528], F32, stA); sB = sb("sB", [128, 528], F32, stA)
                pooled = sb("pooled", [128, 4, 512], BF16, stA); catp = [sb("catp0", [128, 4, 512], BF16, stA)] * 2
                S.op(DVE, lambda e: e.memset(st1[:], 0.0), writes=["st1"])
                S.op(POOL, lambda e: e.memset(uT[0][:, :, 0:16], 0.0), writes=[("uT", 0)])
                for n in range(NG):
                    par = n % 2
                    hk = ("hnT", 0)
                    for j in range(4):
                        t = n * 4 + j
                        xk = ("xt", t % 2)
                        S.op(SP, lambda e, t=t: e.dma_start(out=xt[t % 2][:], in_=x_d[t * 128:(t + 1) * 128, :]), writes=[xk], dma="xt%d" % (t % 2))
                        S.op(ACT, lambda e, t=t: e.activation(out=xjunk[:], in_=xt[t % 2][:], func=AF.Square, accum_out=st1[:, t:t + 1]),
                             reads=[xk, "st1"], writes=["xjunk", ("ss", t)])
                        S.op(ACT, lambda e, t=t: e.activation(out=st1[:, 32 + t:33 + t], in_=st1[:, t:t + 1], func=AF.Ln, scale=1.0 / 1024, bias=epsc[:, 0:1]),
                             reads=[("ss", t)], writes=[("rs", t)])
                        S.op(ACT, lambda e, t=t: e.activation(out=st1[:, 32 + t:33 + t], in_=st1[:, 32 + t:33 + t], func=AF.Exp, scale=-0.5),
                             reads=[("rs", t)], writes=[("rs", t)])
                        S.op(ACT, lambda e, t=t: e.activation(out=xn[t % 2][:], in_=xt[t % 2][:], func=AF.Copy, scale=st1[:, 32 + t:33 + t]),
                             reads=[xk, ("rs", t)], writes=[("xn", t % 2)])
                        for c in range(8):
                            S.op(PE, lambda e, t=t, c=c: e.transpose(out=pbb[7][:, c * 128:(c + 1) * 128], in_=xn[t % 2][:, c * 128:(c + 1) * 128], identity=identb[:]),
                                 reads=[("xn", t % 2), "const"], writes=["pb7"], inc=(c == 7))
                        S.op(DVE, lambda e, j=j, par=par: e.tensor_copy(out=hnT[par][:, :, j * 128:(j + 1) * 128], in_=pbb[7].rearrange("p (c t) -> p c t", c=8)),
                             reads=["pb7"], writes=[hk])
                    precast(4)
                    S.op(SP, lambda e, n=n: e.dma_start(out=posi[:], in_=pos_d[:, n * 512:(n + 1) * 512]), writes=["posi"], dma="posi")
                    S.op(DVE, lambda e: e.tensor_copy(out=ang[:], in_=posi[:]), reads=["posi"], writes=["t1"])
                    S.op(DVE, lambda e: e.tensor_scalar(out=ang[:], in0=ang[:], scalar1=freq[:, 0:1], scalar2=None, op0=ALU.mult), reads=["t1", "const"], writes=["t1"])
                    S.op(DVE, lambda e: e.tensor_scalar(out=ang2[:], in0=ang[:], scalar1=math.pi / 2, scalar2=None, op0=ALU.add), reads=["t1"], writes=["t2"])
                    for (aa, ak) in ((ang, "t1"), (ang2, "t2")):
                        S.op(DVE, lambda e, aa=aa: e.tensor_scalar(out=rstd_t[:], in0=aa[:], scalar1=1.0 / (2 * math.pi), scalar2=None, op0=ALU.mult), reads=[ak], writes=["rstd_t"])
                        S.op(DVE, lambda e: e.tensor_copy(out=posi[:], in_=rstd_t[:]), reads=["rstd_t"], writes=["posi"])
                        S.op(DVE, lambda e: e.tensor_copy(out=rstd_t[:], in_=posi[:]), reads=["posi"], writes=["rstd_t"])
                        S.op(DVE, lambda e, aa=aa: e.scalar_tensor_tensor(out=aa[:], in0=rstd_t[:], scalar=-2 * math.pi, in1=aa[:], op0=ALU.mult, op1=ALU.add), reads=["rstd_t", ak], writes=[ak])
                        S.op(DVE, lambda e, aa=aa: e.tensor_scalar(out=rstd_t[:], in0=aa[:], scalar1=math.pi, scalar2=-2 * math.pi, op0=ALU.is_gt, op1=ALU.mult), reads=[ak], writes=["rstd_t"])
                        S.op(DVE, lambda e, aa=aa: e.tensor_tensor(out=aa[:], in0=aa[:], in1=rstd_t[:], op=ALU.add), reads=[ak, "rstd_t"], writes=[ak])
                        S.op(DVE, lambda e, aa=aa: e.tensor_scalar(out=rstd_t[:], in0=aa[:], scalar1=-math.pi, scalar2=2 * math.pi, op0=ALU.is_lt, op1=ALU.mult), reads=[ak], writes=["rstd_t"])
                        S.op(DVE, lambda e, aa=aa: e.tensor_tensor(out=aa[:], in0=aa[:], in1=rstd_t[:], op=ALU.add), reads=[ak, "rstd_t"], writes=[ak])
                    S.op(ACT, lambda e: e.activation(out=sinn[:], in_=ang[:], func=AF.Sin), reads=["t1"], writes=["sinn"])
                    S.op(ACT, lambda e: e.activation(out=cosn[:], in_=ang2[:], func=AF.Sin), reads=["t2"], writes=["cosn"])
                    S.begin_capture()
                    for ch in range(8):
                        pa = pb[ch % 2]; pak = "pb%d" % (ch % 2)
                        sp_ = ch % 2
                        for c in range(8):
                            S.op(PE, lambda e, c=c, ch=ch, pa=pa, par=par: e.matmul(pa[:], lhsT=w_in[:, c, ch * 128:(ch + 1) * 128], rhs=hnT[par][:, c, :], start=(c == 0), stop=(c == 7)),
                                 reads=[hk, ("w_in", c)], writes=[pak], inc=(c == 7))
                        S.op(ACT, lambda e, pa=pa, sp_=sp_: e.activation(out=sq[sp_][:], in_=pa[:], func=AF.Square), reads=[pak], writes=[("sq", sp_)])
                        gc = qn if ch < 4 else kn
                        S.op(ACT, lambda e, pa=pa, sp_=sp_, gc=gc: e.activation(out=qg[sp_][:], in_=pa[:], func=AF.Copy, scale=gc[:, 0:1]), reads=[pak, "const"], writes=[("qg", sp_)])
                        pm = pb[2 + (ch % 2)]; pmk = "pb%d" % (2 + ch % 2)
                        pr = pb[4 + (ch % 2)]; prk = "pb%d" % (4 + ch % 2)
                        S.op(PE, lambda e, pm=pm, sp_=sp_: e.matmul(pm[:], lhsT=blk[:], rhs=sq[sp_][:], start=True, stop=True), reads=[("sq", sp_), "const"], writes=[pmk], inc=True)
                        S.op(PE, lambda e, pr=pr, sp_=sp_: e.matmul(pr[:], lhsT=rmat[:], rhs=qg[sp_][:], start=True, stop=True), reads=[("qg", sp_), "const"], writes=[prk], inc=True)
                        S.op(ACT, lambda e, pm=pm: e.activation(out=rstd_t[:], in_=pm[:], func=AF.Ln, bias=epsc[:, 0:1]), reads=[pmk], writes=["rstd_t"])
                        S.op(ACT, lambda e: e.activation(out=rstd_t[:], in_=rstd_t[:], func=AF.Exp, scale=-0.5), reads=["rstd_t"], writes=["rstd_t"])
                        S.op(POOL, lambda e, sp_=sp_: e.tensor_tensor(out=t1[:], in0=qg[sp_][:], in1=cosn[:], op=ALU.mult), reads=[("qg", sp_), "cosn"], writes=["t1"])
                        S.op(DVE, lambda e, pr=pr: e.tensor_tensor(out=t2[:], in0=pr[:], in1=sinn[:], op=ALU.mult), reads=[prk, "sinn"], writes=["t2"])
                        S.op(DVE, lambda e: e.tensor_tensor(out=t1[:], in0=t1[:], in1=t2[:], op=ALU.add), reads=["t1", "t2"], writes=["t1"])
                        dst = (qT if ch < 4 else kT)
                        S.op(DVE, lambda e, dst=dst, ch=ch, n=n: e.tensor_tensor(out=dst[:, ch % 4, n * 512:(n + 1) * 512], in0=t1[:], in1=rstd_t[:], op=ALU.mult),
                             reads=["t1", "rstd_t"], writes=[("qk", ch, n)])
                    capA = S.end_capture(); S.begin_capture()
                    for j in range(4):
                        t = n * 4 + j
                        for c in range(8):
                            S.op(PE, lambda e, c=c, j=j, par=par: e.matmul(pb[6][:], lhsT=hnT[par][:, c, j * 128:(j + 1) * 128], rhs=w_in[:, c, 1024:1536], start=(c == 0), stop=(c == 7)),
                                 reads=[hk, ("w_in", c)], writes=["pb6"], inc=(c == 7))
                        S.op(ACT, lambda e, t=t: e.activation(out=Vsb[:, t, :, 0:128], in_=pb[6][:].rearrange("p (h v) -> p h v", h=4), func=AF.Copy),
                             reads=["pb6"], writes=[("V", t)])
                    capB = S.end_capture(); S.begin_capture()
                    uk = ("uT", 0)
                    if n > 0:
                        S.op(POOL, lambda e, par=par: e.tensor_copy(out=uT[par][:, :, 0:16], in_=uT[1 - par][:, :, 512:528]), reads=[("uT", 0)], writes=[uk])
                    for g in range(4):
                        for c in range(8):
                            S.op(PE, lambda e, c=c, g=g, par=par: e.matmul(pb[7][:], lhsT=w_in[:, c, 1536 + g * 128:1536 + (g + 1) * 128], rhs=hnT[par][:, c, :], start=(c == 0), stop=(c == 7)),
                                 reads=[hk, ("w_in", c)], writes=["pb7"], inc=(c == 7))
                        S.op(ACT, lambda e, g=g, par=par: e.activation(out=uT[par][:, g, 16:528], in_=pb[7][:], func=AF.Copy), reads=["pb7"], writes=[uk])
                        src = uT[par][:, g, :]
                        bufs = [sA, sB]
                        cur, curk = src, uk
                        for jj in range(g + 1):
                            sh = 1 << jj
                            lo = (1 << (jj + 1)) - 1
                            o = bufs[jj % 2]; ok_ = "sA" if jj % 2 == 0 else "sB"
                            S.op(POOL, lambda e, o=o, cur=cur, lo=lo, sh=sh: e.tensor_tensor(out=o[:, lo:528], in0=cur[:, lo:528], in1=cur[:, lo - sh:528 - sh], op=ALU.add),
                                 reads=[curk], writes=[ok_])
                            cur, curk = o, ok_
                        w = 1 << (g + 1)
                        S.op(DVE, lambda e, g=g, cur=cur, src=src, w=w: e.scalar_tensor_tensor(out=pooled[:, g, :], in0=cur[:, 16:528], scalar=1.0 / w, in1=src[:, 16:528], op0=ALU.mult, op1=ALU.subtract),
                             reads=[curk, uk], writes=["pooled"])
                        if n == 0:
                            S.op(DVE, lambda e, cur=cur, w=w: e.tensor_tensor(out=cur[:, 16:16 + w - 1], in0=cur[:, 16:16 + w - 1], in1=invc[:, 0:w - 1], op=ALU.mult),
                                 reads=[curk, "const"], writes=[curk])
                            S.op(DVE, lambda e, g=g, cur=cur, src=src, w=w: e.tensor_tensor(out=pooled[:, g, 0:w - 1], in0=cur[:, 16:16 + w - 1], in1=src[:, 16:16 + w - 1], op=ALU.subtract),
                                 reads=[curk, uk], writes=["pooled"])
                        S.op(PE, lambda e, g=g: e.matmul(pb[7][:], lhsT=wpool[:, g, :], rhs=pooled[:, g, :], start=True, stop=True), reads=["pooled", "wpool"], writes=["pb7"], inc=True)
                        S.op(ACT, lambda e, g=g, par=par: e.activation(out=catp[par][:, g, :], in_=pb[7][:], func=AF.Copy, scale=pscale[:, g:g + 1]), reads=["pb7", "const"], writes=[("catp", 0)])
                    for g in range(4):
                        S.op(POOL, lambda e, g=g, n=n, par=par: e.dma_start(out=catp_d[g, :, n * 512:(n + 1) * 512], in_=catp[par][:, g, :]), reads=[("catp", 0)], writes=["catp_d"], dma="catp_st")
                    capC = S.end_capture()
                    S.replay_interleaved([capA, capB, capC])

            barrier()
            if DEBUG and _os.environ.get("KEARLY", "1") == "1":
                S.op(SP, lambda e: e.dma_start(out=dbg_q, in_=qT[:]), dma="dbg")
                S.op(SP, lambda e: e.dma_start(out=dbg_k, in_=kT[:]), dma="dbg")
                S.op(SP, lambda e: e.dma_start(out=dbg_v, in_=Vsb[:]), dma="dbg")
            with ExitStack() as stB:
                catA = sb("catA", [128, 4, S_TOK], BF16, stB)
                PT = [[sb("PT%d_%d" % (i, c), [128, 512], BF16, stB) for c in range(2)] for i in range(2)]
                otmp = sb("otmp", [128, 128], F32, stB); ojunk = sb("ojunk", [128, 128], BF16, stB)
                ob = sb("ob", [128, 4, 128], BF16, stB); st2 = sb("st2", [128, 8], F32, stB)

                def Oap(i, lo, hi):
                    return pb[4 + i // 3][:, (i % 3) * 160 + lo:(i % 3) * 160 + hi]
                osb = [sb("osb%d" % i, [128, 512], F32, stB) for i in range(3)]

                def Osb(i, lo, hi):
                    return osb[i // 3][:, (i % 3) * 160 + lo:(i % 3) * 160 + hi]

                def SE(h, n, kt):
                    bufi = kt % 2
                    j = kt - 4 * n
                    qlo = max(j, 0) * 128
                    for c in range(2):
                        ps = pb[bufi * 2 + c]; psk = "pb%d" % (bufi * 2 + c)
                        S.op(PE, lambda e, ps=ps, c=c: e.matmul(ps[:, qlo:512], lhsT=kT[c * 64:(c + 1) * 64, h, kt * 128:(kt + 1) * 128],
                             rhs=qT[c * 64:(c + 1) * 64, h, n * 512 + qlo:(n + 1) * 512], start=True, stop=True), writes=[psk], inc=True)
                        S.op(ACT, lambda e, ps=ps, c=c: e.activation(out=PT[bufi][c][:, qlo:512], in_=ps[:, qlo:512], func=AF.Exp, scale=0.125),
                             reads=[psk], writes=[("PT", bufi, c)])
                        if j >= 0:
                            S.op(POOL, lambda e, c=c: e.tensor_tensor(out=PT[bufi][c][:, qlo:qlo + 128], in0=PT[bufi][c][:, qlo:qlo + 128], in1=cmask[:], op=ALU.mult),
                                 reads=[("PT", bufi, c)], writes=[("PT", bufi, c)])

                def AVm(h, n, kt):
                    bufi = kt % 2
                    j = kt - 4 * n
                    for qi in range(max(j, 0), 4):
                        for c in range(2):
                            i = qi * 2 + c
                            S.op(PE, lambda e, i=i, c=c, qi=qi: e.matmul(Oap(i, 0, 129), lhsT=PT[bufi][c][:, qi * 128:(qi + 1) * 128],
                                 rhs=Vsb[:, kt, h, 0:129], start=(kt == 0 and i % 3 == 0), stop=((i, kt - 4 * n) in ((2, 1), (5, 2), (7, 3)))), reads=[("PT", bufi, c)], writes=[("O", i // 3)],
                                 inc=(qi == 3 and c == 1))

                def post(h, n):
                    S.op(ACT, lambda e: e.activation(out=osb[0][:], in_=pb[4][:], func=AF.Copy), reads=[("O", 0)], writes=[("osb", 0)])
                    S.op(DVE, lambda e: e.tensor_copy(out=osb[1][:], in_=pb[5][:]), reads=[("O", 1)], writes=[("osb", 1)])
                    S.op(ACT, lambda e: e.activation(out=osb[2][:, 0:320], in_=pb[6][:, 0:320], func=AF.Copy), reads=[("O", 2)], writes=[("osb", 2)])
                    for qi in range(4):
                        i1, i2 = qi * 2, qi * 2 + 1
                        k1, k2 = ("osb", i1 // 3), ("osb", i2 // 3)
                        S.op(DVE, lambda e, i1=i1: e.reciprocal(out=st2[:, 0:1], in_=Osb(i1, 128, 129)), reads=[k1], writes=["st2a"])
                        S.op(DVE, lambda e, i2=i2: e.reciprocal(out=st2[:, 1:2], in_=Osb(i2, 128, 129)), reads=[k2], writes=["st2b"])
                        S.op(DVE, lambda e: e.tensor_tensor(out=st2[:, 1:2], in0=st2[:, 1:2], in1=nlam, op=ALU.mult), reads=["st2b"], writes=["st2b"])
                        S.op(DVE, lambda e, i1=i1: e.tensor_scalar(out=otmp[:], in0=Osb(i1, 0, 128), scalar1=st2[:, 0:1], scalar2=None, op0=ALU.mult), reads=[k1, "st2a"], writes=["otmp"])
                        S.op(DVE, lambda e, i2=i2: e.scalar_tensor_tensor(out=otmp[:], in0=Osb(i2, 0, 128), scalar=st2[:, 1:2], in1=otmp[:], op0=ALU.mult, op1=ALU.add),
                             reads=[k2, "st2b", "otmp"], writes=["otmp"])
                        S.op(DVE, lambda e: e.memset(st2[:, 2:3], 0.0), writes=["st2c"])
                        S.op(ACT, lambda e: e.activation(out=ojunk[:], in_=otmp[:], func=AF.Square, accum_out=st2[:, 2:3]), reads=["otmp", "st2c"], writes=["ojunk", "st2c"])
                        S.op(ACT, lambda e: e.activation(out=st2[:, 3:4], in_=st2[:, 2:3], func=AF.Ln, scale=1.0 / 128, bias=epsc[:, 0:1]), reads=["st2c"], writes=["st2d"])
                        S.op(ACT, lambda e: e.activation(out=st2[:, 3:4], in_=st2[:, 3:4], func=AF.Exp, scale=-0.5), reads=["st2d"], writes=["st2d"])
                        S.op(DVE, lambda e, qi=qi: e.scalar_tensor_tensor(out=ob[:, qi, :], in0=otmp[:], scalar=st2[:, 3:4], in1=subln[:], op0=ALU.mult, op1=ALU.mult),
                             reads=["otmp", "st2d"], writes=["ob"])

                def fin(h, n):
                    for qi in range(4):
                        S.op(PE, lambda e, qi=qi: e.transpose(out=pbb[7][:, qi * 128:(qi + 1) * 128], in_=ob[:, qi, :], identity=identb[:]), reads=["ob"], writes=["pb7"], inc=(qi == 3))
                    S.op(DVE, lambda e: e.tensor_copy(out=catA[:, h, n * 512:(n + 1) * 512], in_=pbb[7][:, 0:512]), reads=["pb7"], writes=[("catA", h)])

                heads = ([int(c) for c in _os.environ.get('KHEADS', '0123')] if stage >= 2 else [])
                groups = [(h, n) for h in heads for n in range(NG)]
                pending_fin = None
                for gi, (h, n) in enumerate(groups):
                    nkt = 4 * n + 4
                    precast(2)
                    SE(h, n, 0)
                    for kt in range(nkt):
                        if kt + 1 < nkt:
                            SE(h, n, kt + 1)
                        AVm(h, n, kt)
                        if kt == min(2, nkt - 1) and pending_fin is not None:
                            fin(*pending_fin)
                            ph = pending_fin[0]
                            if pending_fin[1] == NG - 1:
                                S.op(SP, lambda e, ph=ph: e.dma_start(out=cata_d[:, ph, :], in_=catA[:, ph, :]), reads=[("catA", ph)], writes=["cata_d"], dma="cata_st")
                            pending_fin = None
                    post(h, n)
                    pending_fin = (h, n)
                if pending_fin is not None:
                    fin(*pending_fin)
                    ph = pending_fin[0]
                    S.op(SP, lambda e, ph=ph: e.dma_start(out=cata_d[:, ph, :], in_=catA[:, ph, :]), reads=[("catA", ph)], writes=["cata_d"], dma="cata_st")
            if DEBUG:
                barrier()
                S.op(SP, lambda e: e.dma_start(out=dbg_v2, in_=Vsb[:]), dma="dbg")
        barrier()
        with ExitStack() as stC:
            precast(96)
            w_out = sb("w_out", [128, 8, 1024], BF16, stC)
            for c in range(8):
                S.op(POOL, lambda e, c=c: e.dma_start(out=w_out[:, c, :], in_=w_out_d[c * 128:(c + 1) * 128, :]), writes=["w_out"], dma="w_out")
            catP = sb("catP", [128, 4, S_TOK], BF16, stC); catA2 = sb("catA2", [128, 4, S_TOK], BF16, stC)
            for g in range(4):
                S.op(SP, lambda e, g=g: e.dma_start(out=catA2[:, g, :], in_=cata_d[:, g, :]), writes=["catP"], dma="catP")
            for g in range(4):
                S.op(SP, lambda e, g=g: e.dma_start(out=catP[:, g, :], in_=catp_d[g]), writes=["catP"], dma="catP")
            ffn_b = sb("ffn_b", [128, 1024], F32, stC); w_r = sb("w_r", [128, 8, 32], F32, stC); b_r = sb("b_r", [128, 32], F32, stC)
            S.op(SP, lambda e: e.dma_start(out=ffn_b[:], in_=ffn_b_d), writes=["cC"], dma="cC")
            S.op(SP, lambda e: e.dma_start(out=w_r[:], in_=w_r_d), writes=["cC"], dma="cC")
            S.op(SP, lambda e: e.dma_start(out=b_r[:], in_=b_r_d), writes=["cC"], dma="cC")
            xt2 = [sb("xt2_%d" % i, [128, 1024], F32, stC) for i in range(2)]
            h1t = [sb("h1t%d" % i, [128, 1024], F32, stC) for i in range(2)]
            xn2f = sb("xn2f", [128, 1024], F32, stC); xn2b = [sb("xn2b%d" % i, [128, 1024], BF16, stC) for i in range(2)]
            cjunk = sb("cjunk", [128, 1024], BF16, stC)
            xn2T = sb("xn2T", [128, 1024], F32, stC)
            st3 = sb("st3", [128, 64], F32, stC)
            lgall = sb("lgall", [128, NT, 32], F32, stC); top8 = sb("top8", [128, NT, 8], F32, stC); idx8 = sb("idx8", [128, NT, 8], U32, stC)
            maskall = sb("maskall", [128, NT, 32], F32, stC); Gall = sb("Gall", [128, NT, 32], F32, stC)
            S.op(DVE, lambda e: e.memset(st3[:], 0.0), writes=["st3"])
            NTC = NT if stage >= 3 else 0
            for t in range(NTC):
                xk = ("xt2", t % 2); hk2 = ("h1t", t % 2)
                S.op(SP, lambda e, t=t: e.dma_start(out=xt2[t % 2][:], in_=x_d[t * 128:(t + 1) * 128, :]), writes=[xk], dma="xt2_%d" % (t % 2))
                for hf in range(2):
                    for c in range(8):
                        cat_c = catA2[:, c, t * 128:(t + 1) * 128] if c < 4 else catP[:, c - 4, t * 128:(t + 1) * 128]
                        S.op(PE, lambda e, cat_c=cat_c, c=c, hf=hf: e.matmul(pb[hf][:], lhsT=cat_c, rhs=w_out[:, c, hf * 512:(hf + 1) * 512], start=(c == 0), stop=(c == 7)),
                             reads=["w_out", "catP"], writes=["pb%d" % hf], inc=(c == 7))
                    S.op(DVE, lambda e, t=t, hf=hf: e.tensor_tensor(out=h1t[t % 2][:, hf * 512:(hf + 1) * 512], in0=pb[hf][:], in1=xt2[t % 2][:, hf * 512:(hf + 1) * 512], op=ALU.add),
                         reads=["pb%d" % hf, xk], writes=[hk2])
                S.op(POOL, lambda e, t=t: e.dma_start(out=h1_d[t * 128:(t + 1) * 128, :], in_=h1t[t % 2][:]), reads=[hk2], writes=["h1_d"], dma="h1st%d" % (t % 2))
                S.op(ACT, lambda e, t=t: e.activation(out=cjunk[:], in_=h1t[t % 2][:], func=AF.Square, accum_out=st3[:, t:t + 1]), reads=[hk2, "st3"], writes=["cjunk", ("ss3", t)])
                S.op(ACT, lambda e, t=t: e.activation(out=st3[:, 32 + t:33 + t], in_=st3[:, t:t + 1], func=AF.Ln, scale=1.0 / 1024, bias=epsc[:, 0:1]), reads=[("ss3", t)], writes=[("rs3", t)])
                S.op(ACT, lambda e, t=t: e.activation(out=st3[:, 32 + t:33 + t], in_=st3[:, 32 + t:33 + t], func=AF.Exp, scale=-0.5), reads=[("rs3", t)], writes=[("rs3", t)])
                S.op(DVE, lambda e, t=t: e.scalar_tensor_tensor(out=xn2f[:], in0=h1t[t % 2][:], scalar=st3[:, 32 + t:33 + t], in1=ffn_b[:], op0=ALU.mult, op1=ALU.mult),
                     reads=[hk2, ("rs3", t), "cC"], writes=["xn2f"])
                S.op(ACT, lambda e, t=t: e.activation(out=xn2b[t % 2][:].rearrange("s (c p) -> s c p", p=128), in_=xn2f[:].rearrange("s (p c) -> s c p", c=8), func=AF.Copy), reads=["xn2f"], writes=[("xn2b", t % 2)])
                S.op(POOL, lambda e, t=t: e.dma_start(out=xn2_d[t * 128:(t + 1) * 128, :], in_=xn2b[t % 2][:]), reads=[("xn2b", t % 2)], writes=["xn2_d"], dma="xn2st%d" % (t % 2))
                for c in range(8):
                    bank = 2 + c // 4
                    S.op(PE, lambda e, c=c, bank=bank: e.transpose(out=pb[bank][:, (c % 4) * 128:(c % 4 + 1) * 128], in_=xn2f[:, c * 128:(c + 1) * 128], identity=identf[:]),
                         reads=["xn2f"], writes=["pb%d" % bank], inc=(c % 4 == 3))
                S.op(ACT, lambda e: e.activation(out=xn2T[:, 0:512], in_=pb[2][:], func=AF.Copy), reads=["pb2"], writes=["xn2Ta"])
                S.op(DVE, lambda e: e.tensor_copy(out=xn2T[:, 512:1024], in_=pb[3][:]), reads=["pb3"], writes=["xn2Tb"])
                for c in range(8):
                    S.op(PE, lambda e, c=c: e.matmul(pb[4][:, 0:32], lhsT=xn2T[:, c * 128:(c + 1) * 128], rhs=w_r[:, c, :], start=(c == 0), stop=(c == 7)),
                         reads=["xn2Ta", "xn2Tb", "cC"], writes=["pb4"], inc=(c == 7))
                S.op(DVE, lambda e, t=t: e.tensor_tensor(out=lgall[:, t, :], in0=pb[4][:, 0:32], in1=b_r[:], op=ALU.add), reads=["pb4", "cC"], writes=[("lg", t)])
                S.op(DVE, lambda e, t=t: e.max(out=top8[:, t, :], in_=lgall[:, t, :]), reads=[("lg", t)], writes=[("top8", t)])
                S.op(DVE, lambda e, t=t: e.max_index(out=idx8[:, t, :], in_max=top8[:, t, :], in_values=lgall[:, t, :]), reads=[("lg", t), ("top8", t)], writes=[("idx8", t)])
            if stage >= 3:
                dispatch(nc, S, sb, stC, pb, lgall, top8, idx8, maskall, Gall, onesf, tri, iota, bstart, eb_i, dest_i, gk, xn2_d, xbuf_d, pidx, OHall, ridx_i)
        barrier()
        if stage >= 4:
            moe_blocks(nc, S, sb, st, pb, pbb, OHall, ridx_i, wbf_d, btab_d, xbuf_d, ybuf_d, identb)
            barrier()
        if stage >= 5:
            ple_phase(nc, S, sb, st, pb, pbb, dest_i, gk, h1_d, ybuf_d, p_d, w_pg_d, w_pp_d, post_b_d, gcol_ple, identb, out_d, epsc)
        fin = [Tok(s, S.dcount[id(s)], None) for s in S.dsems.values()]
        S.finish(fin)
    return nc

_NC_CACHE = {}


def _consts():
    bf = ml_dtypes.bfloat16
    c = {}
    c["ident_bf"] = np.eye(128, dtype=np.float32).astype(bf)
    c["ident_f"] = np.eye(128, dtype=np.float32)
    c["ones_f"] = np.ones((128, 128), np.float32)
    k = np.arange(128)
    c["tri_strict"] = (k[:, None] < k[None, :]).astype(np.float32)
    c["cmask"] = (k[:, None] <= k[None, :]).astype(np.float32).astype(bf)
    blk = np.zeros((128, 128), np.float32)
    blk[:64, :64] = 1.0 / 64; blk[64:, 64:] = 1.0 / 64
    c["blkdiag"] = blk.astype(bf)
    r = np.zeros((128, 128), np.float32)
    for base in (0, 64):
        for m in range(8):
            r[base + m + 8, base + m] = -1.0
            r[base + m, base + m + 8] = 1.0
    c["rmat"] = r.astype(bf)
    freqs = (np.float32(500000.0) ** (-np.arange(0, 16, 2, dtype=np.float32) / np.float32(16))).astype(np.float32)
    f = np.zeros((128, 1), np.float32)
    for base in (0, 64):
        for i in range(8):
            f[base + i, 0] = freqs[i]; f[base + 8 + i, 0] = freqs[i]
    c["freq_col"] = f
    c["invcnt"] = np.broadcast_to((1.0 / np.arange(1, 17, dtype=np.float32))[None, :], (128, 16)).copy()
    c["iota_row"] = np.broadcast_to(np.arange(32, dtype=np.float32)[None, :], (128, 32)).copy()
    c["pidx_col"] = np.stack([np.arange(128, dtype=np.float32), (np.arange(128) % 32).astype(np.float32)], axis=1)
    c["bstart_row"] = np.broadcast_to((128.0 * np.arange(NBLK, dtype=np.float32))[None, :], (128, NBLK)).copy()
    return c


def _rep(v, n=128):
    return np.ascontiguousarray(np.broadcast_to(np.asarray(v)[None, ...], (n,) + tuple(np.asarray(v).shape)))


def make_in_maps(inputs):
    f32 = np.float32
    g = {k: np.asarray(v) for k, v in inputs.items()}
    shared = dict(_consts())
    shared["w_in"] = np.ascontiguousarray(g["w_in"][0], f32)
    shared["gcol_attn"] = np.ascontiguousarray(g["attn_norm"][0].reshape(8, 128).T)
    shared["qn_col"] = np.ascontiguousarray(np.tile(g["q_norm"][0], 2).reshape(128, 1))
    shared["kn_col"] = np.ascontiguousarray(np.tile(g["k_norm"][0], 2).reshape(128, 1))
    shared["lamv"] = _rep(np.stack([g["lam_q1"][0], g["lam_k1"][0], g["lam_q2"][0], g["lam_k2"][0]], 0))
    shared["subln_b"] = _rep(g["subln"][0])
    shared["w_pool"] = np.ascontiguousarray(g["w_pool"][0])
    shared["pscale_col"] = np.ascontiguousarray(g["pool_scale"][0].reshape(4, 128).T)
    shared["w_out"] = np.ascontiguousarray(g["w_out"][0])
    shared["ffn_b"] = _rep(g["ffn_norm"][0])
    shared["w_router"] = np.ascontiguousarray(g["w_router"][0].reshape(8, 128, 32).transpose(1, 0, 2))
    shared["b_router_b"] = _rep(g["b_router"][0])
    shared["w_gate"] = np.ascontiguousarray(g["w_gate"][0]); shared["w_up"] = np.ascontiguousarray(g["w_up"][0]); shared["w_down"] = np.ascontiguousarray(g["w_down"][0])
    shared["btab"] = np.ascontiguousarray(np.concatenate([g["b_gate"][0], g["b_up"][0], g["b_down"][0]], axis=1))
    shared["gcol_ple"] = np.ascontiguousarray(g["ple_gate_norm"][0].reshape(8, 128).T)
    shared["w_ple_gate"] = np.ascontiguousarray(g["w_ple_gate"][0]); shared["w_ple_proj"] = np.ascontiguousarray(g["w_ple_proj"][0])
    shared["post_b"] = _rep(g["ple_post_norm"][0])
    maps = []
    for b in range(8):
        m = dict(shared)
        m["x"] = np.ascontiguousarray(g["x"][b]); m["p"] = np.ascontiguousarray(g["p"][0, b])
        m["posb"] = _rep(g["positions"][b].astype(np.int32))
        maps.append(m)
    return maps


def kernel(**inputs):
    if "nc" not in _NC_CACHE:
        _NC_CACHE["nc"] = build()
    nc = _NC_CACHE["nc"]
    maps = make_in_maps(inputs)
    res = run_bass_kernel_spmd(nc, maps, core_ids=list(range(8)))
    return np.stack([np.asarray(r["out"]) for r in res.results], axis=0).astype(np.float32)
```

```python
import numpy as np
import concourse.bass as bass
import concourse.mybir as mybir

F32 = mybir.dt.float32
BF16 = mybir.dt.bfloat16
I32 = mybir.dt.int32
U32 = mybir.dt.uint32
ALU = mybir.AluOpType
AF = mybir.ActivationFunctionType
AX = mybir.AxisListType

PE, ACT, DVE, POOL, SP = "tensor", "scalar", "vector", "gpsimd", "sync"


class Tok:
    __slots__ = ("sem", "val", "eng")

    def __init__(self, sem, val, eng):
        self.sem, self.val, self.eng = sem, val, eng


class Rec:
    __slots__ = ("fn", "waits", "inc", "incv")

    def __init__(self, fn):
        self.fn, self.waits, self.inc, self.incv = fn, [], None, 0


class Sched:
    def __init__(self, nc, stack, n_dma_sems=96):
        self.nc = nc
        self.stack = stack
        self.ops = {e: [] for e in (PE, ACT, DVE, POOL, SP)}
        self.esem = {e: stack.enter_context(nc.semaphore("s_" + e)) for e in self.ops}
        self.ecount = {e: 0 for e in self.ops}
        self.pending = {e: False for e in self.ops}
        self.waited = {e: {} for e in self.ops}
        self.dsems = {}
        self.dcount = {}
        self.free_dsems = [stack.enter_context(nc.semaphore("d%d" % i)) for i in range(n_dma_sems)]
        self.writer = {}
        self.readers = {}

    def _need(self, eng, tok, same_ok):
        if tok is None:
            return None
        if tok.eng is not None:
            if self.ecount[tok.eng] < tok.val:
                last = self.ops[tok.eng][-1]
                assert last.inc is None
                last.inc, last.incv = self.esem[tok.eng], 1
                self.ecount[tok.eng] += 1
                self.pending[tok.eng] = False
                assert self.ecount[tok.eng] == tok.val
            val = tok.val
        else:
            val = self.dcount[id(tok.sem)]
        w = self.waited[eng]
        if w.get(id(tok.sem), 0) >= val:
            return None
        w[id(tok.sem)] = val
        return (tok.sem, val)

    _cap = None

    def begin_capture(self):
        self._cap = []

    def end_capture(self):
        c, self._cap = self._cap, None
        return c

    def replay_interleaved(self, lists):
        lists = [x for x in lists if x]
        if not lists:
            return
        L = max(len(x) for x in lists)
        for i in range(L):
            for x in lists:
                for j in range((i * len(x)) // L, ((i + 1) * len(x)) // L):
                    self.op(*x[j])

    def op(self, eng, fn, reads=(), writes=(), inc=None, dma=None, extra=()):
        if self._cap is not None:
            self._cap.append((eng, fn, tuple(reads), tuple(writes), inc, dma, tuple(extra)))
            return None
        raw, other = [], []
        for k in reads:
            t = self.writer.get(k)
            if t is not None:
                raw.append(t)
        for k in writes:
            t = self.writer.get(k)
            if t is not None:
                other.append(t)
            other.extend(self.readers.get(k, ()))
        raw.extend(extra)
        rec = Rec(fn)
        is_dma = dma is not None
        for t in raw:
            if t.eng == eng and not is_dma and eng == PE:
                continue
            wt = self._need(eng, t, False)
            if wt:
                rec.waits.append(wt)
        for t in other:
            if t.eng == eng and not is_dma and eng == PE:
                continue
            if is_dma and t.eng is None and dma in self.dsems and t.sem is self.dsems[dma]:
                continue
            wt = self._need(eng, t, False)
            if wt:
                rec.waits.append(wt)
        self.ops[eng].append(rec)
        if is_dma:
            if dma not in self.dsems:
                self.dsems[dma] = self.free_dsems.pop()
                self.dcount[id(self.dsems[dma])] = 0
            s = self.dsems[dma]
            self.dcount[id(s)] += 16
            rec.inc, rec.incv = s, 16
            tok = Tok(s, self.dcount[id(s)], None)
        else:
            if inc is None:
                inc = eng != PE
            if inc:
                self.ecount[eng] += 1
                rec.inc, rec.incv = self.esem[eng], 1
                self.pending[eng] = False
                tok = Tok(self.esem[eng], self.ecount[eng], eng)
            else:
                self.pending[eng] = True
                tok = Tok(self.esem[eng], self.ecount[eng] + 1, eng)
        for k in reads:
            self.readers.setdefault(k, []).append(tok)
        for k in writes:
            self.writer[k] = tok
            self.readers[k] = []
        return tok

    def finish(self, final_toks):
        rec = Rec(lambda e: e.nop())
        for t in final_toks:
            wt = self._need(SP, t, False)
            if wt:
                rec.waits.append(wt)
        self.ops[SP].append(rec)
        nc = self.nc
        with nc.Block() as block:
            def emit(name):
                def run(e):
                    for r in self.ops[name]:
                        for (s, v) in r.waits:
                            e.wait_ge(s, v)
                        ins = r.fn(e)
                        if r.inc is not None:
                            ins.then_inc(r.inc, r.incv)
                return run
            block.tensor(emit(PE))
            block.scalar(emit(ACT))
            block.vector(emit(DVE))
            block.gpsimd(emit(POOL))
            block.sync(emit(SP))
def dispatch(nc, S, sb, stC, pb, lgall, top8, idx8, maskall, Gall, onesf, tri, iota, bstart, eb_i, dest_i, gk, xn2_d, xbuf_d, pidx, OHall, ridx_i):
    NT, NBLK = 32, 160
    allk = [("lg", t) for t in range(NT)]
    exl = sb("exl", [128, NT, 32], F32, stC); sums = sb("sums", [128, NT], F32, stC)
    negmax = sb("negmax", [128, NT, 1], F32, stC)
    S.op(DVE, lambda e: e.tensor_tensor(out=maskall[:], in0=lgall[:], in1=top8[:, :, 3:4].to_broadcast([128, NT, 32]), op=ALU.is_ge),
         reads=allk + [("top8", t) for t in range(NT)], writes=["maskall"])
    S.op(DVE, lambda e: e.tensor_tensor(out=exl[:], in0=lgall[:], in1=top8[:, :, 0:1].to_broadcast([128, NT, 32]), op=ALU.subtract),
         reads=allk + [("top8", t) for t in range(NT)], writes=["exl"])
    S.op(ACT, lambda e: e.activation(out=exl[:], in_=exl[:], func=AF.Exp), reads=["exl"], writes=["exl"])
    S.op(DVE, lambda e: e.tensor_tensor(out=exl[:], in0=exl[:], in1=maskall[:], op=ALU.mult), reads=["exl", "maskall"], writes=["exl"])
    S.op(DVE, lambda e: e.tensor_reduce(out=sums[:], in_=exl[:], axis=AX.X, op=ALU.add), reads=["exl"], writes=["sums"])
    S.op(DVE, lambda e: e.reciprocal(out=sums[:], in_=sums[:]), reads=["sums"], writes=["sums"])
    S.op(DVE, lambda e: e.tensor_tensor(out=Gall[:], in0=exl[:], in1=sums[:].unsqueeze(2).to_broadcast([128, NT, 32]), op=ALU.mult), reads=["exl", "sums"], writes=["Gall"])
    mflat = maskall[:].rearrange("p t e -> p (t e)")
    S.op(PE, lambda e: e.matmul(pb[5][:], lhsT=onesf[:], rhs=mflat[:, 0:512], start=True, stop=True), reads=["maskall"], writes=["pb5"], inc=True)
    S.op(PE, lambda e: e.matmul(pb[6][:], lhsT=onesf[:], rhs=mflat[:, 512:1024], start=True, stop=True), reads=["maskall"], writes=["pb6"], inc=True)
    csA = sb("csA", [128, 48, 32], F32, stC); csB = sb("csB", [128, 48, 32], F32, stC); cs0 = sb("cs0", [128, NT, 32], F32, stC)
    S.op(DVE, lambda e: e.memset(csA[:, 0:16, :], 0.0), writes=["csA"])
    S.op(DVE, lambda e: e.memset(csB[:, 0:16, :], 0.0), writes=["csB"])
    S.op(DVE, lambda e: e.tensor_copy(out=csA[:, 16:32, :], in_=pb[5][:].rearrange("p (t e) -> p t e", e=32)), reads=["pb5"], writes=["csA"])
    S.op(DVE, lambda e: e.tensor_copy(out=csA[:, 32:48, :], in_=pb[6][:].rearrange("p (t e) -> p t e", e=32)), reads=["pb6"], writes=["csA"])
    S.op(DVE, lambda e: e.tensor_copy(out=cs0[:], in_=csA[:, 16:48, :]), reads=["csA"], writes=["cs0"])
    cur, curk, oth, othk = csA, "csA", csB, "csB"
    for j in range(5):
        sh = 1 << j
        S.op(DVE, lambda e, cur=cur, oth=oth, sh=sh: e.tensor_tensor(out=oth[:, 16:48, :], in0=cur[:, 16:48, :], in1=cur[:, 16 - sh:48 - sh, :], op=ALU.add), reads=[curk], writes=[othk])
        cur, curk, oth, othk = oth, othk, cur, curk
    incl = cur; inclk = curk
    cnt = sb("cnt", [128, 32], F32, stC); padd = sb("padd", [128, 32], F32, stC)
    scA = sb("scA", [128, 64], F32, stC); scB = sb("scB", [128, 64], F32, stC); pstart = sb("pstart", [128, 32], F32, stC)
    S.op(DVE, lambda e: e.tensor_copy(out=cnt[:], in_=incl[:, 47, :]), reads=[inclk], writes=["cnt"])
    cmpc = sb("cmpc", [128, 32, 32], F32, stC)
    S.op(DVE, lambda e: e.tensor_tensor(out=cmpc[:], in0=cnt[:].unsqueeze(2).to_broadcast([128, 32, 32]), in1=bstart[:, 0:32].unsqueeze(1).to_broadcast([128, 32, 32]), op=ALU.is_gt),
         reads=["cnt"], writes=["cmpc"])
    S.op(DVE, lambda e: e.tensor_reduce(out=padd[:], in_=cmpc[:], axis=AX.X, op=ALU.add), reads=["cmpc"], writes=["padd"])
    S.op(DVE, lambda e: e.tensor_scalar(out=padd[:], in0=padd[:], scalar1=128.0, scalar2=None, op0=ALU.mult), reads=["padd"], writes=["padd"])
    S.op(DVE, lambda e: e.memset(scA[:, 0:32], 0.0), writes=["scA"])
    S.op(DVE, lambda e: e.memset(scB[:, 0:32], 0.0), writes=["scB"])
    S.op(DVE, lambda e: e.tensor_copy(out=scA[:, 32:64], in_=padd[:]), reads=["padd"], writes=["scA"])
    cur, curk, oth, othk = scA, "scA", scB, "scB"
    for j in range(5):
        sh = 1 << j
        S.op(DVE, lambda e, cur=cur, oth=oth, sh=sh: e.tensor_tensor(out=oth[:, 32:64], in0=cur[:, 32:64], in1=cur[:, 32 - sh:64 - sh], op=ALU.add), reads=[curk], writes=[othk])
        cur, curk, oth, othk = oth, othk, cur, curk
    pend = cur; pendk = curk
    S.op(DVE, lambda e: e.tensor_tensor(out=pstart[:], in0=pend[:, 32:64], in1=padd[:], op=ALU.subtract), reads=[pendk, "padd"], writes=["pstart"])
    base = sb("base", [128, NT, 32], F32, stC)
    S.op(DVE, lambda e: e.tensor_tensor(out=base[:], in0=incl[:, 16:48, :], in1=cs0[:], op=ALU.subtract), reads=[inclk, "cs0"], writes=["base"])
    S.op(DVE, lambda e: e.tensor_tensor(out=base[:], in0=base[:], in1=pstart[:].unsqueeze(1).to_broadcast([128, NT, 32]), op=ALU.add), reads=["base", "pstart"], writes=["base"])
    for t in range(NT):
        bank = 5 + t // 16
        S.op(PE, lambda e, t=t, bank=bank: e.matmul(pb[bank][:, (t % 16) * 32:(t % 16 + 1) * 32], lhsT=tri[:], rhs=maskall[:, t, :], start=True, stop=True),
             reads=["maskall"], writes=["pb%d" % bank], inc=(t % 16 == 15))
    slot = sb("slot", [128, NT, 32], F32, stC)
    S.op(DVE, lambda e: e.tensor_tensor(out=slot[:, 0:16, :], in0=pb[5][:].rearrange("p (t e) -> p t e", e=32), in1=base[:, 0:16, :], op=ALU.add), reads=["pb5", "base"], writes=["slot"])
    S.op(DVE, lambda e: e.tensor_tensor(out=slot[:, 16:32, :], in0=pb[6][:].rearrange("p (t e) -> p t e", e=32), in1=base[:, 16:32, :], op=ALU.add), reads=["pb6", "base"], writes=["slot"])
    ebf = sb("ebf", [128, NBLK], F32, stC)
    S.op(DVE, lambda e: e.memset(ebf[:], 0.0), writes=["ebf"])
    for ee in range(32):
        S.op(DVE, lambda e, ee=ee: e.scalar_tensor_tensor(out=ebf[:], in0=bstart[:], scalar=pend[:, 32 + ee:33 + ee], in1=ebf[:], op0=ALU.is_ge, op1=ALU.add),
             reads=[pendk, "ebf"], writes=["ebf"])
    S.op(DVE, lambda e: e.tensor_scalar(out=ebf[:], in0=ebf[:], scalar1=31.0, scalar2=None, op0=ALU.min), reads=["ebf"], writes=["ebf"])
    S.op(DVE, lambda e: e.tensor_copy(out=eb_i[:], in_=ebf[:]), reads=["ebf"], writes=["eb_i"])
    S.op(DVE, lambda e: e.tensor_scalar(out=OHall[:], in0=ebf[:], scalar1=pidx[:, 1:2], scalar2=None, op0=ALU.is_equal), reads=["ebf"], writes=["OHall"])
    neq = sb("neq", [128, NBLK], F32, stC); ridxf = sb("ridxf", [128, NBLK], F32, stC)
    import os as _os2
    S.op(DVE, lambda e: e.memset(neq[:, 0:2], 0.0 if _os2.environ.get('KNOLOAD') else 1.0), writes=["neq"])
    S.op(DVE, lambda e: e.tensor_tensor(out=neq[:, 2:NBLK], in0=ebf[:, 2:NBLK], in1=ebf[:, 0:NBLK - 2], op=(ALU.is_lt if _os2.environ.get('KNOLOAD') else ALU.not_equal)), reads=["ebf"], writes=["neq"])
    S.op(DVE, lambda e: e.tensor_scalar(out=ridxf[:], in0=ebf[:], scalar1=128.0, scalar2=pidx[:, 0:1], op0=ALU.mult, op1=ALU.add), reads=["ebf"], writes=["ridxf"])
    S.op(DVE, lambda e: e.scalar_tensor_tensor(out=ridxf[:], in0=ridxf[:], scalar=-1.0e6, in1=neq[:], op0=ALU.add, op1=ALU.mult), reads=["ridxf", "neq"], writes=["ridxf"])
    S.op(DVE, lambda e: e.tensor_scalar(out=ridxf[:], in0=ridxf[:], scalar1=1.0e6, scalar2=None, op0=ALU.add), reads=["ridxf"], writes=["ridxf"])
    S.op(DVE, lambda e: e.tensor_copy(out=ridx_i[:], in_=ridxf[:]), reads=["ridxf"], writes=["ridx"])
    idxf = sb("idxf", [128, NT, 4], F32, stC); oh = sb("oh", [128, NT, 32], F32, stC); oh2 = sb("oh2", [128, NT, 32], F32, stC)
    destf = sb("destf", [128, NT, 4], F32, stC)
    S.op(DVE, lambda e: e.tensor_copy(out=idxf[:], in_=idx8[:, :, 0:4]), reads=[("idx8", t) for t in range(NT)], writes=["idxf"])
    for k in range(4):
        S.op(DVE, lambda e, k=k: e.tensor_tensor(out=oh[:], in0=iota[:].unsqueeze(1).to_broadcast([128, NT, 32]), in1=idxf[:, :, k:k + 1].to_broadcast([128, NT, 32]), op=ALU.is_equal),
             reads=["idxf"], writes=["oh"])
        S.op(DVE, lambda e: e.tensor_tensor(out=oh2[:], in0=oh[:], in1=slot[:], op=ALU.mult), reads=["oh", "slot"], writes=["oh2"])
        S.op(DVE, lambda e, k=k: e.tensor_reduce(out=destf[:, :, k], in_=oh2[:], axis=AX.X, op=ALU.add), reads=["oh2"], writes=["destf"])
        S.op(DVE, lambda e: e.tensor_tensor(out=oh2[:], in0=oh[:], in1=Gall[:], op=ALU.mult), reads=["oh", "Gall"], writes=["oh2"])
        S.op(DVE, lambda e, k=k: e.tensor_reduce(out=gk[:, :, k], in_=oh2[:], axis=AX.X, op=ALU.add), reads=["oh2"], writes=["gk"])
    S.op(DVE, lambda e: e.tensor_copy(out=dest_i[:], in_=destf[:]), reads=["destf"], writes=["dest_i"])
    xs = [sb("xs%d" % i, [128, 1024], BF16, stC) for i in range(2)]
    for t in range(NT):
        S.op(SP, lambda e, t=t: e.dma_start(out=xs[t % 2][:], in_=xn2_d[t * 128:(t + 1) * 128, :]), reads=["xn2_d"], writes=[("xs", t % 2)], dma="xs%d" % (t % 2))
        for k in range(4):
            S.op(POOL, lambda e, t=t, k=k: e.indirect_dma_start(out=xbuf_d, out_offset=bass.IndirectOffsetOnAxis(ap=dest_i[:, t, k:k + 1], axis=0), in_=xs[t % 2][:], in_offset=None),
                 reads=[("xs", t % 2), "dest_i"], writes=["xbuf"], dma="scat")


def moe_blocks(nc, S, sb, st, pb, pbb, OHall, ridx_i, wbf_d, btab_d, xbuf_d, ybuf_d, identb, nblk=160):
    import os as _o3
    NOW = bool(_o3.environ.get('KNOW'))
    with ExitStack() as stD:
        W = {nm: [sb("%s%d" % (nm, i), [128, 8, 1024], BF16, stD) for i in range(2)] for nm in ("wg", "wu", "wd")}
        Wd_ = {nm: wbf_d[m_].rearrange("(r c2) x -> r (c2 x)", c2=4) for m_, nm in enumerate(("wg", "wu", "wd"))}
        btab = sb("btab", [64, 3072], F32, stD); bt2 = sb("bt2", [64, 3072], BF16, stD); btl = sb("btl", [64, 3072], F32, stD)
        S.op(SP, lambda e: e.dma_start(out=btab[0:32, :], in_=btab_d), writes=["btab"], dma="btab")
        S.op(SP, lambda e: e.dma_start(out=btab[32:64, :], in_=btab_d), writes=["btab"], dma="btab")
        S.op(DVE, lambda e: e.tensor_copy(out=bt2[:], in_=btab[:]), reads=["btab"], writes=["bt2"])
        S.op(DVE, lambda e: e.tensor_copy(out=btl[:], in_=bt2[:]), reads=["bt2"], writes=["btl"])
        S.op(DVE, lambda e: e.tensor_tensor(out=btl[:], in0=btab[:], in1=btl[:], op=ALU.subtract), reads=["btab", "btl"], writes=["btl"])
        S.op(DVE, lambda e: e.tensor_copy(out=bt2[32:64, :], in_=btl[32:64, :]), reads=["btl", "bt2"], writes=["bt2"])
        ohb = [sb("ohb%d" % i, [64, 128], BF16, stD) for i in range(2)]
        xb = [sb("xb%d" % i, [128, 1024], BF16, stD) for i in range(2)]
        xT = [sb("xT%d" % i, [128, 8, 128], BF16, stD) for i in range(2)]
        gtb = [sb("gtb%d" % i, [128, 512], F32, stD) for i in range(2)]; sgb = [sb("sgb%d" % i, [128, 512], F32, stD) for i in range(2)]
        upb = [sb("upb%d" % i, [128, 512], F32, stD) for i in range(2)]
        hdn = [sb("hdn%d" % i, [128, 1024], BF16, stD) for i in range(2)]
        hT = [sb("hT%d" % i, [128, 8, 128], BF16, stD) for i in range(2)]
        yb = [sb("yb%d" % i, [128, 1024], F32, stD) for i in range(2)]
        breg = stD.enter_context(nc.gpsimd.register("bnd_reg"))
        S.op(POOL, lambda e: e.reg_mov(breg, 32 * 128 - 1))

        def gath(nm, b):
            par = b % 2
            S.op(POOL, lambda e: e.indirect_dma_start(out=W[nm][par][:].rearrange("p c f -> p (c f)"), out_offset=None, in_=Wd_[nm],
                 in_offset=bass.IndirectOffsetOnAxis(ap=ridx_i[:, b:b + 1], axis=0), bounds_check=breg, oob_is_err=False),
                 reads=["ridx", "wbf"], writes=[(nm, par, c) for c in range(8)], dma="%s%d" % (nm, par))

        def P1(b):
            par = b % 2
            gath("wg", b); gath("wu", b)
            if b >= 1:
                gath("wd", b - 1)
            if b == 0:
                S.op(SP, lambda e: e.dma_start(out=xb[0][:], in_=xbuf_d[0:128, :]), writes=[("xb", 0)], dma="xb0")
            if b + 1 < nblk:
                S.op(SP, lambda e, b=b: e.dma_start(out=xb[(b + 1) % 2][:], in_=xbuf_d[(b + 1) * 128:(b + 2) * 128, :]), writes=[("xb", (b + 1) % 2)], dma="xb%d" % ((b + 1) % 2))
            S.op(DVE, lambda e, b=b, par=par: e.tensor_copy(out=ohb[par][:], in_=OHall[0:64, b:b + 1].to_broadcast([64, 128])), reads=["OHall"], writes=[("ohb", par)])
            for c in range(8):
                S.op(PE, lambda e, c=c, par=par: e.transpose(out=pbb[6][:, c * 128:(c + 1) * 128], in_=xb[par][:, c * 128:(c + 1) * 128], identity=identb[:]),
                     reads=[("xb", par)], writes=["pb6"], inc=(c == 7))
            S.op(ACT, lambda e, par=par: e.activation(out=xT[par][:], in_=pbb[6].rearrange("p (c t) -> p c t", c=8), func=AF.Copy), reads=["pb6"], writes=[("xT", par)])

        def P2(b):
            par = b % 2
            for hf in range(2):
                accs = [("wg", 2 * hf, hf * 512), ("wu", 2 * hf + 1, 1024 + hf * 512)]
                for (nm, a, boff) in accs:
                    S.op(PE, lambda e, a=a, boff=boff, par=par: e.matmul(pb[a][:], lhsT=ohb[par][:], rhs=bt2[:, boff:boff + 512], start=True, stop=False),
                         reads=[("ohb", par), "bt2"], writes=["pb%d" % a])
                for c in range(8):
                    for (nm, a, boff) in accs:
                        S.op(PE, lambda e, a=a, nm=nm, hf=hf, c=c, par=par: e.matmul(pb[a][:], lhsT=xT[par][:, c, :], rhs=W[nm][par][:, c, hf * 512:(hf + 1) * 512], start=False, stop=(c == 7)),
                             reads=[("xT", par)] + ([] if NOW else [(nm, par, c)]), writes=["pb%d" % a], inc=(c == 7))
                G, U = pb[2 * hf], pb[2 * hf + 1]; gk_, uk_ = "pb%d" % (2 * hf), "pb%d" % (2 * hf + 1)
                S.op(DVE, lambda e, hf=hf, G=G: e.tensor_scalar(out=gtb[hf][:], in0=G[:], scalar1=7.0, scalar2=None, op0=ALU.min), reads=[gk_], writes=[("gtb", hf)])
                S.op(ACT, lambda e, hf=hf: e.activation(out=sgb[hf][:], in_=gtb[hf][:], func=AF.Sigmoid, scale=1.702), reads=[("gtb", hf)], writes=[("sgb", hf)])
                S.op(DVE, lambda e, hf=hf, U=U: e.tensor_scalar(out=upb[hf][:], in0=U[:], scalar1=-7.0, scalar2=7.0, op0=ALU.max, op1=ALU.min), reads=[uk_], writes=[("upb", hf)])
                S.op(DVE, lambda e, hf=hf: e.scalar_tensor_tensor(out=upb[hf][:], in0=upb[hf][:], scalar=1.0, in1=gtb[hf][:], op0=ALU.add, op1=ALU.mult), reads=[("upb", hf), ("gtb", hf)], writes=[("upb", hf)])
                S.op(DVE, lambda e, hf=hf, par=par: e.tensor_tensor(out=hdn[par][:].rearrange("s (c p) -> s p c", p=128)[:, hf * 64:(hf + 1) * 64, :], in0=upb[hf][:].rearrange("s (p c) -> s p c", c=8),
                     in1=sgb[hf][:].rearrange("s (p c) -> s p c", c=8), op=ALU.mult), reads=[("upb", hf), ("sgb", hf)], writes=[("hdn", par)])

        def P3(b):
            par = b % 2
            for c in range(8):
                S.op(PE, lambda e, c=c, par=par: e.transpose(out=pbb[7][:, c * 128:(c + 1) * 128], in_=hdn[par][:, c * 128:(c + 1) * 128], identity=identb[:]),
                     reads=[("hdn", par)], writes=["pb7"], inc=(c == 7))
            S.op(ACT, lambda e, par=par: e.activation(out=hT[par][:], in_=pbb[7].rearrange("p (c t) -> p c t", c=8), func=AF.Copy), reads=["pb7"], writes=[("hT", par)])
            for hf in range(2):
                S.op(PE, lambda e, hf=hf, par=par: e.matmul(pb[4 + hf][:], lhsT=ohb[par][:], rhs=bt2[:, 2048 + hf * 512:2048 + (hf + 1) * 512], start=True, stop=False),
                     reads=[("ohb", par), "bt2"], writes=["pb%d" % (4 + hf)])
                for c in range(8):
                    S.op(PE, lambda e, hf=hf, c=c, par=par: e.matmul(pb[4 + hf][:], lhsT=hT[par][:, c, :], rhs=W["wd"][par][:, c, hf * 512:(hf + 1) * 512], start=False, stop=(c == 7)),
                         reads=[("hT", par)] + ([] if NOW else [("wd", par, c)]), writes=["pb%d" % (4 + hf)], inc=(c == 7))
                if hf == 0:
                    S.op(ACT, lambda e, hf=hf, par=par: e.activation(out=yb[par][:, hf * 512:(hf + 1) * 512], in_=pb[4 + hf][:], func=AF.Copy), reads=["pb%d" % (4 + hf)], writes=[("yb", par)])
                else:
                    S.op(DVE, lambda e, hf=hf, par=par: e.tensor_copy(out=yb[par][:, hf * 512:(hf + 1) * 512], in_=pb[4 + hf][:]), reads=["pb%d" % (4 + hf)], writes=[("yb", par)])
            S.op(SP, lambda e, b=b, par=par: e.dma_start(out=ybuf_d[b * 128:(b + 1) * 128, :], in_=yb[par][:]), reads=[("yb", par)], writes=["ybuf"], dma="yst%d" % par)

        P1(0); P2(0)
        for b in range(1, nblk):
            P1(b); P2(b); P3(b - 1)
        gath("wd", nblk - 1)
        P3(nblk - 1)


def run_pipelined(tile_ops, ntiles, skew):
    lists = {}
    nops = None
    s = 0
    done = 0
    while done < ntiles:
        for t in range(ntiles):
            st_ = s - t * skew
            if st_ < 0:
                break
            if t not in lists:
                lists[t] = tile_ops(t)
                nops = len(lists[t])
            if st_ < len(lists[t]):
                lists[t][st_]()
                if st_ == len(lists[t]) - 1:
                    done += 1
        s += 1


def ple_phase(nc, S, sb, st, pb, pbb, dest_i, gk, h1_d, ybuf_d, p_d, w_pg_d, w_pp_d, post_b_d, gcol_ple, identb, out_d, epsc):
    NT = 32
    with ExitStack() as stE:
        w_pg = sb("w_pg", [128, 8, 1024], BF16, stE); w_pp = sb("w_pp", [128, 2, 1024], BF16, stE); post_b = sb("post_b", [128, 1024], F32, stE)
        for c in range(8):
            S.op(POOL, lambda e, c=c: e.dma_start(out=w_pg[:, c, :], in_=w_pg_d[c * 128:(c + 1) * 128, :]), writes=[("w_pg", c)], dma="w_pg")
        for c in range(8):
            S.op(DVE, lambda e, c=c: e.tensor_scalar(out=w_pg[:, c, :], in0=w_pg[:, c, :], scalar1=gcol_ple[:, c:c + 1], scalar2=None, op0=ALU.mult), reads=[("w_pg", c)], writes=[("w_pg", c)])
        for c in range(2):
            S.op(POOL, lambda e, c=c: e.dma_start(out=w_pp[:, c, :], in_=w_pp_d[c * 128:(c + 1) * 128, :]), writes=["w_pp"], dma="w_pp")
        S.op(SP, lambda e: e.dma_start(out=post_b[:], in_=post_b_d), writes=["post_b"], dma="cE")
        D = lambda nm, shape, dt=F32: [sb("%s_%d" % (nm, i), shape, dt, stE) for i in range(3)]
        yk = [[sb("yk%d_%d" % (i, k), [128, 1024], F32, stE) for k in range(4)] for i in range(3)]
        h2 = D("h2", [128, 1024]); pt = D("pt", [128, 256]); ptb = D("ptb", [128, 256], BF16); pT = D("pT", [128, 2, 128], BF16)
        hn3 = D("hn3", [128, 1024], BF16); hn3T = D("hn3T", [128, 8, 128], BF16); ejunk = D("ejunk", [128, 1024], BF16)
        gate = D("gate", [128, 1024]); et = D("et", [128, 1024]); st5 = D("st5", [128, 8])

        def tile_ops(t):
            par = t % 3
            ops = []
            A = ops.append
            P = lambda nm: (nm, par)
            A(lambda: S.op(SP, lambda e: e.dma_start(out=h2[par][:], in_=h1_d[t * 128:(t + 1) * 128, :]), reads=["h1_d"], writes=[P("h2")], dma="h2_%d" % par))
            A(lambda: S.op(SP, lambda e: e.dma_start(out=pt[par][:], in_=p_d[t * 128:(t + 1) * 128, :]), writes=[P("pt")], dma="pt%d" % par))
            for k in range(4):
                A(lambda k=k: S.op(POOL, lambda e: e.indirect_dma_start(out=yk[par][k][:], out_offset=None, in_=ybuf_d, in_offset=bass.IndirectOffsetOnAxis(ap=dest_i[:, t, k:k + 1], axis=0)),
                                   reads=["ybuf", "dest_i"], writes=[("yk", par, k)], dma="yk%d" % par))
            A(lambda: S.op(ACT, lambda e: e.activation(out=ptb[par][:], in_=pt[par][:], func=AF.Copy), reads=[P("pt")], writes=[P("ptb")]))
            for c in range(2):
                A(lambda c=c: S.op(PE, lambda e: e.transpose(out=pbb[3][:, c * 128:(c + 1) * 128], in_=ptb[par][:, c * 128:(c + 1) * 128], identity=identb[:]), reads=[P("ptb")], writes=["pb3"], inc=(c == 1)))
            A(lambda: S.op(DVE, lambda e: e.tensor_copy(out=pT[par][:], in_=pbb[3][:, 0:256].rearrange("p (c t) -> p c t", c=2)), reads=["pb3"], writes=[P("pT")]))
            A(lambda: S.op(DVE, lambda e: e.memset(st5[par][:, 0:3], 0.0), writes=[P("st5")]))
            for hf in range(2):
                for c in range(2):
                    A(lambda c=c, hf=hf: S.op(PE, lambda e: e.matmul(pb[4 + hf][:], lhsT=pT[par][:, c, :], rhs=w_pp[:, c, hf * 512:(hf + 1) * 512], start=(c == 0), stop=(c == 1)),
                                              reads=[P("pT"), "w_pp"], writes=["pb%d" % (4 + hf)], inc=(c == 1)))
                A(lambda hf=hf: S.op(ACT, lambda e: e.activation(out=ejunk[par][:, hf * 512:(hf + 1) * 512], in_=pb[4 + hf][:], func=AF.Square, accum_out=st5[par][:, 1 + hf:2 + hf]),
                                     reads=["pb%d" % (4 + hf), P("st5")], writes=[P("ejunk"), ("st5e", par, hf)]))
            A(lambda: S.op(DVE, lambda e: e.tensor_tensor(out=st5[par][:, 4:5], in0=st5[par][:, 1:2], in1=st5[par][:, 2:3], op=ALU.add), reads=[("st5e", par, 0), ("st5e", par, 1)], writes=[P("st5s")]))
            A(lambda: S.op(ACT, lambda e: e.activation(out=st5[par][:, 4:5], in_=st5[par][:, 4:5], func=AF.Ln, scale=1.0 / 1024, bias=epsc[:, 0:1]), reads=[P("st5s")], writes=[P("st5s")]))
            A(lambda: S.op(ACT, lambda e: e.activation(out=st5[par][:, 4:5], in_=st5[par][:, 4:5], func=AF.Exp, scale=-0.5), reads=[P("st5s")], writes=[P("st5s")]))
            for hf in range(2):
                A(lambda hf=hf: S.op(DVE, lambda e: e.scalar_tensor_tensor(out=et[par][:, hf * 512:(hf + 1) * 512], in0=pb[4 + hf][:], scalar=st5[par][:, 4:5], in1=post_b[:, hf * 512:(hf + 1) * 512], op0=ALU.mult, op1=ALU.mult),
                                     reads=["pb%d" % (4 + hf), P("st5s"), "post_b"], writes=[P("et")]))
            A('STAGE')
            for k in range(4):
                A(lambda k=k: S.op(DVE, lambda e: e.scalar_tensor_tensor(out=h2[par][:], in0=yk[par][k][:], scalar=gk[:, t, k:k + 1], in1=h2[par][:], op0=ALU.mult, op1=ALU.add),
                                   reads=[("yk", par, k), P("h2"), "gk"], writes=[P("h2")]))
            A(lambda: S.op(ACT, lambda e: e.activation(out=ejunk[par][:], in_=h2[par][:], func=AF.Square, accum_out=st5[par][:, 0:1]), reads=[P("h2"), P("st5")], writes=[P("ejunk"), P("st5a")]))
            A(lambda: S.op(ACT, lambda e: e.activation(out=st5[par][:, 3:4], in_=st5[par][:, 0:1], func=AF.Ln, scale=1.0 / 1024, bias=epsc[:, 0:1]), reads=[P("st5a")], writes=[P("st5r")]))
            A(lambda: S.op(ACT, lambda e: e.activation(out=st5[par][:, 3:4], in_=st5[par][:, 3:4], func=AF.Exp, scale=-0.5), reads=[P("st5r")], writes=[P("st5r")]))
            A(lambda: S.op(ACT, lambda e: e.activation(out=hn3[par][:], in_=h2[par][:], func=AF.Copy, scale=st5[par][:, 3:4]), reads=[P("h2"), P("st5r")], writes=[P("hn3")]))
            for c in range(8):
                A(lambda c=c: S.op(PE, lambda e: e.transpose(out=pbb[0][:, c * 128:(c + 1) * 128], in_=hn3[par][:, c * 128:(c + 1) * 128], identity=identb[:]), reads=[P("hn3")], writes=["pb0"], inc=(c == 7)))
            A(lambda: S.op(DVE, lambda e: e.tensor_copy(out=hn3T[par][:], in_=pbb[0].rearrange("p (c t) -> p c t", c=8)), reads=["pb0"], writes=[P("hn3T")]))
            A('STAGE')
            for hf in range(2):
                for c in range(8):
                    A(lambda c=c, hf=hf: S.op(PE, lambda e: e.matmul(pb[1 + hf][:], lhsT=hn3T[par][:, c, :], rhs=w_pg[:, c, hf * 512:(hf + 1) * 512], start=(c == 0), stop=(c == 7)),
                                              reads=[P("hn3T"), ("w_pg", c)], writes=["pb%d" % (1 + hf)], inc=(c == 7)))
                A(lambda hf=hf: S.op(ACT, lambda e: e.activation(out=gate[par][:, hf * 512:(hf + 1) * 512], in_=pb[1 + hf][:], func=AF.Sigmoid), reads=["pb%d" % (1 + hf)], writes=[P("gate")]))
            A(lambda: S.op(DVE, lambda e: e.tensor_tensor(out=et[par][:], in0=et[par][:], in1=gate[par][:], op=ALU.mult), reads=[P("et"), P("gate")], writes=[P("et")]))
            A(lambda: S.op(DVE, lambda e: e.tensor_tensor(out=et[par][:], in0=et[par][:], in1=h2[par][:], op=ALU.add), reads=[P("et"), P("h2")], writes=[P("et")]))
            A(lambda: S.op(SP, lambda e: e.dma_start(out=out_d[t * 128:(t + 1) * 128, :], in_=et[par][:]), reads=[P("et")], writes=["out_d"], dma="out%d" % par))
            stages = [[]]
            for o in ops:
                if o == 'STAGE':
                    stages.append([])
                else:
                    stages[-1].append(o)
            return stages
        cache = {}

        def get(t):
            if t not in cache:
                cache[t] = tile_ops(t)
            return cache[t]
        for it in range(NT + 2):
            lists = [get(t)[stg] for stg, t in ((2, it - 2), (1, it - 1), (0, it)) if 0 <= t < NT]
            L = max(len(x) for x in lists)
            for i in range(L):
                for x in lists:
                    j0 = (i * len(x)) // L
                    j1 = ((i + 1) * len(x)) // L
                    for j in range(j0, j1):
                        x[j]()
import math
import os as _os
from contextlib import ExitStack
import ml_dtypes
from concourse.bass_utils import run_bass_kernel_spmd

S_TOK = 4096
NT = 32
NG = 8
NBLK = 160
NSLOT = NBLK * 128
LAMBDA_INIT = 0.2
EPS = 1e-6
DEBUG = False


def build(stage=99):
    nc = bass.Bass("TRN2", target_bir_lowering=False)

    def din(name, shape, dt=F32):
        return nc.dram_tensor(name, list(shape), dt, kind="ExternalInput").ap()

    x_d = din("x", [S_TOK, 1024]); p_d = din("p", [S_TOK, 256]); pos_d = din("posb", [128, S_TOK], I32)
    w_in_d = din("w_in", [1024, 2048]); gcol_attn_d = din("gcol_attn", [128, 8])
    qn_d = din("qn_col", [128, 1]); kn_d = din("kn_col", [128, 1]); lamv_d = din("lamv", [128, 4, 64])
    subln_d = din("subln_b", [128, 128]); w_pool_d = din("w_pool", [4, 128, 128]); pscale_d = din("pscale_col", [128, 4])
    w_out_d = din("w_out", [1024, 1024]); ffn_b_d = din("ffn_b", [128, 1024]); w_r_d = din("w_router", [128, 8, 32])
    b_r_d = din("b_router_b", [128, 32])
    wg_d = din("w_gate", [32, 1024, 1024]); wu_d = din("w_up", [32, 1024, 1024]); wd_d = din("w_down", [32, 1024, 1024])
    btab_d = din("btab", [32, 3072])
    gcol_ple_d = din("gcol_ple", [128, 8]); w_pg_d = din("w_ple_gate", [1024, 1024]); w_pp_d = din("w_ple_proj", [256, 1024])
    post_b_d = din("post_b", [128, 1024])
    identb_d = din("ident_bf", [128, 128], BF16); identf_d = din("ident_f", [128, 128]); onesf_d = din("ones_f", [128, 128])
    tri_d = din("tri_strict", [128, 128]); blk_d = din("blkdiag", [128, 128], BF16); rmat_d = din("rmat", [128, 128], BF16)
    cmask_d = din("cmask", [128, 128], BF16); freq_d = din("freq_col", [128, 1]); invc_d = din("invcnt", [128, 16])
    iota_d = din("iota_row", [128, 32]); bstart_d = din("bstart_row", [128, NBLK]); pidx_d = din("pidx_col", [128, 2])
    out_d = nc.dram_tensor("out", [S_TOK, 1024], F32, kind="ExternalOutput").ap()
    KS = "ExternalOutput" if DEBUG else "Internal"
    catp_d = nc.dram_tensor("catp_s", [4, 128, S_TOK], BF16, kind=KS).ap()
    if DEBUG:
        dbg_q = nc.dram_tensor("dbg_q", [128, 4, S_TOK], BF16, kind=KS).ap(); dbg_k = nc.dram_tensor("dbg_k", [128, 4, S_TOK], BF16, kind=KS).ap()
        dbg_v = nc.dram_tensor("dbg_v", [128, NT, 4, 130], BF16, kind=KS).ap(); dbg_g = nc.dram_tensor("dbg_g", [128, 1024], BF16, kind=KS).ap(); dbg_v2 = nc.dram_tensor("dbg_v2", [128, NT, 4, 130], BF16, kind=KS).ap()
        dbg_r = nc.dram_tensor("dbg_r", [128, NT * 4 * 2 + NBLK], F32, kind=KS).ap()
    cata_d = nc.dram_tensor("cata_s", [128, 4, S_TOK], BF16, kind=KS).ap()
    wbf_d = [nc.dram_tensor("wbf_s%d" % i, [32 * 512, 2048], BF16, kind="Internal").ap() for i in range(3)]
    h1_d = nc.dram_tensor("h1_s", [S_TOK, 1024], F32, kind=KS).ap()
    xn2_d = nc.dram_tensor("xn2_s", [S_TOK, 1024], BF16, kind=KS).ap()
    xbuf_d = nc.dram_tensor("xbuf_s", [NSLOT, 1024], BF16, kind=KS).ap()
    ybuf_d = nc.dram_tensor("ybuf_s", [NSLOT, 1024], F32, kind=KS).ap()

    with ExitStack() as st:
        S = Sched(nc, st)

        def sb(name, shape, dt=F32, stack=st):
            return stack.enter_context(nc.sbuf_tensor("s_" + name, list(shape), dt))

        pb = [st.enter_context(nc.psum_tensor("pb%d" % i, [128, 512], F32)) for i in range(8)]
        pbb = [pb[i][:].bitcast(BF16) for i in range(8)]

        def barrier():
            toks = []
            for e in (PE, ACT, DVE, POOL, SP):
                if S.pending[e]:
                    last = S.ops[e][-1]
                    last.inc, last.incv = S.esem[e], 1
                    S.ecount[e] += 1
                    S.pending[e] = False
                if S.ecount[e] > 0:
                    toks.append(Tok(S.esem[e], S.ecount[e], e))
            for name, s in S.dsems.items():
                toks.append(Tok(s, S.dcount[id(s)], None))
            for e in (PE, ACT, DVE, POOL, SP):
                S.op(e, lambda h: h.nop(), extra=toks, inc=False if e == PE else None)
            S.writer.clear(); S.readers.clear()

        identb = sb("identb", [128, 128], BF16); identf = sb("identf", [128, 128]); onesf = sb("onesf", [128, 128])
        tri = sb("tri", [128, 128]); blk = sb("blk", [128, 128], BF16); rmat = sb("rmat", [128, 128], BF16)
        cmask = sb("cmask", [128, 128], BF16); freq = sb("freq", [128, 1]); invc = sb("invc", [128, 16])
        iota = sb("iota", [128, 32]); bstart = sb("bstart", [128, NBLK])
        qn = sb("qn", [128, 1]); kn = sb("kn", [128, 1]); lamv = sb("lamv", [128, 4, 64]); subln = sb("subln", [128, 128])
        pscale = sb("pscale", [128, 4]); gcol_attn = sb("gcol_attn", [128, 8]); gcol_ple = sb("gcol_ple", [128, 8])
        lam_c = sb("lam_c", [128, 4]); epsc = sb("epsc", [128, 1])
        OHall = sb("OHall", [128, NBLK], F32); ridx_i = sb("ridx_i", [128, NBLK], I32); pidx = sb("pidx", [128, 2], F32)
        S.op(DVE, lambda e: e.memset(epsc[:], EPS), writes=["epsc"])
        for i, (t_, d_) in enumerate([(identb, identb_d), (identf, identf_d), (onesf, onesf_d), (tri, tri_d), (blk, blk_d), (rmat, rmat_d),
                       (cmask, cmask_d), (freq, freq_d), (invc, invc_d), (iota, iota_d), (bstart, bstart_d), (qn, qn_d),
                       (kn, kn_d), (lamv, lamv_d), (subln, subln_d), (pscale, pscale_d), (gcol_attn, gcol_attn_d),
                       (gcol_ple, gcol_ple_d), (pidx, pidx_d)]):
            S.op(SP, lambda e, t_=t_, d_=d_: e.dma_start(out=t_[:], in_=d_), writes=["const"], dma="const")
        lamt = sb("lamt", [128, 2, 64])
        S.op(DVE, lambda e: e.tensor_tensor(out=lamt[:, 0, :], in0=lamv[:, 0, :], in1=lamv[:, 1, :], op=ALU.mult), reads=["const"], writes=["lamt"])
        S.op(DVE, lambda e: e.tensor_tensor(out=lamt[:, 1, :], in0=lamv[:, 2, :], in1=lamv[:, 3, :], op=ALU.mult), reads=["const"], writes=["lamt"])
        S.op(DVE, lambda e: e.tensor_reduce(out=lam_c[:, 0:2], in_=lamt[:], axis=AX.X, op=ALU.add), reads=["lamt"], writes=["lam_c"])
        S.op(ACT, lambda e: e.activation(out=lam_c[:, 0:2], in_=lam_c[:, 0:2], func=AF.Exp), reads=["lam_c"], writes=["lam_c"])
        S.op(DVE, lambda e: e.tensor_tensor(out=lam_c[:, 2:3], in0=lam_c[:, 1:2], in1=lam_c[:, 0:1], op=ALU.subtract), reads=["lam_c"], writes=["lam_c2"])
        S.op(DVE, lambda e: e.tensor_scalar(out=lam_c[:, 3:4], in0=lam_c[:, 2:3], scalar1=-LAMBDA_INIT, scalar2=None, op0=ALU.add), reads=["lam_c2"], writes=["nlam"])
        nlam = lam_c[:, 3:4]
        S.op(DVE, lambda e: e.tensor_scalar(out=subln[:], in0=subln[:], scalar1=1.0 - LAMBDA_INIT, scalar2=None, op0=ALU.mult), reads=["const"], writes=["const"])

        eb_i = sb("eb_i", [128, NBLK], I32); dest_i = sb("dest_i", [128, NT, 4], I32); gk = sb("gk", [128, NT, 4], F32)
        def _precast_gen():
            for e_ in range(32):
                for m_, wsrc in enumerate((wg_d, wu_d, wd_d)):
                    S.op(POOL, lambda e, m_=m_, e_=e_, wsrc=wsrc: e.dma_start(out=wbf_d[m_][e_ * 512:(e_ + 1) * 512, :], in_=wsrc[e_].rearrange("(r two) f -> r (two f)", two=2)),
                         writes=["wbf"], dma="precast")
                    yield
        _pc = _precast_gen()

        def precast(k_):
            for _ in range(k_):
                try:
                    next(_pc)
                except StopIteration:
                    return
        with ExitStack() as stAB:
            qT = sb("qT", [128, 4, S_TOK], BF16, stAB); kT = sb("kT", [128, 4, S_TOK], BF16, stAB)
            Vsb = sb("Vsb", [128, NT, 4, 130], BF16, stAB)
            S.op(POOL, lambda e: e.memset(Vsb[:, :, :, 128:130], 1.0), writes=["Vones"])
            with ExitStack() as stA:
                w_in = sb("w_in", [128, 8, 2048], BF16, stA)
                for c in range(8):
                    S.op(POOL, lambda e, c=c: e.dma_start(out=w_in[:, c, :], in_=w_in_d[c * 128:(c + 1) * 128, :]), writes=[("w_in", c)], dma="w_in")
                for c in range(8):
                    S.op(DVE, lambda e, c=c: e.tensor_scalar(out=w_in[:, c, :], in0=w_in[:, c, :], scalar1=gcol_attn[:, c:c + 1], scalar2=None, op0=ALU.mult),
                         reads=[("w_in", c), "const"], writes=[("w_in", c)])
                wpool = sb("wpool", [128, 4, 128], BF16, stA)
                for g in range(4):
                    S.op(POOL, lambda e, g=g: e.dma_start(out=wpool[:, g, :], in_=w_pool_d[g]), writes=["wpool"], dma="wpool")
                xt = [sb("xt%d" % i, [128, 1024], F32, stA) for i in range(2)]
                xjunk = sb("xjunk", [128, 1024], BF16, stA)
                xn = [sb("xn%d" % i, [128, 1024], BF16, stA) for i in range(2)]
                st1 = sb("st1", [128, 64], F32, stA)
                hnT = [sb("hnT%d" % i, [128, 8, 512], BF16, stA) for i in range(2)]
                posi = sb("posi", [128, 512], I32, stA)
                cosn = sb("cosn", [128, 512], F32, stA); sinn = sb("sinn", [128, 512], F32, stA)
                sq = [sb("sq%d" % i, [128, 512], BF16, stA) for i in range(2)]
                qg = [sb("qg%d" % i, [128, 512], BF16, stA) for i in range(2)]
                rstd_t = sb("rstd_t", [128, 512], F32, stA); t1 = sb("t1", [128, 512], F32, stA); t2 = sb("t2", [128, 512], F32, stA); ang = t1; ang2 = t2
                uT = [sb("uT0", [128, 4, 528], F32, stA)] * 2
                sA = sb("sA", [128, 528], F32, stA); sB = sb("sB", [128, 528], F32, stA)
                pooled = sb("pooled", [128, 4, 512], BF16, stA); catp = [sb("catp0", [128, 4, 512], BF16, stA)] * 2
                S.op(DVE, lambda e: e.memset(st1[:], 0.0), writes=["st1"])
                S.op(POOL, lambda e: e.memset(uT[0][:, :, 0:16], 0.0), writes=[("uT", 0)])
                def front_ops(n):
                    par = n % 2
                    hk = ("hnT", par)
                    for j in range(4):
                        t = n * 4 + j
                        xk = ("xt", t % 2)
                        S.op(SP, lambda e, t=t: e.dma_start(out=xt[t % 2][:], in_=x_d[t * 128:(t + 1) * 128, :]), writes=[xk], dma="xt%d" % (t % 2))
                        S.op(ACT, lambda e, t=t: e.activation(out=xjunk[:], in_=xt[t % 2][:], func=AF.Square, accum_out=st1[:, t:t + 1]),
                             reads=[xk, "st1"], writes=["xjunk", ("ss", t)])
                        S.op(ACT, lambda e, t=t: e.activation(out=st1[:, 32 + t:33 + t], in_=st1[:, t:t + 1], func=AF.Ln, scale=1.0 / 1024, bias=epsc[:, 0:1]),
                             reads=[("ss", t)], writes=[("rs", t)])
                        S.op(ACT, lambda e, t=t: e.activation(out=st1[:, 32 + t:33 + t], in_=st1[:, 32 + t:33 + t], func=AF.Exp, scale=-0.5),
                             reads=[("rs", t)], writes=[("rs", t)])
                        S.op(ACT, lambda e, t=t: e.activation(out=xn[t % 2][:], in_=xt[t % 2][:], func=AF.Copy, scale=st1[:, 32 + t:33 + t]),
                             reads=[xk, ("rs", t)], writes=[("xn", t % 2)])
                        for c in range(8):
                            S.op(PE, lambda e, t=t, c=c: e.transpose(out=pbb[6][:, c * 128:(c + 1) * 128], in_=xn[t % 2][:, c * 128:(c + 1) * 128], identity=identb[:]),
                                 reads=[("xn", t % 2), "const"], writes=["pb6"], inc=(c == 7))
                        S.op(DVE, lambda e, j=j, par=par: e.tensor_copy(out=hnT[par][:, :, j * 128:(j + 1) * 128], in_=pbb[6].rearrange("p (c t) -> p c t", c=8)),
                             reads=["pb6"], writes=[hk])
                S.begin_capture(); front_ops(0); capF = S.end_capture()
                S.replay_interleaved([capF])
                for n in range(NG):
                    par = n % 2
                    hk = ("hnT", par)
                    pass
                    precast(4)
                    S.op(SP, lambda e, n=n: e.dma_start(out=posi[:], in_=pos_d[:, n * 512:(n + 1) * 512]), writes=["posi"], dma="posi")
                    S.op(DVE, lambda e: e.tensor_copy(out=ang[:], in_=posi[:]), reads=["posi"], writes=["t1"])
                    S.op(DVE, lambda e: e.tensor_scalar(out=ang[:], in0=ang[:], scalar1=freq[:, 0:1], scalar2=None, op0=ALU.mult), reads=["t1", "const"], writes=["t1"])
                    S.op(DVE, lambda e: e.tensor_scalar(out=ang2[:], in0=ang[:], scalar1=math.pi / 2, scalar2=None, op0=ALU.add), reads=["t1"], writes=["t2"])
                    for (aa, ak) in ((ang, "t1"), (ang2, "t2")):
                        S.op(DVE, lambda e, aa=aa: e.tensor_scalar(out=rstd_t[:], in0=aa[:], scalar1=1.0 / (2 * math.pi), scalar2=None, op0=ALU.mult), reads=[ak], writes=["rstd_t"])
                        S.op(DVE, lambda e: e.tensor_copy(out=posi[:], in_=rstd_t[:]), reads=["rstd_t"], writes=["posi"])
                        S.op(DVE, lambda e: e.tensor_copy(out=rstd_t[:], in_=posi[:]), reads=["posi"], writes=["rstd_t"])
                        S.op(DVE, lambda e, aa=aa: e.scalar_tensor_tensor(out=aa[:], in0=rstd_t[:], scalar=-2 * math.pi, in1=aa[:], op0=ALU.mult, op1=ALU.add), reads=["rstd_t", ak], writes=[ak])
                        S.op(DVE, lambda e, aa=aa: e.tensor_scalar(out=rstd_t[:], in0=aa[:], scalar1=math.pi, scalar2=-2 * math.pi, op0=ALU.is_gt, op1=ALU.mult), reads=[ak], writes=["rstd_t"])
                        S.op(DVE, lambda e, aa=aa: e.tensor_tensor(out=aa[:], in0=aa[:], in1=rstd_t[:], op=ALU.add), reads=[ak, "rstd_t"], writes=[ak])
                        S.op(DVE, lambda e, aa=aa: e.tensor_scalar(out=rstd_t[:], in0=aa[:], scalar1=-math.pi, scalar2=2 * math.pi, op0=ALU.is_lt, op1=ALU.mult), reads=[ak], writes=["rstd_t"])
                        S.op(DVE, lambda e, aa=aa: e.tensor_tensor(out=aa[:], in0=aa[:], in1=rstd_t[:], op=ALU.add), reads=[ak, "rstd_t"], writes=[ak])
                    S.op(ACT, lambda e: e.activation(out=sinn[:], in_=ang[:], func=AF.Sin), reads=["t1"], writes=["sinn"])
                    S.op(ACT, lambda e: e.activation(out=cosn[:], in_=ang2[:], func=AF.Sin), reads=["t2"], writes=["cosn"])
                    S.begin_capture()
                    for ch in range(8):
                        pa = pb[ch % 2]; pak = "pb%d" % (ch % 2)
                        sp_ = ch % 2
                        for c in range(8):
                            S.op(PE, lambda e, c=c, ch=ch, pa=pa, par=par: e.matmul(pa[:], lhsT=w_in[:, c, ch * 128:(ch + 1) * 128], rhs=hnT[par][:, c, :], start=(c == 0), stop=(c == 7)),
                                 reads=[hk, ("w_in", c)], writes=[pak], inc=(c == 7))
                        S.op(ACT, lambda e, pa=pa, sp_=sp_: e.activation(out=sq[sp_][:], in_=pa[:], func=AF.Square), reads=[pak], writes=[("sq", sp_)])
                        gc = qn if ch < 4 else kn
                        S.op(ACT, lambda e, pa=pa, sp_=sp_, gc=gc: e.activation(out=qg[sp_][:], in_=pa[:], func=AF.Copy, scale=gc[:, 0:1]), reads=[pak, "const"], writes=[("qg", sp_)])
                        pm = pb[2 + (ch % 2)]; pmk = "pb%d" % (2 + ch % 2)
                        pr = pb[4 + (ch % 2)]; prk = "pb%d" % (4 + ch % 2)
                        S.op(PE, lambda e, pm=pm, sp_=sp_: e.matmul(pm[:], lhsT=blk[:], rhs=sq[sp_][:], start=True, stop=True), reads=[("sq", sp_), "const"], writes=[pmk], inc=True)
                        S.op(PE, lambda e, pr=pr, sp_=sp_: e.matmul(pr[:], lhsT=rmat[:], rhs=qg[sp_][:], start=True, stop=True), reads=[("qg", sp_), "const"], writes=[prk], inc=True)
                        S.op(ACT, lambda e, pm=pm: e.activation(out=rstd_t[:], in_=pm[:], func=AF.Ln, bias=epsc[:, 0:1]), reads=[pmk], writes=["rstd_t"])
                        S.op(ACT, lambda e: e.activation(out=rstd_t[:], in_=rstd_t[:], func=AF.Exp, scale=-0.5), reads=["rstd_t"], writes=["rstd_t"])
                        S.op(POOL, lambda e, sp_=sp_: e.tensor_tensor(out=t1[:], in0=qg[sp_][:], in1=cosn[:], op=ALU.mult), reads=[("qg", sp_), "cosn"], writes=["t1"])
                        S.op(DVE, lambda e, pr=pr: e.tensor_tensor(out=t2[:], in0=pr[:], in1=sinn[:], op=ALU.mult), reads=[prk, "sinn"], writes=["t2"])
                        S.op(DVE, lambda e: e.tensor_tensor(out=t1[:], in0=t1[:], in1=t2[:], op=ALU.add), reads=["t1", "t2"], writes=["t1"])
                        dst = (qT if ch < 4 else kT)
                        S.op(DVE, lambda e, dst=dst, ch=ch, n=n: e.tensor_tensor(out=dst[:, ch % 4, n * 512:(n + 1) * 512], in0=t1[:], in1=rstd_t[:], op=ALU.mult),
                             reads=["t1", "rstd_t"], writes=[("qk", ch, n)])
                    capA = S.end_capture(); S.begin_capture()
                    for j in range(4):
                        t = n * 4 + j
                        for c in range(8):
                            S.op(PE, lambda e, c=c, j=j, par=par: e.matmul(pb[6][:], lhsT=hnT[par][:, c, j * 128:(j + 1) * 128], rhs=w_in[:, c, 1024:1536], start=(c == 0), stop=(c == 7)),
                                 reads=[hk, ("w_in", c)], writes=["pb6"], inc=(c == 7))
                        S.op(ACT, lambda e, t=t: e.activation(out=Vsb[:, t, :, 0:128], in_=pb[6][:].rearrange("p (h v) -> p h v", h=4), func=AF.Copy),
                             reads=["pb6"], writes=[("V", t)])
                    capB = S.end_capture(); S.begin_capture()
                    uk = ("uT", 0)
                    if n > 0:
                        S.op(POOL, lambda e, par=par: e.tensor_copy(out=uT[par][:, :, 0:16], in_=uT[1 - par][:, :, 512:528]), reads=[("uT", 0)], writes=[uk])
                    for g in range(4):
                        for c in range(8):
                            S.op(PE, lambda e, c=c, g=g, par=par: e.matmul(pb[7][:], lhsT=w_in[:, c, 1536 + g * 128:1536 + (g + 1) * 128], rhs=hnT[par][:, c, :], start=(c == 0), stop=(c == 7)),
                                 reads=[hk, ("w_in", c)], writes=["pb7"], inc=(c == 7))
                        S.op(ACT, lambda e, g=g, par=par: e.activation(out=uT[par][:, g, 16:528], in_=pb[7][:], func=AF.Copy), reads=["pb7"], writes=[uk])
                        src = uT[par][:, g, :]
                        bufs = [sA, sB]
                        cur, curk = src, uk
                        for jj in range(g + 1):
                            sh = 1 << jj
                            lo = (1 << (jj + 1)) - 1
                            o = bufs[jj % 2]; ok_ = "sA" if jj % 2 == 0 else "sB"
                            S.op(POOL, lambda e, o=o, cur=cur, lo=lo, sh=sh: e.tensor_tensor(out=o[:, lo:528], in0=cur[:, lo:528], in1=cur[:, lo - sh:528 - sh], op=ALU.add),
                                 reads=[curk], writes=[ok_])
                            cur, curk = o, ok_
                        w = 1 << (g + 1)
                        S.op(DVE, lambda e, g=g, cur=cur, src=src, w=w: e.scalar_tensor_tensor(out=pooled[:, g, :], in0=cur[:, 16:528], scalar=1.0 / w, in1=src[:, 16:528], op0=ALU.mult, op1=ALU.subtract),
                             reads=[curk, uk], writes=["pooled"])
                        if n == 0:
                            S.op(DVE, lambda e, cur=cur, w=w: e.tensor_tensor(out=cur[:, 16:16 + w - 1], in0=cur[:, 16:16 + w - 1], in1=invc[:, 0:w - 1], op=ALU.mult),
                                 reads=[curk, "const"], writes=[curk])
                            S.op(DVE, lambda e, g=g, cur=cur, src=src, w=w: e.tensor_tensor(out=pooled[:, g, 0:w - 1], in0=cur[:, 16:16 + w - 1], in1=src[:, 16:16 + w - 1], op=ALU.subtract),
                                 reads=[curk, uk], writes=["pooled"])
                        S.op(PE, lambda e, g=g: e.matmul(pb[7][:], lhsT=wpool[:, g, :], rhs=pooled[:, g, :], start=True, stop=True), reads=["pooled", "wpool"], writes=["pb7"], inc=True)
                        S.op(ACT, lambda e, g=g, par=par: e.activation(out=catp[par][:, g, :], in_=pb[7][:], func=AF.Copy, scale=pscale[:, g:g + 1]), reads=["pb7", "const"], writes=[("catp", 0)])
                    for g in range(4):
                        S.op(POOL, lambda e, g=g, n=n, par=par: e.dma_start(out=catp_d[g, :, n * 512:(n + 1) * 512], in_=catp[par][:, g, :]), reads=[("catp", 0)], writes=["catp_d"], dma="catp_st")
                    capC = S.end_capture()
                    capF = []
                    if n + 1 < NG:
                        S.begin_capture(); front_ops(n + 1); capF = S.end_capture()
                    S.replay_interleaved([capA, capB + capF, capC])

            barrier()
            if DEBUG and _os.environ.get("KEARLY", "1") == "1":
                S.op(SP, lambda e: e.dma_start(out=dbg_q, in_=qT[:]), dma="dbg")
                S.op(SP, lambda e: e.dma_start(out=dbg_k, in_=kT[:]), dma="dbg")
                S.op(SP, lambda e: e.dma_start(out=dbg_v, in_=Vsb[:]), dma="dbg")
            with ExitStack() as stB:
                catA = sb("catA", [128, 4, S_TOK], BF16, stB)
                PT = [[sb("PT%d_%d" % (i, c), [128, 512], BF16, stB) for c in range(2)] for i in range(2)]
                otmp = sb("otmp", [128, 128], F32, stB); ojunk = sb("ojunk", [128, 128], BF16, stB)
                ob = sb("ob", [128, 4, 128], BF16, stB); st2 = sb("st2", [128, 8], F32, stB)

                def Oap(i, lo, hi):
                    return pb[4 + i // 3][:, (i % 3) * 160 + lo:(i % 3) * 160 + hi]
                osb = [sb("osb%d" % i, [128, 512], F32, stB) for i in range(3)]

                def Osb(i, lo, hi):
                    return osb[i // 3][:, (i % 3) * 160 + lo:(i % 3) * 160 + hi]

                def SE(h, n, kt):
                    bufi = kt % 2
                    j = kt - 4 * n
                    qlo = max(j, 0) * 128
                    for c in range(2):
                        ps = pb[bufi * 2 + c]; psk = "pb%d" % (bufi * 2 + c)
                        S.op(PE, lambda e, ps=ps, c=c: e.matmul(ps[:, qlo:512], lhsT=kT[c * 64:(c + 1) * 64, h, kt * 128:(kt + 1) * 128],
                             rhs=qT[c * 64:(c + 1) * 64, h, n * 512 + qlo:(n + 1) * 512], start=True, stop=True), writes=[psk], inc=True)
                        S.op(ACT, lambda e, ps=ps, c=c: e.activation(out=PT[bufi][c][:, qlo:512], in_=ps[:, qlo:512], func=AF.Exp, scale=0.125),
                             reads=[psk], writes=[("PT", bufi, c)])
                        if j >= 0:
                            S.op(POOL, lambda e, c=c: e.tensor_tensor(out=PT[bufi][c][:, qlo:qlo + 128], in0=PT[bufi][c][:, qlo:qlo + 128], in1=cmask[:], op=ALU.mult),
                                 reads=[("PT", bufi, c)], writes=[("PT", bufi, c)])

                def AVm(h, n, kt):
                    bufi = kt % 2
                    j = kt - 4 * n
                    for qi in range(max(j, 0), 4):
                        for c in range(2):
                            i = qi * 2 + c
                            S.op(PE, lambda e, i=i, c=c, qi=qi: e.matmul(Oap(i, 0, 129), lhsT=PT[bufi][c][:, qi * 128:(qi + 1) * 128],
                                 rhs=Vsb[:, kt, h, 0:129], start=(kt == 0 and i % 3 == 0), stop=((i, kt - 4 * n) in ((2, 1), (5, 2), (7, 3)))), reads=[("PT", bufi, c)], writes=[("O", i // 3)],
                                 inc=(qi == 3 and c == 1))

                def post(h, n):
                    S.op(ACT, lambda e: e.activation(out=osb[0][:], in_=pb[4][:], func=AF.Copy), reads=[("O", 0)], writes=[("osb", 0)])
                    S.op(DVE, lambda e: e.tensor_copy(out=osb[1][:], in_=pb[5][:]), reads=[("O", 1)], writes=[("osb", 1)])
                    S.op(ACT, lambda e: e.activation(out=osb[2][:, 0:320], in_=pb[6][:, 0:320], func=AF.Copy), reads=[("O", 2)], writes=[("osb", 2)])
                    for qi in range(4):
                        i1, i2 = qi * 2, qi * 2 + 1
                        k1, k2 = ("osb", i1 // 3), ("osb", i2 // 3)
                        S.op(DVE, lambda e, i1=i1: e.reciprocal(out=st2[:, 0:1], in_=Osb(i1, 128, 129)), reads=[k1], writes=["st2a"])
                        S.op(DVE, lambda e, i2=i2: e.reciprocal(out=st2[:, 1:2], in_=Osb(i2, 128, 129)), reads=[k2], writes=["st2b"])
                        S.op(DVE, lambda e: e.tensor_tensor(out=st2[:, 1:2], in0=st2[:, 1:2], in1=nlam, op=ALU.mult), reads=["st2b"], writes=["st2b"])
                        S.op(DVE, lambda e, i1=i1: e.tensor_scalar(out=otmp[:], in0=Osb(i1, 0, 128), scalar1=st2[:, 0:1], scalar2=None, op0=ALU.mult), reads=[k1, "st2a"], writes=["otmp"])
                        S.op(DVE, lambda e, i2=i2: e.scalar_tensor_tensor(out=otmp[:], in0=Osb(i2, 0, 128), scalar=st2[:, 1:2], in1=otmp[:], op0=ALU.mult, op1=ALU.add),
                             reads=[k2, "st2b", "otmp"], writes=["otmp"])
                        S.op(DVE, lambda e: e.memset(st2[:, 2:3], 0.0), writes=["st2c"])
                        S.op(ACT, lambda e: e.activation(out=ojunk[:], in_=otmp[:], func=AF.Square, accum_out=st2[:, 2:3]), reads=["otmp", "st2c"], writes=["ojunk", "st2c"])
                        S.op(ACT, lambda e: e.activation(out=st2[:, 3:4], in_=st2[:, 2:3], func=AF.Ln, scale=1.0 / 128, bias=epsc[:, 0:1]), reads=["st2c"], writes=["st2d"])
                        S.op(ACT, lambda e: e.activation(out=st2[:, 3:4], in_=st2[:, 3:4], func=AF.Exp, scale=-0.5), reads=["st2d"], writes=["st2d"])
                        S.op(DVE, lambda e, qi=qi: e.scalar_tensor_tensor(out=ob[:, qi, :], in0=otmp[:], scalar=st2[:, 3:4], in1=subln[:], op0=ALU.mult, op1=ALU.mult),
                             reads=["otmp", "st2d"], writes=["ob"])

                def fin(h, n):
                    for qi in range(4):
                        S.op(PE, lambda e, qi=qi: e.transpose(out=pbb[7][:, qi * 128:(qi + 1) * 128], in_=ob[:, qi, :], identity=identb[:]), reads=["ob"], writes=["pb7"], inc=(qi == 3))
                    S.op(DVE, lambda e: e.tensor_copy(out=catA[:, h, n * 512:(n + 1) * 512], in_=pbb[7][:, 0:512]), reads=["pb7"], writes=[("catA", h)])

                heads = ([int(c) for c in _os.environ.get('KHEADS', '0123')] if stage >= 2 else [])
                groups = [(h, n) for h in heads for n in range(NG)]
                pending_fin = None
                for gi, (h, n) in enumerate(groups):
                    nkt = 4 * n + 4
                    precast(2)
                    SE(h, n, 0)
                    for kt in range(nkt):
                        if kt + 1 < nkt:
                            SE(h, n, kt + 1)
                        AVm(h, n, kt)
                        if kt == min(2, nkt - 1) and pending_fin is not None:
                            fin(*pending_fin)
                            ph = pending_fin[0]
                            if pending_fin[1] == NG - 1:
                                S.op(SP, lambda e, ph=ph: e.dma_start(out=cata_d[:, ph, :], in_=catA[:, ph, :]), reads=[("catA", ph)], writes=["cata_d"], dma="cata_st")
                            pending_fin = None
                    post(h, n)
                    pending_fin = (h, n)
                if pending_fin is not None:
                    fin(*pending_fin)
                    ph = pending_fin[0]
                    S.op(SP, lambda e, ph=ph: e.dma_start(out=cata_d[:, ph, :], in_=catA[:, ph, :]), reads=[("catA", ph)], writes=["cata_d"], dma="cata_st")
            if DEBUG:
                barrier()
                S.op(SP, lambda e: e.dma_start(out=dbg_v2, in_=Vsb[:]), dma="dbg")
        barrier()
        with ExitStack() as stC:
            precast(96)
            w_out = sb("w_out", [128, 8, 1024], BF16, stC)
            for c in range(8):
                S.op(POOL, lambda e, c=c: e.dma_start(out=w_out[:, c, :], in_=w_out_d[c * 128:(c + 1) * 128, :]), writes=["w_out"], dma="w_out")
            catP = sb("catP", [128, 4, S_TOK], BF16, stC); catA2 = sb("catA2", [128, 4, S_TOK], BF16, stC)
            for g in range(4):
                S.op(SP, lambda e, g=g: e.dma_start(out=catA2[:, g, :], in_=cata_d[:, g, :]), writes=["catP"], dma="catP")
            for g in range(4):
                S.op(SP, lambda e, g=g: e.dma_start(out=catP[:, g, :], in_=catp_d[g]), writes=["catP"], dma="catP")
            ffn_b = sb("ffn_b", [128, 1024], F32, stC); w_r = sb("w_r", [128, 8, 32], F32, stC); b_r = sb("b_r", [128, 32], F32, stC)
            S.op(SP, lambda e: e.dma_start(out=ffn_b[:], in_=ffn_b_d), writes=["cC"], dma="cC")
            S.op(SP, lambda e: e.dma_start(out=w_r[:], in_=w_r_d), writes=["cC"], dma="cC")
            S.op(SP, lambda e: e.dma_start(out=b_r[:], in_=b_r_d), writes=["cC"], dma="cC")
            xt2 = [sb("xt2_%d" % i, [128, 1024], F32, stC) for i in range(2)]
            h1t = [sb("h1t%d" % i, [128, 1024], F32, stC) for i in range(2)]
            xn2f = sb("xn2f", [128, 1024], F32, stC); xn2b = [sb("xn2b%d" % i, [128, 1024], BF16, stC) for i in range(2)]
            cjunk = sb("cjunk", [128, 1024], BF16, stC)
            xn2T = sb("xn2T", [128, 1024], F32, stC)
            st3 = sb("st3", [128, 64], F32, stC)
            lgall = sb("lgall", [128, NT, 32], F32, stC); top8 = sb("top8", [128, NT, 8], F32, stC); idx8 = sb("idx8", [128, NT, 8], U32, stC)
            maskall = sb("maskall", [128, NT, 32], F32, stC); Gall = sb("Gall", [128, NT, 32], F32, stC)
            S.op(DVE, lambda e: e.memset(st3[:], 0.0), writes=["st3"])
            NTC = NT if stage >= 3 else 0
            for t in range(NTC):
                xk = ("xt2", t % 2); hk2 = ("h1t", t % 2)
                S.op(SP, lambda e, t=t: e.dma_start(out=xt2[t % 2][:], in_=x_d[t * 128:(t + 1) * 128, :]), writes=[xk], dma="xt2_%d" % (t % 2))
                for hf in range(2):
                    for c in range(8):
                        cat_c = catA2[:, c, t * 128:(t + 1) * 128] if c < 4 else catP[:, c - 4, t * 128:(t + 1) * 128]
                        S.op(PE, lambda e, cat_c=cat_c, c=c, hf=hf: e.matmul(pb[hf][:], lhsT=cat_c, rhs=w_out[:, c, hf * 512:(hf + 1) * 512], start=(c == 0), stop=(c == 7)),
                             reads=["w_out", "catP"], writes=["pb%d" % hf], inc=(c == 7))
                    S.op(DVE, lambda e, t=t, hf=hf: e.tensor_tensor(out=h1t[t % 2][:, hf * 512:(hf + 1) * 512], in0=pb[hf][:], in1=xt2[t % 2][:, hf * 512:(hf + 1) * 512], op=ALU.add),
                         reads=["pb%d" % hf, xk], writes=[hk2])
                S.op(POOL, lambda e, t=t: e.dma_start(out=h1_d[t * 128:(t + 1) * 128, :], in_=h1t[t % 2][:]), reads=[hk2], writes=["h1_d"], dma="h1st%d" % (t % 2))
                S.op(ACT, lambda e, t=t: e.activation(out=cjunk[:], in_=h1t[t % 2][:], func=AF.Square, accum_out=st3[:, t:t + 1]), reads=[hk2, "st3"], writes=["cjunk", ("ss3", t)])
                S.op(ACT, lambda e, t=t: e.activation(out=st3[:, 32 + t:33 + t], in_=st3[:, t:t + 1], func=AF.Ln, scale=1.0 / 1024, bias=epsc[:, 0:1]), reads=[("ss3", t)], writes=[("rs3", t)])
                S.op(ACT, lambda e, t=t: e.activation(out=st3[:, 32 + t:33 + t], in_=st3[:, 32 + t:33 + t], func=AF.Exp, scale=-0.5), reads=[("rs3", t)], writes=[("rs3", t)])
                S.op(DVE, lambda e, t=t: e.scalar_tensor_tensor(out=xn2f[:], in0=h1t[t % 2][:], scalar=st3[:, 32 + t:33 + t], in1=ffn_b[:], op0=ALU.mult, op1=ALU.mult),
                     reads=[hk2, ("rs3", t), "cC"], writes=["xn2f"])
                S.op(ACT, lambda e, t=t: e.activation(out=xn2b[t % 2][:].rearrange("s (c p) -> s c p", p=128), in_=xn2f[:].rearrange("s (p c) -> s c p", c=8), func=AF.Copy), reads=["xn2f"], writes=[("xn2b", t % 2)])
                S.op(POOL, lambda e, t=t: e.dma_start(out=xn2_d[t * 128:(t + 1) * 128, :], in_=xn2b[t % 2][:]), reads=[("xn2b", t % 2)], writes=["xn2_d"], dma="xn2st%d" % (t % 2))
                for c in range(8):
                    bank = 2 + c // 4
                    S.op(PE, lambda e, c=c, bank=bank: e.transpose(out=pb[bank][:, (c % 4) * 128:(c % 4 + 1) * 128], in_=xn2f[:, c * 128:(c + 1) * 128], identity=identf[:]),
                         reads=["xn2f"], writes=["pb%d" % bank], inc=(c % 4 == 3))
                S.op(ACT, lambda e: e.activation(out=xn2T[:, 0:512], in_=pb[2][:], func=AF.Copy), reads=["pb2"], writes=["xn2Ta"])
                S.op(DVE, lambda e: e.tensor_copy(out=xn2T[:, 512:1024], in_=pb[3][:]), reads=["pb3"], writes=["xn2Tb"])
                for c in range(8):
                    S.op(PE, lambda e, c=c: e.matmul(pb[4][:, 0:32], lhsT=xn2T[:, c * 128:(c + 1) * 128], rhs=w_r[:, c, :], start=(c == 0), stop=(c == 7)),
                         reads=["xn2Ta", "xn2Tb", "cC"], writes=["pb4"], inc=(c == 7))
                S.op(DVE, lambda e, t=t: e.tensor_tensor(out=lgall[:, t, :], in0=pb[4][:, 0:32], in1=b_r[:], op=ALU.add), reads=["pb4", "cC"], writes=[("lg", t)])
                S.op(DVE, lambda e, t=t: e.max(out=top8[:, t, :], in_=lgall[:, t, :]), reads=[("lg", t)], writes=[("top8", t)])
                S.op(DVE, lambda e, t=t: e.max_index(out=idx8[:, t, :], in_max=top8[:, t, :], in_values=lgall[:, t, :]), reads=[("lg", t), ("top8", t)], writes=[("idx8", t)])
            if stage >= 3:
                dispatch(nc, S, sb, stC, pb, lgall, top8, idx8, maskall, Gall, onesf, tri, iota, bstart, eb_i, dest_i, gk, xn2_d, xbuf_d, pidx, OHall, ridx_i)
        barrier()
        if stage >= 4:
            moe_blocks(nc, S, sb, st, pb, pbb, OHall, ridx_i, wbf_d, btab_d, xbuf_d, ybuf_d, identb)
            barrier()
        if stage >= 5:
            ple_phase(nc, S, sb, st, pb, pbb, dest_i, gk, h1_d, ybuf_d, p_d, w_pg_d, w_pp_d, post_b_d, gcol_ple, identb, out_d, epsc)
        fin = [Tok(s, S.dcount[id(s)], None) for s in S.dsems.values()]
        S.finish(fin)
    return nc

_NC_CACHE = {}


def _consts():
    bf = ml_dtypes.bfloat16
    c = {}
    c["ident_bf"] = np.eye(128, dtype=np.float32).astype(bf)
    c["ident_f"] = np.eye(128, dtype=np.float32)
    c["ones_f"] = np.ones((128, 128), np.float32)
    k = np.arange(128)
    c["tri_strict"] = (k[:, None] < k[None, :]).astype(np.float32)
    c["cmask"] = (k[:, None] <= k[None, :]).astype(np.float32).astype(bf)
    blk = np.zeros((128, 128), np.float32)
    blk[:64, :64] = 1.0 / 64; blk[64:, 64:] = 1.0 / 64
    c["blkdiag"] = blk.astype(bf)
    r = np.zeros((128, 128), np.float32)
    for base in (0, 64):
        for m in range(8):
            r[base + m + 8, base + m] = -1.0
            r[base + m, base + m + 8] = 1.0
    c["rmat"] = r.astype(bf)
    freqs = (np.float32(500000.0) ** (-np.arange(0, 16, 2, dtype=np.float32) / np.float32(16))).astype(np.float32)
    f = np.zeros((128, 1), np.float32)
    for base in (0, 64):
        for i in range(8):
            f[base + i, 0] = freqs[i]; f[base + 8 + i, 0] = freqs[i]
    c["freq_col"] = f
    c["invcnt"] = np.broadcast_to((1.0 / np.arange(1, 17, dtype=np.float32))[None, :], (128, 16)).copy()
    c["iota_row"] = np.broadcast_to(np.arange(32, dtype=np.float32)[None, :], (128, 32)).copy()
    c["pidx_col"] = np.stack([np.arange(128, dtype=np.float32), (np.arange(128) % 32).astype(np.float32)], axis=1)
    c["bstart_row"] = np.broadcast_to((128.0 * np.arange(NBLK, dtype=np.float32))[None, :], (128, NBLK)).copy()
    return c


def _rep(v, n=128):
    return np.ascontiguousarray(np.broadcast_to(np.asarray(v)[None, ...], (n,) + tuple(np.asarray(v).shape)))


def make_in_maps(inputs):
    f32 = np.float32
    g = {k: np.asarray(v) for k, v in inputs.items()}
    shared = dict(_consts())
    shared["w_in"] = np.ascontiguousarray(g["w_in"][0], f32)
    shared["gcol_attn"] = np.ascontiguousarray(g["attn_norm"][0].reshape(8, 128).T)
    shared["qn_col"] = np.ascontiguousarray(np.tile(g["q_norm"][0], 2).reshape(128, 1))
    shared["kn_col"] = np.ascontiguousarray(np.tile(g["k_norm"][0], 2).reshape(128, 1))
    shared["lamv"] = _rep(np.stack([g["lam_q1"][0], g["lam_k1"][0], g["lam_q2"][0], g["lam_k2"][0]], 0))
    shared["subln_b"] = _rep(g["subln"][0])
    shared["w_pool"] = np.ascontiguousarray(g["w_pool"][0])
    shared["pscale_col"] = np.ascontiguousarray(g["pool_scale"][0].reshape(4, 128).T)
    shared["w_out"] = np.ascontiguousarray(g["w_out"][0])
    shared["ffn_b"] = _rep(g["ffn_norm"][0])
    shared["w_router"] = np.ascontiguousarray(g["w_router"][0].reshape(8, 128, 32).transpose(1, 0, 2))
    shared["b_router_b"] = _rep(g["b_router"][0])
    shared["w_gate"] = np.ascontiguousarray(g["w_gate"][0]); shared["w_up"] = np.ascontiguousarray(g["w_up"][0]); shared["w_down"] = np.ascontiguousarray(g["w_down"][0])
    shared["btab"] = np.ascontiguousarray(np.concatenate([g["b_gate"][0], g["b_up"][0], g["b_down"][0]], axis=1))
    shared["gcol_ple"] = np.ascontiguousarray(g["ple_gate_norm"][0].reshape(8, 128).T)
    shared["w_ple_gate"] = np.ascontiguousarray(g["w_ple_gate"][0]); shared["w_ple_proj"] = np.ascontiguousarray(g["w_ple_proj"][0])
    shared["post_b"] = _rep(g["ple_post_norm"][0])
    maps = []
    for b in range(8):
        m = dict(shared)
        m["x"] = np.ascontiguousarray(g["x"][b]); m["p"] = np.ascontiguousarray(g["p"][0, b])
        m["posb"] = _rep(g["positions"][b].astype(np.int32))
        maps.append(m)
    return maps


def kernel(**inputs):
    if "nc" not in _NC_CACHE:
        _NC_CACHE["nc"] = build()
    nc = _NC_CACHE["nc"]
    maps = make_in_maps(inputs)
    res = run_bass_kernel_spmd(nc, maps, core_ids=list(range(8)))
    return np.stack([np.asarray(r["out"]) for r in res.results], axis=0).astype(np.float32)
```

```python
import numpy as np
import concourse.bass as bass
import concourse.mybir as mybir

F32 = mybir.dt.float32
BF16 = mybir.dt.bfloat16
I32 = mybir.dt.int32
U32 = mybir.dt.uint32
ALU = mybir.AluOpType
AF = mybir.ActivationFunctionType
AX = mybir.AxisListType

PE, ACT, DVE, POOL, SP = "tensor", "scalar", "vector", "gpsimd", "sync"


class Tok:
    __slots__ = ("sem", "val", "eng")

    def __init__(self, sem, val, eng):
        self.sem, self.val, self.eng = sem, val, eng


class Rec:
    __slots__ = ("fn", "waits", "inc", "incv")

    def __init__(self, fn):
        self.fn, self.waits, self.inc, self.incv = fn, [], None, 0


class Sched:
    def __init__(self, nc, stack, n_dma_sems=96):
        self.nc = nc
        self.stack = stack
        self.ops = {e: [] for e in (PE, ACT, DVE, POOL, SP)}
        self.esem = {e: stack.enter_context(nc.semaphore("s_" + e)) for e in self.ops}
        self.ecount = {e: 0 for e in self.ops}
        self.pending = {e: False for e in self.ops}
        self.waited = {e: {} for e in self.ops}
        self.dsems = {}
        self.dcount = {}
        self.free_dsems = [stack.enter_context(nc.semaphore("d%d" % i)) for i in range(n_dma_sems)]
        self.writer = {}
        self.readers = {}

    def _need(self, eng, tok, same_ok):
        if tok is None:
            return None
        if tok.eng is not None:
            if self.ecount[tok.eng] < tok.val:
                last = self.ops[tok.eng][-1]
                assert last.inc is None
                last.inc, last.incv = self.esem[tok.eng], 1
                self.ecount[tok.eng] += 1
                self.pending[tok.eng] = False
                assert self.ecount[tok.eng] == tok.val
            val = tok.val
        else:
            val = self.dcount[id(tok.sem)]
        w = self.waited[eng]
        if w.get(id(tok.sem), 0) >= val:
            return None
        w[id(tok.sem)] = val
        return (tok.sem, val)

    _cap = None

    def begin_capture(self):
        self._cap = []

    def end_capture(self):
        c, self._cap = self._cap, None
        return c

    def replay_interleaved(self, lists):
        lists = [x for x in lists if x]
        if not lists:
            return
        L = max(len(x) for x in lists)
        for i in range(L):
            for x in lists:
                for j in range((i * len(x)) // L, ((i + 1) * len(x)) // L):
                    self.op(*x[j])

    def op(self, eng, fn, reads=(), writes=(), inc=None, dma=None, extra=()):
        if self._cap is not None:
            self._cap.append((eng, fn, tuple(reads), tuple(writes), inc, dma, tuple(extra)))
            return None
        raw, other = [], []
        for k in reads:
            t = self.writer.get(k)
            if t is not None:
                raw.append(t)
        for k in writes:
            t = self.writer.get(k)
            if t is not None:
                other.append(t)
            other.extend(self.readers.get(k, ()))
        raw.extend(extra)
        rec = Rec(fn)
        is_dma = dma is not None
        for t in raw:
            if t.eng == eng and not is_dma and eng == PE:
                continue
            wt = self._need(eng, t, False)
            if wt:
                rec.waits.append(wt)
        for t in other:
            if t.eng == eng and not is_dma and eng == PE:
                continue
            if is_dma and t.eng is None and dma in self.dsems and t.sem is self.dsems[dma]:
                continue
            wt = self._need(eng, t, False)
            if wt:
                rec.waits.append(wt)
        self.ops[eng].append(rec)
        if is_dma:
            if dma not in self.dsems:
                self.dsems[dma] = self.free_dsems.pop()
                self.dcount[id(self.dsems[dma])] = 0
            s = self.dsems[dma]
            self.dcount[id(s)] += 16
            rec.inc, rec.incv = s, 16
            tok = Tok(s, self.dcount[id(s)], None)
        else:
            if inc is None:
                inc = eng != PE
            if inc:
                self.ecount[eng] += 1
                rec.inc, rec.incv = self.esem[eng], 1
                self.pending[eng] = False
                tok = Tok(self.esem[eng], self.ecount[eng], eng)
            else:
                self.pending[eng] = True
                tok = Tok(self.esem[eng], self.ecount[eng] + 1, eng)
        for k in reads:
            self.readers.setdefault(k, []).append(tok)
        for k in writes:
            self.writer[k] = tok
            self.readers[k] = []
        return tok

    def finish(self, final_toks):
        rec = Rec(lambda e: e.nop())
        for t in final_toks:
            wt = self._need(SP, t, False)
            if wt:
                rec.waits.append(wt)
        self.ops[SP].append(rec)
        nc = self.nc
        with nc.Block() as block:
            def emit(name):
                def run(e):
                    for r in self.ops[name]:
                        for (s, v) in r.waits:
                            e.wait_ge(s, v)
                        ins = r.fn(e)
                        if r.inc is not None:
                            ins.then_inc(r.inc, r.incv)
                return run
            block.tensor(emit(PE))
            block.scalar(emit(ACT))
            block.vector(emit(DVE))
            block.gpsimd(emit(POOL))
            block.sync(emit(SP))
def dispatch(nc, S, sb, stC, pb, lgall, top8, idx8, maskall, Gall, onesf, tri, iota, bstart, eb_i, dest_i, gk, xn2_d, xbuf_d, pidx, OHall, ridx_i):
    NT, NBLK = 32, 160
    allk = [("lg", t) for t in range(NT)]
    exl = sb("exl", [128, NT, 32], F32, stC); sums = sb("sums", [128, NT], F32, stC)
    negmax = sb("negmax", [128, NT, 1], F32, stC)
    S.op(DVE, lambda e: e.tensor_tensor(out=maskall[:], in0=lgall[:], in1=top8[:, :, 3:4].to_broadcast([128, NT, 32]), op=ALU.is_ge),
         reads=allk + [("top8", t) for t in range(NT)], writes=["maskall"])
    S.op(DVE, lambda e: e.tensor_tensor(out=exl[:], in0=lgall[:], in1=top8[:, :, 0:1].to_broadcast([128, NT, 32]), op=ALU.subtract),
         reads=allk + [("top8", t) for t in range(NT)], writes=["exl"])
    S.op(ACT, lambda e: e.activation(out=exl[:], in_=exl[:], func=AF.Exp), reads=["exl"], writes=["exl"])
    S.op(DVE, lambda e: e.tensor_tensor(out=exl[:], in0=exl[:], in1=maskall[:], op=ALU.mult), reads=["exl", "maskall"], writes=["exl"])
    S.op(DVE, lambda e: e.tensor_reduce(out=sums[:], in_=exl[:], axis=AX.X, op=ALU.add), reads=["exl"], writes=["sums"])
    S.op(DVE, lambda e: e.reciprocal(out=sums[:], in_=sums[:]), reads=["sums"], writes=["sums"])
    S.op(DVE, lambda e: e.tensor_tensor(out=Gall[:], in0=exl[:], in1=sums[:].unsqueeze(2).to_broadcast([128, NT, 32]), op=ALU.mult), reads=["exl", "sums"], writes=["Gall"])
    mflat = maskall[:].rearrange("p t e -> p (t e)")
    S.op(PE, lambda e: e.matmul(pb[5][:], lhsT=onesf[:], rhs=mflat[:, 0:512], start=True, stop=True), reads=["maskall"], writes=["pb5"], inc=True)
    S.op(PE, lambda e: e.matmul(pb[6][:], lhsT=onesf[:], rhs=mflat[:, 512:1024], start=True, stop=True), reads=["maskall"], writes=["pb6"], inc=True)
    csA = sb("csA", [128, 48, 32], F32, stC); csB = sb("csB", [128, 48, 32], F32, stC); cs0 = sb("cs0", [128, NT, 32], F32, stC)
    S.op(DVE, lambda e: e.memset(csA[:, 0:16, :], 0.0), writes=["csA"])
    S.op(DVE, lambda e: e.memset(csB[:, 0:16, :], 0.0), writes=["csB"])
    S.op(DVE, lambda e: e.tensor_copy(out=csA[:, 16:32, :], in_=pb[5][:].rearrange("p (t e) -> p t e", e=32)), reads=["pb5"], writes=["csA"])
    S.op(DVE, lambda e: e.tensor_copy(out=csA[:, 32:48, :], in_=pb[6][:].rearrange("p (t e) -> p t e", e=32)), reads=["pb6"], writes=["csA"])
    S.op(DVE, lambda e: e.tensor_copy(out=cs0[:], in_=csA[:, 16:48, :]), reads=["csA"], writes=["cs0"])
    cur, curk, oth, othk = csA, "csA", csB, "csB"
    for j in range(5):
        sh = 1 << j
        S.op(DVE, lambda e, cur=cur, oth=oth, sh=sh: e.tensor_tensor(out=oth[:, 16:48, :], in0=cur[:, 16:48, :], in1=cur[:, 16 - sh:48 - sh, :], op=ALU.add), reads=[curk], writes=[othk])
        cur, curk, oth, othk = oth, othk, cur, curk
    incl = cur; inclk = curk
    cnt = sb("cnt", [128, 32], F32, stC); padd = sb("padd", [128, 32], F32, stC)
    scA = sb("scA", [128, 64], F32, stC); scB = sb("scB", [128, 64], F32, stC); pstart = sb("pstart", [128, 32], F32, stC)
    S.op(DVE, lambda e: e.tensor_copy(out=cnt[:], in_=incl[:, 47, :]), reads=[inclk], writes=["cnt"])
    cmpc = sb("cmpc", [128, 32, 32], F32, stC)
    S.op(DVE, lambda e: e.tensor_tensor(out=cmpc[:], in0=cnt[:].unsqueeze(2).to_broadcast([128, 32, 32]), in1=bstart[:, 0:32].unsqueeze(1).to_broadcast([128, 32, 32]), op=ALU.is_gt),
         reads=["cnt"], writes=["cmpc"])
    S.op(DVE, lambda e: e.tensor_reduce(out=padd[:], in_=cmpc[:], axis=AX.X, op=ALU.add), reads=["cmpc"], writes=["padd"])
    S.op(DVE, lambda e: e.tensor_scalar(out=padd[:], in0=padd[:], scalar1=128.0, scalar2=None, op0=ALU.mult), reads=["padd"], writes=["padd"])
    S.op(DVE, lambda e: e.memset(scA[:, 0:32], 0.0), writes=["scA"])
    S.op(DVE, lambda e: e.memset(scB[:, 0:32], 0.0), writes=["scB"])
    S.op(DVE, lambda e: e.tensor_copy(out=scA[:, 32:64], in_=padd[:]), reads=["padd"], writes=["scA"])
    cur, curk, oth, othk = scA, "scA", scB, "scB"
    for j in range(5):
        sh = 1 << j
        S.op(DVE, lambda e, cur=cur, oth=oth, sh=sh: e.tensor_tensor(out=oth[:, 32:64], in0=cur[:, 32:64], in1=cur[:, 32 - sh:64 - sh], op=ALU.add), reads=[curk], writes=[othk])
        cur, curk, oth, othk = oth, othk, cur, curk
    pend = cur; pendk = curk
    S.op(DVE, lambda e: e.tensor_tensor(out=pstart[:], in0=pend[:, 32:64], in1=padd[:], op=ALU.subtract), reads=[pendk, "padd"], writes=["pstart"])
    base = sb("base", [128, NT, 32], F32, stC)
    S.op(DVE, lambda e: e.tensor_tensor(out=base[:], in0=incl[:, 16:48, :], in1=cs0[:], op=ALU.subtract), reads=[inclk, "cs0"], writes=["base"])
    S.op(DVE, lambda e: e.tensor_tensor(out=base[:], in0=base[:], in1=pstart[:].unsqueeze(1).to_broadcast([128, NT, 32]), op=ALU.add), reads=["base", "pstart"], writes=["base"])
    for t in range(NT):
        bank = 5 + t // 16
        S.op(PE, lambda e, t=t, bank=bank: e.matmul(pb[bank][:, (t % 16) * 32:(t % 16 + 1) * 32], lhsT=tri[:], rhs=maskall[:, t, :], start=True, stop=True),
             reads=["maskall"], writes=["pb%d" % bank], inc=(t % 16 == 15))
    slot = sb("slot", [128, NT, 32], F32, stC)
    S.op(DVE, lambda e: e.tensor_tensor(out=slot[:, 0:16, :], in0=pb[5][:].rearrange("p (t e) -> p t e", e=32), in1=base[:, 0:16, :], op=ALU.add), reads=["pb5", "base"], writes=["slot"])
    S.op(DVE, lambda e: e.tensor_tensor(out=slot[:, 16:32, :], in0=pb[6][:].rearrange("p (t e) -> p t e", e=32), in1=base[:, 16:32, :], op=ALU.add), reads=["pb6", "base"], writes=["slot"])
    ebf = sb("ebf", [128, NBLK], F32, stC)
    S.op(DVE, lambda e: e.memset(ebf[:], 0.0), writes=["ebf"])
    for ee in range(32):
        S.op(DVE, lambda e, ee=ee: e.scalar_tensor_tensor(out=ebf[:], in0=bstart[:], scalar=pend[:, 32 + ee:33 + ee], in1=ebf[:], op0=ALU.is_ge, op1=ALU.add),
             reads=[pendk, "ebf"], writes=["ebf"])
    S.op(DVE, lambda e: e.tensor_scalar(out=ebf[:], in0=ebf[:], scalar1=31.0, scalar2=None, op0=ALU.min), reads=["ebf"], writes=["ebf"])
    S.op(DVE, lambda e: e.tensor_copy(out=eb_i[:], in_=ebf[:]), reads=["ebf"], writes=["eb_i"])
    S.op(DVE, lambda e: e.tensor_scalar(out=OHall[:], in0=ebf[:], scalar1=pidx[:, 1:2], scalar2=None, op0=ALU.is_equal), reads=["ebf"], writes=["OHall"])
    neq = sb("neq", [128, NBLK], F32, stC); ridxf = sb("ridxf", [128, NBLK], F32, stC)
    import os as _os2
    S.op(DVE, lambda e: e.memset(neq[:, 0:2], 0.0 if _os2.environ.get('KNOLOAD') else 1.0), writes=["neq"])
    S.op(DVE, lambda e: e.tensor_tensor(out=neq[:, 2:NBLK], in0=ebf[:, 2:NBLK], in1=ebf[:, 0:NBLK - 2], op=(ALU.is_lt if _os2.environ.get('KNOLOAD') else ALU.not_equal)), reads=["ebf"], writes=["neq"])
    S.op(DVE, lambda e: e.tensor_scalar(out=ridxf[:], in0=ebf[:], scalar1=128.0, scalar2=pidx[:, 0:1], op0=ALU.mult, op1=ALU.add), reads=["ebf"], writes=["ridxf"])
    S.op(DVE, lambda e: e.scalar_tensor_tensor(out=ridxf[:], in0=ridxf[:], scalar=-1.0e6, in1=neq[:], op0=ALU.add, op1=ALU.mult), reads=["ridxf", "neq"], writes=["ridxf"])
    S.op(DVE, lambda e: e.tensor_scalar(out=ridxf[:], in0=ridxf[:], scalar1=1.0e6, scalar2=None, op0=ALU.add), reads=["ridxf"], writes=["ridxf"])
    S.op(DVE, lambda e: e.tensor_copy(out=ridx_i[:], in_=ridxf[:]), reads=["ridxf"], writes=["ridx"])
    idxf = sb("idxf", [128, NT, 4], F32, stC); oh = sb("oh", [128, NT, 32], F32, stC); oh2 = sb("oh2", [128, NT, 32], F32, stC)
    destf = sb("destf", [128, NT, 4], F32, stC)
    S.op(DVE, lambda e: e.tensor_copy(out=idxf[:], in_=idx8[:, :, 0:4]), reads=[("idx8", t) for t in range(NT)], writes=["idxf"])
    for k in range(4):
        S.op(DVE, lambda e, k=k: e.tensor_tensor(out=oh[:], in0=iota[:].unsqueeze(1).to_broadcast([128, NT, 32]), in1=idxf[:, :, k:k + 1].to_broadcast([128, NT, 32]), op=ALU.is_equal),
             reads=["idxf"], writes=["oh"])
        S.op(DVE, lambda e: e.tensor_tensor(out=oh2[:], in0=oh[:], in1=slot[:], op=ALU.mult), reads=["oh", "slot"], writes=["oh2"])
        S.op(DVE, lambda e, k=k: e.tensor_reduce(out=destf[:, :, k], in_=oh2[:], axis=AX.X, op=ALU.add), reads=["oh2"], writes=["destf"])
        S.op(DVE, lambda e: e.tensor_tensor(out=oh2[:], in0=oh[:], in1=Gall[:], op=ALU.mult), reads=["oh", "Gall"], writes=["oh2"])
        S.op(DVE, lambda e, k=k: e.tensor_reduce(out=gk[:, :, k], in_=oh2[:], axis=AX.X, op=ALU.add), reads=["oh2"], writes=["gk"])
    S.op(DVE, lambda e: e.tensor_copy(out=dest_i[:], in_=destf[:]), reads=["destf"], writes=["dest_i"])
    xs = [sb("xs%d" % i, [128, 1024], BF16, stC) for i in range(2)]
    for t in range(NT):
        S.op(SP, lambda e, t=t: e.dma_start(out=xs[t % 2][:], in_=xn2_d[t * 128:(t + 1) * 128, :]), reads=["xn2_d"], writes=[("xs", t % 2)], dma="xs%d" % (t % 2))
        for k in range(4):
            S.op(POOL, lambda e, t=t, k=k: e.indirect_dma_start(out=xbuf_d, out_offset=bass.IndirectOffsetOnAxis(ap=dest_i[:, t, k:k + 1], axis=0), in_=xs[t % 2][:], in_offset=None),
                 reads=[("xs", t % 2), "dest_i"], writes=["xbuf"], dma="scat")


def moe_blocks(nc, S, sb, st, pb, pbb, OHall, ridx_i, wbf_d, btab_d, xbuf_d, ybuf_d, identb, nblk=160):
    import os as _o3
    NOW = bool(_o3.environ.get('KNOW'))
    with ExitStack() as stD:
        W = {nm: [sb("%s%d" % (nm, i), [128, 8, 1024], BF16, stD) for i in range(2)] for nm in ("wg", "wu", "wd")}
        Wd_ = {nm: wbf_d[m_].rearrange("(r c2) x -> r (c2 x)", c2=4) for m_, nm in enumerate(("wg", "wu", "wd"))}
        btab = sb("btab", [64, 3072], F32, stD); bt2 = sb("bt2", [64, 3072], BF16, stD); btl = sb("btl", [64, 3072], F32, stD)
        S.op(SP, lambda e: e.dma_start(out=btab[0:32, :], in_=btab_d), writes=["btab"], dma="btab")
        S.op(SP, lambda e: e.dma_start(out=btab[32:64, :], in_=btab_d), writes=["btab"], dma="btab")
        S.op(DVE, lambda e: e.tensor_copy(out=bt2[:], in_=btab[:]), reads=["btab"], writes=["bt2"])
        S.op(DVE, lambda e: e.tensor_copy(out=btl[:], in_=bt2[:]), reads=["bt2"], writes=["btl"])
        S.op(DVE, lambda e: e.tensor_tensor(out=btl[:], in0=btab[:], in1=btl[:], op=ALU.subtract), reads=["btab", "btl"], writes=["btl"])
        S.op(DVE, lambda e: e.tensor_copy(out=bt2[32:64, :], in_=btl[32:64, :]), reads=["btl", "bt2"], writes=["bt2"])
        ohb = [sb("ohb%d" % i, [64, 128], BF16, stD) for i in range(2)]
        xb = [sb("xb%d" % i, [128, 1024], BF16, stD) for i in range(2)]
        xT = [sb("xT%d" % i, [128, 8, 128], BF16, stD) for i in range(2)]
        gtb = [sb("gtb%d" % i, [128, 512], F32, stD) for i in range(2)]; sgb = [sb("sgb%d" % i, [128, 512], F32, stD) for i in range(2)]
        upb = [sb("upb%d" % i, [128, 512], F32, stD) for i in range(2)]
        hdn = [sb("hdn%d" % i, [128, 1024], BF16, stD) for i in range(2)]
        hT = [sb("hT%d" % i, [128, 8, 128], BF16, stD) for i in range(2)]
        yb = [sb("yb%d" % i, [128, 1024], F32, stD) for i in range(2)]
        breg = stD.enter_context(nc.gpsimd.register("bnd_reg"))
        S.op(POOL, lambda e: e.reg_mov(breg, 32 * 128 - 1))

        def gath(nm, b):
            par = b % 2
            S.op(POOL, lambda e: e.indirect_dma_start(out=W[nm][par][:].rearrange("p c f -> p (c f)"), out_offset=None, in_=Wd_[nm],
                 in_offset=bass.IndirectOffsetOnAxis(ap=ridx_i[:, b:b + 1], axis=0), bounds_check=breg, oob_is_err=False),
                 reads=["ridx", "wbf"], writes=[(nm, par, c) for c in range(8)], dma="%s%d" % (nm, par))

        def P1(b):
            par = b % 2
            gath("wg", b); gath("wu", b)
            if b >= 1:
                gath("wd", b - 1)
            if b == 0:
                S.op(SP, lambda e: e.dma_start(out=xb[0][:], in_=xbuf_d[0:128, :]), writes=[("xb", 0)], dma="xb0")
            if b + 1 < nblk:
                S.op(SP, lambda e, b=b: e.dma_start(out=xb[(b + 1) % 2][:], in_=xbuf_d[(b + 1) * 128:(b + 2) * 128, :]), writes=[("xb", (b + 1) % 2)], dma="xb%d" % ((b + 1) % 2))
            S.op(DVE, lambda e, b=b, par=par: e.tensor_copy(out=ohb[par][:], in_=OHall[0:64, b:b + 1].to_broadcast([64, 128])), reads=["OHall"], writes=[("ohb", par)])
            for c in range(8):
                S.op(PE, lambda e, c=c, par=par: e.transpose(out=pbb[6][:, c * 128:(c + 1) * 128], in_=xb[par][:, c * 128:(c + 1) * 128], identity=identb[:]),
                     reads=[("xb", par)], writes=["pb6"], inc=(c == 7))
            S.op(ACT, lambda e, par=par: e.activation(out=xT[par][:], in_=pbb[6].rearrange("p (c t) -> p c t", c=8), func=AF.Copy), reads=["pb6"], writes=[("xT", par)])

        def P2(b):
            par = b % 2
            for hf in range(2):
                accs = [("wg", 2 * hf, hf * 512), ("wu", 2 * hf + 1, 1024 + hf * 512)]
                for (nm, a, boff) in accs:
                    S.op(PE, lambda e, a=a, boff=boff, par=par: e.matmul(pb[a][:], lhsT=ohb[par][:], rhs=bt2[:, boff:boff + 512], start=True, stop=False),
                         reads=[("ohb", par), "bt2"], writes=["pb%d" % a])
                for c in range(8):
                    for (nm, a, boff) in accs:
                        S.op(PE, lambda e, a=a, nm=nm, hf=hf, c=c, par=par: e.matmul(pb[a][:], lhsT=xT[par][:, c, :], rhs=W[nm][par][:, c, hf * 512:(hf + 1) * 512], start=False, stop=(c == 7)),
                             reads=[("xT", par)] + ([] if NOW else [(nm, par, c)]), writes=["pb%d" % a], inc=(c == 7))
                G, U = pb[2 * hf], pb[2 * hf + 1]; gk_, uk_ = "pb%d" % (2 * hf), "pb%d" % (2 * hf + 1)
                S.op(DVE, lambda e, hf=hf, G=G: e.tensor_scalar(out=gtb[hf][:], in0=G[:], scalar1=7.0, scalar2=None, op0=ALU.min), reads=[gk_], writes=[("gtb", hf)])
                S.op(ACT, lambda e, hf=hf: e.activation(out=sgb[hf][:], in_=gtb[hf][:], func=AF.Sigmoid, scale=1.702), reads=[("gtb", hf)], writes=[("sgb", hf)])
                S.op(DVE, lambda e, hf=hf, U=U: e.tensor_scalar(out=upb[hf][:], in0=U[:], scalar1=-7.0, scalar2=7.0, op0=ALU.max, op1=ALU.min), reads=[uk_], writes=[("upb", hf)])
                S.op(DVE, lambda e, hf=hf: e.scalar_tensor_tensor(out=upb[hf][:], in0=upb[hf][:], scalar=1.0, in1=gtb[hf][:], op0=ALU.add, op1=ALU.mult), reads=[("upb", hf), ("gtb", hf)], writes=[("upb", hf)])
                S.op(DVE, lambda e, hf=hf, par=par: e.tensor_tensor(out=hdn[par][:].rearrange("s (c p) -> s p c", p=128)[:, hf * 64:(hf + 1) * 64, :], in0=upb[hf][:].rearrange("s (p c) -> s p c", c=8),
                     in1=sgb[hf][:].rearrange("s (p c) -> s p c", c=8), op=ALU.mult), reads=[("upb", hf), ("sgb", hf)], writes=[("hdn", par)])

        def P3(b):
            par = b % 2
            for c in range(8):
                S.op(PE, lambda e, c=c, par=par: e.transpose(out=pbb[7][:, c * 128:(c + 1) * 128], in_=hdn[par][:, c * 128:(c + 1) * 128], identity=identb[:]),
                     reads=[("hdn", par)], writes=["pb7"], inc=(c == 7))
            S.op(ACT, lambda e, par=par: e.activation(out=hT[par][:], in_=pbb[7].rearrange("p (c t) -> p c t", c=8), func=AF.Copy), reads=["pb7"], writes=[("hT", par)])
            for hf in range(2):
                S.op(PE, lambda e, hf=hf, par=par: e.matmul(pb[4 + hf][:], lhsT=ohb[par][:], rhs=bt2[:, 2048 + hf * 512:2048 + (hf + 1) * 512], start=True, stop=False),
                     reads=[("ohb", par), "bt2"], writes=["pb%d" % (4 + hf)])
                for c in range(8):
                    S.op(PE, lambda e, hf=hf, c=c, par=par: e.matmul(pb[4 + hf][:], lhsT=hT[par][:, c, :], rhs=W["wd"][par][:, c, hf * 512:(hf + 1) * 512], start=False, stop=(c == 7)),
                         reads=[("hT", par)] + ([] if NOW else [("wd", par, c)]), writes=["pb%d" % (4 + hf)], inc=(c == 7))
                if hf == 0:
                    S.op(ACT, lambda e, hf=hf, par=par: e.activation(out=yb[par][:, hf * 512:(hf + 1) * 512], in_=pb[4 + hf][:], func=AF.Copy), reads=["pb%d" % (4 + hf)], writes=[("yb", par)])
                else:
                    S.op(DVE, lambda e, hf=hf, par=par: e.tensor_copy(out=yb[par][:, hf * 512:(hf + 1) * 512], in_=pb[4 + hf][:]), reads=["pb%d" % (4 + hf)], writes=[("yb", par)])
            S.op(SP, lambda e, b=b, par=par: e.dma_start(out=ybuf_d[b * 128:(b + 1) * 128, :], in_=yb[par][:]), reads=[("yb", par)], writes=["ybuf"], dma="yst%d" % par)

        P1(0); P2(0)
        for b in range(1, nblk):
            P1(b); P2(b); P3(b - 1)
        gath("wd", nblk - 1)
        P3(nblk - 1)


def run_pipelined(tile_ops, ntiles, skew):
    lists = {}
    nops = None
    s = 0
    done = 0
    while done < ntiles:
        for t in range(ntiles):
            st_ = s - t * skew
            if st_ < 0:
                break
            if t not in lists:
                lists[t] = tile_ops(t)
                nops = len(lists[t])
            if st_ < len(lists[t]):
                lists[t][st_]()
                if st_ == len(lists[t]) - 1:
                    done += 1
        s += 1


def ple_phase(nc, S, sb, st, pb, pbb, dest_i, gk, h1_d, ybuf_d, p_d, w_pg_d, w_pp_d, post_b_d, gcol_ple, identb, out_d, epsc):
    NT = 32
    with ExitStack() as stE:
        w_pg = sb("w_pg", [128, 8, 1024], BF16, stE); w_pp = sb("w_pp", [128, 2, 1024], BF16, stE); post_b = sb("post_b", [128, 1024], F32, stE)
        for c in range(8):
            S.op(POOL, lambda e, c=c: e.dma_start(out=w_pg[:, c, :], in_=w_pg_d[c * 128:(c + 1) * 128, :]), writes=[("w_pg", c)], dma="w_pg")
        for c in range(8):
            S.op(DVE, lambda e, c=c: e.tensor_scalar(out=w_pg[:, c, :], in0=w_pg[:, c, :], scalar1=gcol_ple[:, c:c + 1], scalar2=None, op0=ALU.mult), reads=[("w_pg", c)], writes=[("w_pg", c)])
        for c in range(2):
            S.op(POOL, lambda e, c=c: e.dma_start(out=w_pp[:, c, :], in_=w_pp_d[c * 128:(c + 1) * 128, :]), writes=["w_pp"], dma="w_pp")
        S.op(SP, lambda e: e.dma_start(out=post_b[:], in_=post_b_d), writes=["post_b"], dma="cE")
        D = lambda nm, shape, dt=F32: [sb("%s_%d" % (nm, i), shape, dt, stE) for i in range(3)]
        yk = [[sb("yk%d_%d" % (i, k), [128, 1024], F32, stE) for k in range(4)] for i in range(3)]
        h2 = D("h2", [128, 1024]); pt = D("pt", [128, 256]); ptb = D("ptb", [128, 256], BF16); pT = D("pT", [128, 2, 128], BF16)
        hn3 = D("hn3", [128, 1024], BF16); hn3T = D("hn3T", [128, 8, 128], BF16); ejunk = D("ejunk", [128, 1024], BF16)
        gate = D("gate", [128, 1024]); et = D("et", [128, 1024]); st5 = D("st5", [128, 8])

        def tile_ops(t):
            par = t % 3
            ops = []
            A = ops.append
            P = lambda nm: (nm, par)
            A(lambda: S.op(SP, lambda e: e.dma_start(out=h2[par][:], in_=h1_d[t * 128:(t + 1) * 128, :]), reads=["h1_d"], writes=[P("h2")], dma="h2_%d" % par))
            A(lambda: S.op(SP, lambda e: e.dma_start(out=pt[par][:], in_=p_d[t * 128:(t + 1) * 128, :]), writes=[P("pt")], dma="pt%d" % par))
            for k in range(4):
                A(lambda k=k: S.op(POOL, lambda e: e.indirect_dma_start(out=yk[par][k][:], out_offset=None, in_=ybuf_d, in_offset=bass.IndirectOffsetOnAxis(ap=dest_i[:, t, k:k + 1], axis=0)),
                                   reads=["ybuf", "dest_i"], writes=[("yk", par, k)], dma="yk%d" % par))
            A(lambda: S.op(ACT, lambda e: e.activation(out=ptb[par][:], in_=pt[par][:], func=AF.Copy), reads=[P("pt")], writes=[P("ptb")]))
            for c in range(2):
                A(lambda c=c: S.op(PE, lambda e: e.transpose(out=pbb[3][:, c * 128:(c + 1) * 128], in_=ptb[par][:, c * 128:(c + 1) * 128], identity=identb[:]), reads=[P("ptb")], writes=["pb3"], inc=(c == 1)))
            A(lambda: S.op(DVE, lambda e: e.tensor_copy(out=pT[par][:], in_=pbb[3][:, 0:256].rearrange("p (c t) -> p c t", c=2)), reads=["pb3"], writes=[P("pT")]))
            A(lambda: S.op(DVE, lambda e: e.memset(st5[par][:, 0:3], 0.0), writes=[P("st5")]))
            for hf in range(2):
                for c in range(2):
                    A(lambda c=c, hf=hf: S.op(PE, lambda e: e.matmul(pb[4 + hf][:], lhsT=pT[par][:, c, :], rhs=w_pp[:, c, hf * 512:(hf + 1) * 512], start=(c == 0), stop=(c == 1)),
                                              reads=[P("pT"), "w_pp"], writes=["pb%d" % (4 + hf)], inc=(c == 1)))
                A(lambda hf=hf: S.op(ACT, lambda e: e.activation(out=ejunk[par][:, hf * 512:(hf + 1) * 512], in_=pb[4 + hf][:], func=AF.Square, accum_out=st5[par][:, 1 + hf:2 + hf]),
                                     reads=["pb%d" % (4 + hf), P("st5")], writes=[P("ejunk"), ("st5e", par, hf)]))
            A(lambda: S.op(DVE, lambda e: e.tensor_tensor(out=st5[par][:, 4:5], in0=st5[par][:, 1:2], in1=st5[par][:, 2:3], op=ALU.add), reads=[("st5e", par, 0), ("st5e", par, 1)], writes=[P("st5s")]))
            A(lambda: S.op(ACT, lambda e: e.activation(out=st5[par][:, 4:5], in_=st5[par][:, 4:5], func=AF.Ln, scale=1.0 / 1024, bias=epsc[:, 0:1]), reads=[P("st5s")], writes=[P("st5s")]))
            A(lambda: S.op(ACT, lambda e: e.activation(out=st5[par][:, 4:5], in_=st5[par][:, 4:5], func=AF.Exp, scale=-0.5), reads=[P("st5s")], writes=[P("st5s")]))
            for hf in range(2):
                A(lambda hf=hf: S.op(DVE, lambda e: e.scalar_tensor_tensor(out=et[par][:, hf * 512:(hf + 1) * 512], in0=pb[4 + hf][:], scalar=st5[par][:, 4:5], in1=post_b[:, hf * 512:(hf + 1) * 512], op0=ALU.mult, op1=ALU.mult),
                                     reads=["pb%d" % (4 + hf), P("st5s"), "post_b"], writes=[P("et")]))
            A('STAGE')
            for k in range(4):
                A(lambda k=k: S.op(DVE, lambda e: e.scalar_tensor_tensor(out=h2[par][:], in0=yk[par][k][:], scalar=gk[:, t, k:k + 1], in1=h2[par][:], op0=ALU.mult, op1=ALU.add),
                                   reads=[("yk", par, k), P("h2"), "gk"], writes=[P("h2")]))
            A(lambda: S.op(ACT, lambda e: e.activation(out=ejunk[par][:], in_=h2[par][:], func=AF.Square, accum_out=st5[par][:, 0:1]), reads=[P("h2"), P("st5")], writes=[P("ejunk"), P("st5a")]))
            A(lambda: S.op(ACT, lambda e: e.activation(out=st5[par][:, 3:4], in_=st5[par][:, 0:1], func=AF.Ln, scale=1.0 / 1024, bias=epsc[:, 0:1]), reads=[P("st5a")], writes=[P("st5r")]))
            A(lambda: S.op(ACT, lambda e: e.activation(out=st5[par][:, 3:4], in_=st5[par][:, 3:4], func=AF.Exp, scale=-0.5), reads=[P("st5r")], writes=[P("st5r")]))
            A(lambda: S.op(ACT, lambda e: e.activation(out=hn3[par][:], in_=h2[par][:], func=AF.Copy, scale=st5[par][:, 3:4]), reads=[P("h2"), P("st5r")], writes=[P("hn3")]))
            for c in range(8):
                A(lambda c=c: S.op(PE, lambda e: e.transpose(out=pbb[0][:, c * 128:(c + 1) * 128], in_=hn3[par][:, c * 128:(c + 1) * 128], identity=identb[:]), reads=[P("hn3")], writes=["pb0"], inc=(c == 7)))
            A(lambda: S.op(DVE, lambda e: e.tensor_copy(out=hn3T[par][:], in_=pbb[0].rearrange("p (c t) -> p c t", c=8)), reads=["pb0"], writes=[P("hn3T")]))
            A('STAGE')
            for hf in range(2):
                for c in range(8):
                    A(lambda c=c, hf=hf: S.op(PE, lambda e: e.matmul(pb[1 + hf][:], lhsT=hn3T[par][:, c, :], rhs=w_pg[:, c, hf * 512:(hf + 1) * 512], start=(c == 0), stop=(c == 7)),
                                              reads=[P("hn3T"), ("w_pg", c)], writes=["pb%d" % (1 + hf)], inc=(c == 7)))
                A(lambda hf=hf: S.op(ACT, lambda e: e.activation(out=gate[par][:, hf * 512:(hf + 1) * 512], in_=pb[1 + hf][:], func=AF.Sigmoid), reads=["pb%d" % (1 + hf)], writes=[P("gate")]))
            A(lambda: S.op(DVE, lambda e: e.tensor_tensor(out=et[par][:], in0=et[par][:], in1=gate[par][:], op=ALU.mult), reads=[P("et"), P("gate")], writes=[P("et")]))
            A(lambda: S.op(DVE, lambda e: e.tensor_tensor(out=et[par][:], in0=et[par][:], in1=h2[par][:], op=ALU.add), reads=[P("et"), P("h2")], writes=[P("et")]))
            A(lambda: S.op(SP, lambda e: e.dma_start(out=out_d[t * 128:(t + 1) * 128, :], in_=et[par][:]), reads=[P("et")], writes=["out_d"], dma="out%d" % par))
            stages = [[]]
            for o in ops:
                if o == 'STAGE':
                    stages.append([])
                else:
                    stages[-1].append(o)
            return stages
        cache = {}

        def get(t):
            if t not in cache:
                cache[t] = tile_ops(t)
            return cache[t]
        for it in range(NT + 2):
            lists = [get(t)[stg] for stg, t in ((2, it - 2), (1, it - 1), (0, it)) if 0 <= t < NT]
            L = max(len(x) for x in lists)
            for i in range(L):
                for x in lists:
                    j0 = (i * len(x)) // L
                    j1 = ((i + 1) * len(x)) // L
                    for j in range(j0, j1):
                        x[j]()
import math
import os as _os
from contextlib import ExitStack
import ml_dtypes
from concourse.bass_utils import run_bass_kernel_spmd

S_TOK = 4096
NT = 32
NG = 8
NBLK = 160
NSLOT = NBLK * 128
LAMBDA_INIT = 0.2
EPS = 1e-6
DEBUG = False


def build(stage=99):
    nc = bass.Bass("TRN2", target_bir_lowering=False)

    def din(name, shape, dt=F32):
        return nc.dram_tensor(name, list(shape), dt, kind="ExternalInput").ap()

    x_d = din("x", [S_TOK, 1024]); p_d = din("p", [S_TOK, 256]); pos_d = din("posb", [128, S_TOK], I32)
    w_in_d = din("w_in", [1024, 2048]); gcol_attn_d = din("gcol_attn", [128, 8])
    qn_d = din("qn_col", [128, 1]); kn_d = din("kn_col", [128, 1]); lamv_d = din("lamv", [128, 4, 64])
    subln_d = din("subln_b", [128, 128]); w_pool_d = din("w_pool", [4, 128, 128]); pscale_d = din("pscale_col", [128, 4])
    w_out_d = din("w_out", [1024, 1024]); ffn_b_d = din("ffn_b", [128, 1024]); w_r_d = din("w_router", [128, 8, 32])
    b_r_d = din("b_router_b", [128, 32])
    wg_d = din("w_gate", [32, 1024, 1024]); wu_d = din("w_up", [32, 1024, 1024]); wd_d = din("w_down", [32, 1024, 1024])
    btab_d = din("btab", [32, 3072])
    gcol_ple_d = din("gcol_ple", [128, 8]); w_pg_d = din("w_ple_gate", [1024, 1024]); w_pp_d = din("w_ple_proj", [256, 1024])
    post_b_d = din("post_b", [128, 1024])
    identb_d = din("ident_bf", [128, 128], BF16); identf_d = din("ident_f", [128, 128]); onesf_d = din("ones_f", [128, 128])
    tri_d = din("tri_strict", [128, 128]); blk_d = din("blkdiag", [128, 128], BF16); rmat_d = din("rmat", [128, 128], BF16)
    cmask_d = din("cmask", [128, 128], BF16); freq_d = din("freq_col", [128, 1]); invc_d = din("invcnt", [128, 16])
    iota_d = din("iota_row", [128, 32]); bstart_d = din("bstart_row", [128, NBLK]); pidx_d = din("pidx_col", [128, 2])
    out_d = nc.dram_tensor("out", [S_TOK, 1024], F32, kind="ExternalOutput").ap()
    KS = "ExternalOutput" if DEBUG else "Internal"
    catp_d = nc.dram_tensor("catp_s", [4, 128, S_TOK], BF16, kind=KS).ap()
    if DEBUG:
        dbg_q = nc.dram_tensor("dbg_q", [128, 4, S_TOK], BF16, kind=KS).ap(); dbg_k = nc.dram_tensor("dbg_k", [128, 4, S_TOK], BF16, kind=KS).ap()
        dbg_v = nc.dram_tensor("dbg_v", [128, NT, 4, 130], BF16, kind=KS).ap(); dbg_g = nc.dram_tensor("dbg_g", [128, 1024], BF16, kind=KS).ap(); dbg_v2 = nc.dram_tensor("dbg_v2", [128, NT, 4, 130], BF16, kind=KS).ap()
        dbg_r = nc.dram_tensor("dbg_r", [128, NT * 4 * 2 + NBLK], F32, kind=KS).ap()
    cata_d = nc.dram_tensor("cata_s", [128, 4, S_TOK], BF16, kind=KS).ap()
    wbf_d = [nc.dram_tensor("wbf_s%d" % i, [32 * 512, 2048], BF16, kind="Internal").ap() for i in range(3)]
    h1_d = nc.dram_tensor("h1_s", [S_TOK, 1024], F32, kind=KS).ap()
    xn2_d = nc.dram_tensor("xn2_s", [S_TOK, 1024], BF16, kind=KS).ap()
    xbuf_d = nc.dram_tensor("xbuf_s", [NSLOT, 1024], BF16, kind=KS).ap()
    ybuf_d = nc.dram_tensor("ybuf_s", [NSLOT, 1024], F32, kind=KS).ap()

    with ExitStack() as st:
        S = Sched(nc, st)

        def sb(name, shape, dt=F32, stack=st):
            return stack.enter_context(nc.sbuf_tensor("s_" + name, list(shape), dt))

        pb = [st.enter_context(nc.psum_tensor("pb%d" % i, [128, 512], F32)) for i in range(8)]
        pbb = [pb[i][:].bitcast(BF16) for i in range(8)]

        def barrier():
            toks = []
            for e in (PE, ACT, DVE, POOL, SP):
                if S.pending[e]:
                    last = S.ops[e][-1]
                    last.inc, last.incv = S.esem[e], 1
                    S.ecount[e] += 1
                    S.pending[e] = False
                if S.ecount[e] > 0:
                    toks.append(Tok(S.esem[e], S.ecount[e], e))
            for name, s in S.dsems.items():
                toks.append(Tok(s, S.dcount[id(s)], None))
            for e in (PE, ACT, DVE, POOL, SP):
                S.op(e, lambda h: h.nop(), extra=toks, inc=False if e == PE else None)
            S.writer.clear(); S.readers.clear()

        identb = sb("identb", [128, 128], BF16); identf = sb("identf", [128, 128]); onesf = sb("onesf", [128, 128])
        tri = sb("tri", [128, 128]); blk = sb("blk", [128, 128], BF16); rmat = sb("rmat", [128, 128], BF16)
        cmask = sb("cmask", [128, 128], BF16); freq = sb("freq", [128, 1]); invc = sb("invc", [128, 16])
        iota = sb("iota", [128, 32]); bstart = sb("bstart", [128, NBLK])
        qn = sb("qn", [128, 1]); kn = sb("kn", [128, 1]); lamv = sb("lamv", [128, 4, 64]); subln = sb("subln", [128, 128])
        pscale = sb("pscale", [128, 4]); gcol_attn = sb("gcol_attn", [128, 8]); gcol_ple = sb("gcol_ple", [128, 8])
        lam_c = sb("lam_c", [128, 4]); epsc = sb("epsc", [128, 1])
        OHall = sb("OHall", [128, NBLK], F32); ridx_i = sb("ridx_i", [128, NBLK], I32); pidx = sb("pidx", [128, 2], F32)
        S.op(DVE, lambda e: e.memset(epsc[:], EPS), writes=["epsc"])
        for i, (t_, d_) in enumerate([(identb, identb_d), (identf, identf_d), (onesf, onesf_d), (tri, tri_d), (blk, blk_d), (rmat, rmat_d),
                       (cmask, cmask_d), (freq, freq_d), (invc, invc_d), (iota, iota_d), (bstart, bstart_d), (qn, qn_d),
                       (kn, kn_d), (lamv, lamv_d), (subln, subln_d), (pscale, pscale_d), (gcol_attn, gcol_attn_d),
                       (gcol_ple, gcol_ple_d), (pidx, pidx_d)]):
            S.op(SP, lambda e, t_=t_, d_=d_: e.dma_start(out=t_[:], in_=d_), writes=["const"], dma="const")
        lamt = sb("lamt", [128, 2, 64])
        S.op(DVE, lambda e: e.tensor_tensor(out=lamt[:, 0, :], in0=lamv[:, 0, :], in1=lamv[:, 1, :], op=ALU.mult), reads=["const"], writes=["lamt"])
        S.op(DVE, lambda e: e.tensor_tensor(out=lamt[:, 1, :], in0=lamv[:, 2, :], in1=lamv[:, 3, :], op=ALU.mult), reads=["const"], writes=["lamt"])
        S.op(DVE, lambda e: e.tensor_reduce(out=lam_c[:, 0:2], in_=lamt[:], axis=AX.X, op=ALU.add), reads=["lamt"], writes=["lam_c"])
        S.op(ACT, lambda e: e.activation(out=lam_c[:, 0:2], in_=lam_c[:, 0:2], func=AF.Exp), reads=["lam_c"], writes=["lam_c"])
        S.op(DVE, lambda e: e.tensor_tensor(out=lam_c[:, 2:3], in0=lam_c[:, 1:2], in1=lam_c[:, 0:1], op=ALU.subtract), reads=["lam_c"], writes=["lam_c2"])
        S.op(DVE, lambda e: e.tensor_scalar(out=lam_c[:, 3:4], in0=lam_c[:, 2:3], scalar1=-LAMBDA_INIT, scalar2=None, op0=ALU.add), reads=["lam_c2"], writes=["nlam"])
        nlam = lam_c[:, 3:4]
        S.op(DVE, lambda e: e.tensor_scalar(out=subln[:], in0=subln[:], scalar1=1.0 - LAMBDA_INIT, scalar2=None, op0=ALU.mult), reads=["const"], writes=["const"])

        eb_i = sb("eb_i", [128, NBLK], I32); dest_i = sb("dest_i", [128, NT, 4], I32); gk = sb("gk", [128, NT, 4], F32)
        def _precast_gen():
            for e_ in range(32):
                for m_, wsrc in enumerate((wg_d, wu_d, wd_d)):
                    S.op(POOL, lambda e, m_=m_, e_=e_, wsrc=wsrc: e.dma_start(out=wbf_d[m_][e_ * 512:(e_ + 1) * 512, :], in_=wsrc[e_].rearrange("(r two) f -> r (two f)", two=2)),
                         writes=["wbf"], dma="precast")
                    yield
        _pc = _precast_gen()

        def precast(k_):
            for _ in range(k_):
                try:
                    next(_pc)
                except StopIteration:
                    return
        with ExitStack() as stAB:
            qT = sb("qT", [128, 4, S_TOK], BF16, stAB); kT = sb("kT", [128, 4, S_TOK], BF16, stAB)
            Vsb = sb("Vsb", [128, NT, 4, 130], BF16, stAB)
            S.op(POOL, lambda e: e.memset(Vsb[:, :, :, 128:130], 1.0), writes=["Vones"])
            with ExitStack() as stA:
                w_in = sb("w_in", [128, 8, 2048], BF16, stA)
                for c in range(8):
                    S.op(POOL, lambda e, c=c: e.dma_start(out=w_in[:, c, :], in_=w_in_d[c * 128:(c + 1) * 128, :]), writes=[("w_in", c)], dma="w_in")
                for c in range(8):
                    S.op(DVE, lambda e, c=c: e.tensor_scalar(out=w_in[:, c, :], in0=w_in[:, c, :], scalar1=gcol_attn[:, c:c + 1], scalar2=None, op0=ALU.mult),
                         reads=[("w_in", c), "const"], writes=[("w_in", c)])
                wpool = sb("wpool", [128, 4, 128], BF16, stA)
                for g in range(4):
                    S.op(POOL, lambda e, g=g: e.dma_start(out=wpool[:, g, :], in_=w_pool_d[g]), writes=["wpool"], dma="wpool")
                xt = [sb("xt%d" % i, [128, 1024], F32, stA) for i in range(2)]
                xjunk = sb("xjunk", [128, 1024], BF16, stA)
                xn = [sb("xn%d" % i, [128, 1024], BF16, stA) for i in range(2)]
                st1 = sb("st1", [128, 64], F32, stA)
                hnT = [sb("hnT%d" % i, [128, 8, 512], BF16, stA) for i in range(2)]
                posi = sb("posi", [128, 512], I32, stA)
                cosn = sb("cosn", [128, 512], F32, stA); sinn = sb("sinn", [128, 512], F32, stA)
                sq = [sb("sq%d" % i, [128, 512], BF16, stA) for i in range(2)]
                qg = [sb("qg%d" % i, [128, 512], BF16, stA) for i in range(2)]
                rstd_t = sb("rstd_t", [128, 512], F32, stA); t1 = sb("t1", [128, 512], F32, stA); t2 = sb("t2", [128, 512], F32, stA); ang = t1; ang2 = t2
                uT = [sb("uT0", [128, 4, 528], F32, stA)] * 2
                sA = sb("sA", [128, 528], F32, stA); sB = sb("sB", [128, 528], F32, stA)
                pooled = sb("pooled", [128, 4, 512], BF16, stA); catp = [sb("catp0", [128, 4, 512], BF16, stA)] * 2
                S.op(DVE, lambda e: e.memset(st1[:], 0.0), writes=["st1"])
                S.op(POOL, lambda e: e.memset(uT[0][:, :, 0:16], 0.0), writes=[("uT", 0)])
                def front_ops(n):
                    par = n % 2
                    hk = ("hnT", par)
                    for j in range(4):
                        t = n * 4 + j
                        xk = ("xt", t % 2)
                        S.op(SP, lambda e, t=t: e.dma_start(out=xt[t % 2][:], in_=x_d[t * 128:(t + 1) * 128, :]), writes=[xk], dma="xt%d" % (t % 2))
                        S.op(ACT, lambda e, t=t: e.activation(out=xjunk[:], in_=xt[t % 2][:], func=AF.Square, accum_out=st1[:, t:t + 1]),
                             reads=[xk, "st1"], writes=["xjunk", ("ss", t)])
                        S.op(ACT, lambda e, t=t: e.activation(out=st1[:, 32 + t:33 + t], in_=st1[:, t:t + 1], func=AF.Ln, scale=1.0 / 1024, bias=epsc[:, 0:1]),
                             reads=[("ss", t)], writes=[("rs", t)])
                        S.op(ACT, lambda e, t=t: e.activation(out=st1[:, 32 + t:33 + t], in_=st1[:, 32 + t:33 + t], func=AF.Exp, scale=-0.5),
                             reads=[("rs", t)], writes=[("rs", t)])
                        S.op(ACT, lambda e, t=t: e.activation(out=xn[t % 2][:], in_=xt[t % 2][:], func=AF.Copy, scale=st1[:, 32 + t:33 + t]),
                             reads=[xk, ("rs", t)], writes=[("xn", t % 2)])
                        for c in range(8):
                            S.op(PE, lambda e, t=t, c=c: e.transpose(out=pbb[6][:, c * 128:(c + 1) * 128], in_=xn[t % 2][:, c * 128:(c + 1) * 128], identity=identb[:]),
                                 reads=[("xn", t % 2), "const"], writes=["pb6"], inc=(c == 7))
                        S.op(DVE, lambda e, j=j, par=par: e.tensor_copy(out=hnT[par][:, :, j * 128:(j + 1) * 128], in_=pbb[6].rearrange("p (c t) -> p c t", c=8)),
                             reads=["pb6"], writes=[hk])
                S.begin_capture(); front_ops(0); capF = S.end_capture()
                S.replay_interleaved([capF])
                for n in range(NG):
                    par = n % 2
                    hk = ("hnT", par)
                    pass
                    precast(4)
                    S.op(SP, lambda e, n=n: e.dma_start(out=posi[:], in_=pos_d[:, n * 512:(n + 1) * 512]), writes=["posi"], dma="posi")
                    S.op(DVE, lambda e: e.tensor_copy(out=ang[:], in_=posi[:]), reads=["posi"], writes=["t1"])
                    S.op(DVE, lambda e: e.tensor_scalar(out=ang[:], in0=ang[:], scalar1=freq[:, 0:1], scalar2=None, op0=ALU.mult), reads=["t1", "const"], writes=["t1"])
                    S.op(DVE, lambda e: e.tensor_scalar(out=ang2[:], in0=ang[:], scalar1=math.pi / 2, scalar2=None, op0=ALU.add), reads=["t1"], writes=["t2"])
                    for (aa, ak) in ((ang, "t1"), (ang2, "t2")):
                        S.op(DVE, lambda e, aa=aa: e.tensor_scalar(out=rstd_t[:], in0=aa[:], scalar1=1.0 / (2 * math.pi), scalar2=None, op0=ALU.mult), reads=[ak], writes=["rstd_t"])
                        S.op(DVE, lambda e: e.tensor_copy(out=posi[:], in_=rstd_t[:]), reads=["rstd_t"], writes=["posi"])
                        S.op(DVE, lambda e: e.tensor_copy(out=rstd_t[:], in_=posi[:]), reads=["posi"], writes=["rstd_t"])
                        S.op(DVE, lambda e, aa=aa: e.scalar_tensor_tensor(out=aa[:], in0=rstd_t[:], scalar=-2 * math.pi, in1=aa[:], op0=ALU.mult, op1=ALU.add), reads=["rstd_t", ak], writes=[ak])
                        S.op(DVE, lambda e, aa=aa: e.tensor_scalar(out=rstd_t[:], in0=aa[:], scalar1=math.pi, scalar2=-2 * math.pi, op0=ALU.is_gt, op1=ALU.mult), reads=[ak], writes=["rstd_t"])
                        S.op(DVE, lambda e, aa=aa: e.tensor_tensor(out=aa[:], in0=aa[:], in1=rstd_t[:], op=ALU.add), reads=[ak, "rstd_t"], writes=[ak])
                        S.op(DVE, lambda e, aa=aa: e.tensor_scalar(out=rstd_t[:], in0=aa[:], scalar1=-math.pi, scalar2=2 * math.pi, op0=ALU.is_lt, op1=ALU.mult), reads=[ak], writes=["rstd_t"])
                        S.op(DVE, lambda e, aa=aa: e.tensor_tensor(out=aa[:], in0=aa[:], in1=rstd_t[:], op=ALU.add), reads=[ak, "rstd_t"], writes=[ak])
                    S.op(ACT, lambda e: e.activation(out=sinn[:], in_=ang[:], func=AF.Sin), reads=["t1"], writes=["sinn"])
                    S.op(ACT, lambda e: e.activation(out=cosn[:], in_=ang2[:], func=AF.Sin), reads=["t2"], writes=["cosn"])
                    S.begin_capture()
                    for ch in range(8):
                        pa = pb[ch % 2]; pak = "pb%d" % (ch % 2)
                        sp_ = ch % 2
                        for c in range(8):
                            S.op(PE, lambda e, c=c, ch=ch, pa=pa, par=par: e.matmul(pa[:], lhsT=w_in[:, c, ch * 128:(ch + 1) * 128], rhs=hnT[par][:, c, :], start=(c == 0), stop=(c == 7)),
                                 reads=[hk, ("w_in", c)], writes=[pak], inc=(c == 7))
                        S.op(ACT, lambda e, pa=pa, sp_=sp_: e.activation(out=sq[sp_][:], in_=pa[:], func=AF.Square), reads=[pak], writes=[("sq", sp_)])
                        gc = qn if ch < 4 else kn
                        S.op(ACT, lambda e, pa=pa, sp_=sp_, gc=gc: e.activation(out=qg[sp_][:], in_=pa[:], func=AF.Copy, scale=gc[:, 0:1]), reads=[pak, "const"], writes=[("qg", sp_)])
                        pm = pb[2 + (ch % 2)]; pmk = "pb%d" % (2 + ch % 2)
                        pr = pb[4 + (ch % 2)]; prk = "pb%d" % (4 + ch % 2)
                        S.op(PE, lambda e, pm=pm, sp_=sp_: e.matmul(pm[:], lhsT=blk[:], rhs=sq[sp_][:], start=True, stop=True), reads=[("sq", sp_), "const"], writes=[pmk], inc=True)
                        S.op(PE, lambda e, pr=pr, sp_=sp_: e.matmul(pr[:], lhsT=rmat[:], rhs=qg[sp_][:], start=True, stop=True), reads=[("qg", sp_), "const"], writes=[prk], inc=True)
                        S.op(ACT, lambda e, pm=pm: e.activation(out=rstd_t[:], in_=pm[:], func=AF.Ln, bias=epsc[:, 0:1]), reads=[pmk], writes=["rstd_t"])
                        S.op(ACT, lambda e: e.activation(out=rstd_t[:], in_=rstd_t[:], func=AF.Exp, scale=-0.5), reads=["rstd_t"], writes=["rstd_t"])
                        S.op(POOL, lambda e, sp_=sp_: e.tensor_tensor(out=t1[:], in0=qg[sp_][:], in1=cosn[:], op=ALU.mult), reads=[("qg", sp_), "cosn"], writes=["t1"])
                        S.op(DVE, lambda e, pr=pr: e.tensor_tensor(out=t2[:], in0=pr[:], in1=sinn[:], op=ALU.mult), reads=[prk, "sinn"], writes=["t2"])
                        S.op(DVE, lambda e: e.tensor_tensor(out=t1[:], in0=t1[:], in1=t2[:], op=ALU.add), reads=["t1", "t2"], writes=["t1"])
                        dst = (qT if ch < 4 else kT)
                        S.op(DVE, lambda e, dst=dst, ch=ch, n=n: e.tensor_tensor(out=dst[:, ch % 4, n * 512:(n + 1) * 512], in0=t1[:], in1=rstd_t[:], op=ALU.mult),
                             reads=["t1", "rstd_t"], writes=[("qk", ch, n)])
                    capA = S.end_capture(); S.begin_capture()
                    for j in range(4):
                        t = n * 4 + j
                        for c in range(8):
                            S.op(PE, lambda e, c=c, j=j, par=par: e.matmul(pb[6][:], lhsT=hnT[par][:, c, j * 128:(j + 1) * 128], rhs=w_in[:, c, 1024:1536], start=(c == 0), stop=(c == 7)),
                                 reads=[hk, ("w_in", c)], writes=["pb6"], inc=(c == 7))
                        S.op(ACT, lambda e, t=t: e.activation(out=Vsb[:, t, :, 0:128], in_=pb[6][:].rearrange("p (h v) -> p h v", h=4), func=AF.Copy),
                             reads=["pb6"], writes=[("V", t)])
                    capB = S.end_capture(); S.begin_capture()
                    uk = ("uT", 0)
                    if n > 0:
                        S.op(POOL, lambda e, par=par: e.tensor_copy(out=uT[par][:, :, 0:16], in_=uT[1 - par][:, :, 512:528]), reads=[("uT", 0)], writes=[uk])
                    for g in range(4):
                        for c in range(8):
                            S.op(PE, lambda e, c=c, g=g, par=par: e.matmul(pb[7][:], lhsT=w_in[:, c, 1536 + g * 128:1536 + (g + 1) * 128], rhs=hnT[par][:, c, :], start=(c == 0), stop=(c == 7)),
                                 reads=[hk, ("w_in", c)], writes=["pb7"], inc=(c == 7))
                        S.op(ACT, lambda e, g=g, par=par: e.activation(out=uT[par][:, g, 16:528], in_=pb[7][:], func=AF.Copy), reads=["pb7"], writes=[uk])
                        src = uT[par][:, g, :]
                        bufs = [sA, sB]
                        cur, curk = src, uk
                        for jj in range(g + 1):
                            sh = 1 << jj
                            lo = (1 << (jj + 1)) - 1
                            o = bufs[jj % 2]; ok_ = "sA" if jj % 2 == 0 else "sB"
                            S.op(POOL, lambda e, o=o, cur=cur, lo=lo, sh=sh: e.tensor_tensor(out=o[:, lo:528], in0=cur[:, lo:528], in1=cur[:, lo - sh:528 - sh], op=ALU.add),
                                 reads=[curk], writes=[ok_])
                            cur, curk = o, ok_
                        w = 1 << (g + 1)
                        S.op(DVE, lambda e, g=g, cur=cur, src=src, w=w: e.scalar_tensor_tensor(out=pooled[:, g, :], in0=cur[:, 16:528], scalar=1.0 / w, in1=src[:, 16:528], op0=ALU.mult, op1=ALU.subtract),
                             reads=[curk, uk], writes=["pooled"])
                        if n == 0:
                            S.op(DVE, lambda e, cur=cur, w=w: e.tensor_tensor(out=cur[:, 16:16 + w - 1], in0=cur[:, 16:16 + w - 1], in1=invc[:, 0:w - 1], op=ALU.mult),
                                 reads=[curk, "const"], writes=[curk])
                            S.op(DVE, lambda e, g=g, cur=cur, src=src, w=w: e.tensor_tensor(out=pooled[:, g, 0:w - 1], in0=cur[:, 16:16 + w - 1], in1=src[:, 16:16 + w - 1], op=ALU.subtract),
                                 reads=[curk, uk], writes=["pooled"])
                        S.op(PE, lambda e, g=g: e.matmul(pb[7][:], lhsT=wpool[:, g, :], rhs=pooled[:, g, :], start=True, stop=True), reads=["pooled", "wpool"], writes=["pb7"], inc=True)
                        S.op(ACT, lambda e, g=g, par=par: e.activation(out=catp[par][:, g, :], in_=pb[7][:], func=AF.Copy, scale=pscale[:, g:g + 1]), reads=["pb7", "const"], writes=[("catp", 0)])
                    for g in range(4):
                        S.op(POOL, lambda e, g=g, n=n, par=par: e.dma_start(out=catp_d[g, :, n * 512:(n + 1) * 512], in_=catp[par][:, g, :]), reads=[("catp", 0)], writes=["catp_d"], dma="catp_st")
                    capC = S.end_capture()
                    capF = []
                    if n + 1 < NG:
                        S.begin_capture(); front_ops(n + 1); capF = S.end_capture()
                    S.replay_interleaved([capA, capB + capF, capC])

            barrier()
            if DEBUG and _os.environ.get("KEARLY", "1") == "1":
                S.op(SP, lambda e: e.dma_start(out=dbg_q, in_=qT[:]), dma="dbg")
                S.op(SP, lambda e: e.dma_start(out=dbg_k, in_=kT[:]), dma="dbg")
                S.op(SP, lambda e: e.dma_start(out=dbg_v, in_=Vsb[:]), dma="dbg")
            with ExitStack() as stB:
                catA = sb("catA", [128, 4, S_TOK], BF16, stB)
                PT = [[sb("PT%d_%d" % (i, c), [128, 512], BF16, stB) for c in range(2)] for i in range(2)]
                otmp = sb("otmp", [128, 128], F32, stB); ojunk = sb("ojunk", [128, 128], BF16, stB)
                ob = sb("ob", [128, 4, 128], BF16, stB); st2 = sb("st2", [128, 8], F32, stB)

                def Oap(i, lo, hi):
                    return pb[4 + i // 3][:, (i % 3) * 160 + lo:(i % 3) * 160 + hi]
                osb = [sb("osb%d" % i, [128, 512], F32, stB) for i in range(3)]

                def Osb(i, lo, hi):
                    return osb[i // 3][:, (i % 3) * 160 + lo:(i % 3) * 160 + hi]

                def SE(h, n, kt):
                    bufi = kt % 2
                    j = kt - 4 * n
                    qlo = max(j, 0) * 128
                    for c in range(2):
                        ps = pb[bufi * 2 + c]; psk = "pb%d" % (bufi * 2 + c)
                        S.op(PE, lambda e, ps=ps, c=c: e.matmul(ps[:, qlo:512], lhsT=kT[c * 64:(c + 1) * 64, h, kt * 128:(kt + 1) * 128],
                             rhs=qT[c * 64:(c + 1) * 64, h, n * 512 + qlo:(n + 1) * 512], start=True, stop=True), writes=[psk], inc=True)
                        S.op(ACT, lambda e, ps=ps, c=c: e.activation(out=PT[bufi][c][:, qlo:512], in_=ps[:, qlo:512], func=AF.Exp, scale=0.125),
                             reads=[psk], writes=[("PT", bufi, c)])
                        if j >= 0:
                            S.op(POOL, lambda e, c=c: e.tensor_tensor(out=PT[bufi][c][:, qlo:qlo + 128], in0=PT[bufi][c][:, qlo:qlo + 128], in1=cmask[:], op=ALU.mult),
                                 reads=[("PT", bufi, c)], writes=[("PT", bufi, c)])

                def AVm(h, n, kt):
                    bufi = kt % 2
                    j = kt - 4 * n
                    for qi in range(max(j, 0), 4):
                        for c in range(2):
                            i = qi * 2 + c
                            S.op(PE, lambda e, i=i, c=c, qi=qi: e.matmul(Oap(i, 0, 129), lhsT=PT[bufi][c][:, qi * 128:(qi + 1) * 128],
                                 rhs=Vsb[:, kt, h, 0:129], start=(kt == 0 and i % 3 == 0), stop=((i, kt - 4 * n) in ((2, 1), (5, 2), (7, 3)))), reads=[("PT", bufi, c)], writes=[("O", i // 3)],
                                 inc=(qi == 3 and c == 1))

                def post(h, n):
                    S.op(ACT, lambda e: e.activation(out=osb[0][:], in_=pb[4][:], func=AF.Copy), reads=[("O", 0)], writes=[("osb", 0)])
                    S.op(DVE, lambda e: e.tensor_copy(out=osb[1][:], in_=pb[5][:]), reads=[("O", 1)], writes=[("osb", 1)])
                    S.op(ACT, lambda e: e.activation(out=osb[2][:, 0:320], in_=pb[6][:, 0:320], func=AF.Copy), reads=[("O", 2)], writes=[("osb", 2)])
                    for qi in range(4):
                        i1, i2 = qi * 2, qi * 2 + 1
                        k1, k2 = ("osb", i1 // 3), ("osb", i2 // 3)
                        S.op(DVE, lambda e, i1=i1: e.reciprocal(out=st2[:, 0:1], in_=Osb(i1, 128, 129)), reads=[k1], writes=["st2a"])
                        S.op(DVE, lambda e, i2=i2: e.reciprocal(out=st2[:, 1:2], in_=Osb(i2, 128, 129)), reads=[k2], writes=["st2b"])
                        S.op(DVE, lambda e: e.tensor_tensor(out=st2[:, 1:2], in0=st2[:, 1:2], in1=nlam, op=ALU.mult), reads=["st2b"], writes=["st2b"])
                        S.op(DVE, lambda e, i1=i1: e.tensor_scalar(out=otmp[:], in0=Osb(i1, 0, 128), scalar1=st2[:, 0:1], scalar2=None, op0=ALU.mult), reads=[k1, "st2a"], writes=["otmp"])
                        S.op(DVE, lambda e, i2=i2: e.scalar_tensor_tensor(out=otmp[:], in0=Osb(i2, 0, 128), scalar=st2[:, 1:2], in1=otmp[:], op0=ALU.mult, op1=ALU.add),
                             reads=[k2, "st2b", "otmp"], writes=["otmp"])
                        S.op(DVE, lambda e: e.memset(st2[:, 2:3], 0.0), writes=["st2c"])
                        S.op(ACT, lambda e: e.activation(out=ojunk[:], in_=otmp[:], func=AF.Square, accum_out=st2[:, 2:3]), reads=["otmp", "st2c"], writes=["ojunk", "st2c"])
                        S.op(ACT, lambda e: e.activation(out=st2[:, 3:4], in_=st2[:, 2:3], func=AF.Ln, scale=1.0 / 128, bias=epsc[:, 0:1]), reads=["st2c"], writes=["st2d"])
                        S.op(ACT, lambda e: e.activation(out=st2[:, 3:4], in_=st2[:, 3:4], func=AF.Exp, scale=-0.5), reads=["st2d"], writes=["st2d"])
                        S.op(DVE, lambda e, qi=qi: e.scalar_tensor_tensor(out=ob[:, qi, :], in0=otmp[:], scalar=st2[:, 3:4], in1=subln[:], op0=ALU.mult, op1=ALU.mult),
                             reads=["otmp", "st2d"], writes=["ob"])

                def fin(h, n):
                    for qi in range(4):
                        S.op(PE, lambda e, qi=qi: e.transpose(out=pbb[7][:, qi * 128:(qi + 1) * 128], in_=ob[:, qi, :], identity=identb[:]), reads=["ob"], writes=["pb7"], inc=(qi == 3))
                    S.op(DVE, lambda e: e.tensor_copy(out=catA[:, h, n * 512:(n + 1) * 512], in_=pbb[7][:, 0:512]), reads=["pb7"], writes=[("catA", h)])

                heads = ([int(c) for c in _os.environ.get('KHEADS', '0123')] if stage >= 2 else [])
                groups = [(h, n) for h in heads for n in range(NG)]
                pending_fin = None
                for gi, (h, n) in enumerate(groups):
                    nkt = 4 * n + 4
                    precast(2)
                    SE(h, n, 0)
                    for kt in range(nkt):
                        if kt + 1 < nkt:
                            SE(h, n, kt + 1)
                        AVm(h, n, kt)
                        if kt == min(2, nkt - 1) and pending_fin is not None:
                            fin(*pending_fin)
                            ph = pending_fin[0]
                            if pending_fin[1] == NG - 1:
                                S.op(SP, lambda e, ph=ph: e.dma_start(out=cata_d[:, ph, :], in_=catA[:, ph, :]), reads=[("catA", ph)], writes=["cata_d"], dma="cata_st")
                            pending_fin = None
                    post(h, n)
                    pending_fin = (h, n)
                if pending_fin is not None:
                    fin(*pending_fin)
                    ph = pending_fin[0]
                    S.op(SP, lambda e, ph=ph: e.dma_start(out=cata_d[:, ph, :], in_=catA[:, ph, :]), reads=[("catA", ph)], writes=["cata_d"], dma="cata_st")
            if DEBUG:
                barrier()
                S.op(SP, lambda e: e.dma_start(out=dbg_v2, in_=Vsb[:]), dma="dbg")
        barrier()
        with ExitStack() as stC:
            precast(96)
            w_out = sb("w_out", [128, 8, 1024], BF16, stC)
            for c in range(8):
                S.op(POOL, lambda e, c=c: e.dma_start(out=w_out[:, c, :], in_=w_out_d[c * 128:(c + 1) * 128, :]), writes=["w_out"], dma="w_out")
            catP = sb("catP", [128, 4, S_TOK], BF16, stC); catA2 = sb("catA2", [128, 4, S_TOK], BF16, stC)
            for g in range(4):
                S.op(SP, lambda e, g=g: e.dma_start(out=catA2[:, g, :], in_=cata_d[:, g, :]), writes=["catP"], dma="catP")
            for g in range(4):
                S.op(SP, lambda e, g=g: e.dma_start(out=catP[:, g, :], in_=catp_d[g]), writes=["catP"], dma="catP")
            ffn_b = sb("ffn_b", [128, 1024], F32, stC); w_r = sb("w_r", [128, 8, 32], F32, stC); b_r = sb("b_r", [128, 32], F32, stC)
            S.op(SP, lambda e: e.dma_start(out=ffn_b[:], in_=ffn_b_d), writes=["cC"], dma="cC")
            S.op(SP, lambda e: e.dma_start(out=w_r[:], in_=w_r_d), writes=["cC"], dma="cC")
            S.op(SP, lambda e: e.dma_start(out=b_r[:], in_=b_r_d), writes=["cC"], dma="cC")
            xt2 = [sb("xt2_%d" % i, [128, 1024], F32, stC) for i in range(2)]
            h1t = [sb("h1t%d" % i, [128, 1024], F32, stC) for i in range(2)]
            xn2f = [sb("xn2f%d" % i, [128, 1024], F32, stC) for i in range(2)]; xn2b = [sb("xn2b%d" % i, [128, 1024], BF16, stC) for i in range(2)]
            cjunk = [sb("cjunk%d" % i, [128, 1024], BF16, stC) for i in range(2)]
            xn2T = [sb("xn2T%d" % i, [128, 1024], F32, stC) for i in range(2)]
            st3 = sb("st3", [128, 64], F32, stC)
            lgall = sb("lgall", [128, NT, 32], F32, stC); top8 = sb("top8", [128, NT, 8], F32, stC); idx8 = sb("idx8", [128, NT, 8], U32, stC)
            maskall = sb("maskall", [128, NT, 32], F32, stC); Gall = sb("Gall", [128, NT, 32], F32, stC)
            S.op(DVE, lambda e: e.memset(st3[:], 0.0), writes=["st3"])
            NTC = NT if stage >= 3 else 0
            def c_tile(t):
                q4 = 4 * (t % 2)
                xk = ("xt2", t % 2); hk2 = ("h1t", t % 2)
                S.op(SP, lambda e, t=t: e.dma_start(out=xt2[t % 2][:], in_=x_d[t * 128:(t + 1) * 128, :]), writes=[xk], dma="xt2_%d" % (t % 2))
                for hf in range(2):
                    for c in range(8):
                        cat_c = catA2[:, c, t * 128:(t + 1) * 128] if c < 4 else catP[:, c - 4, t * 128:(t + 1) * 128]
                        S.op(PE, lambda e, cat_c=cat_c, c=c, hf=hf: e.matmul(pb[hf + q4][:], lhsT=cat_c, rhs=w_out[:, c, hf * 512:(hf + 1) * 512], start=(c == 0), stop=(c == 7)),
                             reads=["w_out", "catP"], writes=["pb%d" % (hf + q4)], inc=(c == 7))
                    S.op(DVE, lambda e, t=t, hf=hf: e.tensor_tensor(out=h1t[t % 2][:, hf * 512:(hf + 1) * 512], in0=pb[hf + q4][:], in1=xt2[t % 2][:, hf * 512:(hf + 1) * 512], op=ALU.add),
                         reads=["pb%d" % (hf + q4), xk], writes=[hk2])
                S.op(POOL, lambda e, t=t: e.dma_start(out=h1_d[t * 128:(t + 1) * 128, :], in_=h1t[t % 2][:]), reads=[hk2], writes=["h1_d"], dma="h1st%d" % (t % 2))
                S.op(ACT, lambda e, t=t: e.activation(out=cjunk[t % 2][:], in_=h1t[t % 2][:], func=AF.Square, accum_out=st3[:, t:t + 1]), reads=[hk2, "st3"], writes=[("cjunk", t % 2), ("ss3", t)])
                S.op(ACT, lambda e, t=t: e.activation(out=st3[:, 32 + t:33 + t], in_=st3[:, t:t + 1], func=AF.Ln, scale=1.0 / 1024, bias=epsc[:, 0:1]), reads=[("ss3", t)], writes=[("rs3", t)])
                S.op(ACT, lambda e, t=t: e.activation(out=st3[:, 32 + t:33 + t], in_=st3[:, 32 + t:33 + t], func=AF.Exp, scale=-0.5), reads=[("rs3", t)], writes=[("rs3", t)])
                S.op(DVE, lambda e, t=t: e.scalar_tensor_tensor(out=xn2f[t % 2][:], in0=h1t[t % 2][:], scalar=st3[:, 32 + t:33 + t], in1=ffn_b[:], op0=ALU.mult, op1=ALU.mult),
                     reads=[hk2, ("rs3", t), "cC"], writes=[("xn2f", t % 2)])
                S.op(ACT, lambda e, t=t: e.activation(out=xn2b[t % 2][:].rearrange("s (c p) -> s c p", p=128), in_=xn2f[t % 2][:].rearrange("s (p c) -> s c p", c=8), func=AF.Copy), reads=[("xn2f", t % 2)], writes=[("xn2b", t % 2)])
                S.op(POOL, lambda e, t=t: e.dma_start(out=xn2_d[t * 128:(t + 1) * 128, :], in_=xn2b[t % 2][:]), reads=[("xn2b", t % 2)], writes=["xn2_d"], dma="xn2st%d" % (t % 2))
                for c in range(8):
                    bank = 2 + c // 4 + q4
                    S.op(PE, lambda e, c=c, bank=bank: e.transpose(out=pb[bank][:, (c % 4) * 128:(c % 4 + 1) * 128], in_=xn2f[t % 2][:, c * 128:(c + 1) * 128], identity=identf[:]),
                         reads=[("xn2f", t % 2)], writes=["pb%d" % bank], inc=(c % 4 == 3))
                S.op(ACT, lambda e, t=t: e.activation(out=xn2T[t % 2][:, 0:512], in_=pb[2 + 4 * (t % 2)][:], func=AF.Copy), reads=["pb%d" % (2 + q4)], writes=[("xn2Ta", t % 2)])
                S.op(DVE, lambda e, t=t: e.tensor_copy(out=xn2T[t % 2][:, 512:1024], in_=pb[3 + 4 * (t % 2)][:]), reads=["pb%d" % (3 + q4)], writes=[("xn2Tb", t % 2)])
                for c in range(8):
                    S.op(PE, lambda e, c=c, t=t: e.matmul(pb[2 + 4 * (t % 2)][:, 0:32], lhsT=xn2T[t % 2][:, c * 128:(c + 1) * 128], rhs=w_r[:, c, :], start=(c == 0), stop=(c == 7)),
                         reads=[("xn2Ta", t % 2), ("xn2Tb", t % 2), "cC"], writes=["pb%d" % (2 + q4)], inc=(c == 7))
                S.op(DVE, lambda e, t=t: e.tensor_tensor(out=lgall[:, t, :], in0=pb[2 + 4 * (t % 2)][:, 0:32], in1=b_r[:], op=ALU.add), reads=["pb%d" % (2 + q4), "cC"], writes=[("lg", t)])
                S.op(DVE, lambda e, t=t: e.max(out=top8[:, t, :], in_=lgall[:, t, :]), reads=[("lg", t)], writes=[("top8", t)])
                S.op(DVE, lambda e, t=t: e.max_index(out=idx8[:, t, :], in_max=top8[:, t, :], in_values=lgall[:, t, :]), reads=[("lg", t), ("top8", t)], writes=[("idx8", t)])
            for t0_ in range(0, NTC, 2):
                caps = []
                for t in (t0_, t0_ + 1):
                    S.begin_capture(); c_tile(t); caps.append(S.end_capture())
                S.replay_interleaved(caps)
            if stage >= 3:
                dispatch(nc, S, sb, stC, pb, lgall, top8, idx8, maskall, Gall, onesf, tri, iota, bstart, eb_i, dest_i, gk, xn2_d, xbuf_d, pidx, OHall, ridx_i)
        barrier()
        if stage >= 4:
            moe_blocks(nc, S, sb, st, pb, pbb, OHall, ridx_i, wbf_d, btab_d, xbuf_d, ybuf_d, identb)
            barrier()
        if stage >= 5:
            ple_phase(nc, S, sb, st, pb, pbb, dest_i, gk, h1_d, ybuf_d, p_d, w_pg_d, w_pp_d, post_b_d, gcol_ple, identb, out_d, epsc)
        fin = [Tok(s, S.dcount[id(s)], None) for s in S.dsems.values()]
        S.finish(fin)
    return nc

_NC_CACHE = {}


def _consts():
    bf = ml_dtypes.bfloat16
    c = {}
    c["ident_bf"] = np.eye(128, dtype=np.float32).astype(bf)
    c["ident_f"] = np.eye(128, dtype=np.float32)
    c["ones_f"] = np.ones((128, 128), np.float32)
    k = np.arange(128)
    c["tri_strict"] = (k[:, None] < k[None, :]).astype(np.float32)
    c["cmask"] = (k[:, None] <= k[None, :]).astype(np.float32).astype(bf)
    blk = np.zeros((128, 128), np.float32)
    blk[:64, :64] = 1.0 / 64; blk[64:, 64:] = 1.0 / 64
    c["blkdiag"] = blk.astype(bf)
    r = np.zeros((128, 128), np.float32)
    for base in (0, 64):
        for m in range(8):
            r[base + m + 8, base + m] = -1.0
            r[base + m, base + m + 8] = 1.0
    c["rmat"] = r.astype(bf)
    freqs = (np.float32(500000.0) ** (-np.arange(0, 16, 2, dtype=np.float32) / np.float32(16))).astype(np.float32)
    f = np.zeros((128, 1), np.float32)
    for base in (0, 64):
        for i in range(8):
            f[base + i, 0] = freqs[i]; f[base + 8 + i, 0] = freqs[i]
    c["freq_col"] = f
    c["invcnt"] = np.broadcast_to((1.0 / np.arange(1, 17, dtype=np.float32))[None, :], (128, 16)).copy()
    c["iota_row"] = np.broadcast_to(np.arange(32, dtype=np.float32)[None, :], (128, 32)).copy()
    c["pidx_col"] = np.stack([np.arange(128, dtype=np.float32), (np.arange(128) % 32).astype(np.float32)], axis=1)
    c["bstart_row"] = np.broadcast_to((128.0 * np.arange(NBLK, dtype=np.float32))[None, :], (128, NBLK)).copy()
    return c


def _rep(v, n=128):
    return np.ascontiguousarray(np.broadcast_to(np.asarray(v)[None, ...], (n,) + tuple(np.asarray(v).shape)))


def make_in_maps(inputs):
    f32 = np.float32
    g = {k: np.asarray(v) for k, v in inputs.items()}
    shared = dict(_consts())
    shared["w_in"] = np.ascontiguousarray(g["w_in"][0], f32)
    shared["gcol_attn"] = np.ascontiguousarray(g["attn_norm"][0].reshape(8, 128).T)
    shared["qn_col"] = np.ascontiguousarray(np.tile(g["q_norm"][0], 2).reshape(128, 1))
    shared["kn_col"] = np.ascontiguousarray(np.tile(g["k_norm"][0], 2).reshape(128, 1))
    shared["lamv"] = _rep(np.stack([g["lam_q1"][0], g["lam_k1"][0], g["lam_q2"][0], g["lam_k2"][0]], 0))
    shared["subln_b"] = _rep(g["subln"][0])
    shared["w_pool"] = np.ascontiguousarray(g["w_pool"][0])
    shared["pscale_col"] = np.ascontiguousarray(g["pool_scale"][0].reshape(4, 128).T)
    shared["w_out"] = np.ascontiguousarray(g["w_out"][0])
    shared["ffn_b"] = _rep(g["ffn_norm"][0])
    shared["w_router"] = np.ascontiguousarray(g["w_router"][0].reshape(8, 128, 32).transpose(1, 0, 2))
    shared["b_router_b"] = _rep(g["b_router"][0])
    shared["w_gate"] = np.ascontiguousarray(g["w_gate"][0]); shared["w_up"] = np.ascontiguousarray(g["w_up"][0]); shared["w_down"] = np.ascontiguousarray(g["w_down"][0])
    shared["btab"] = np.ascontiguousarray(np.concatenate([g["b_gate"][0], g["b_up"][0], g["b_down"][0]], axis=1))
    shared["gcol_ple"] = np.ascontiguousarray(g["ple_gate_norm"][0].reshape(8, 128).T)
    shared["w_ple_gate"] = np.ascontiguousarray(g["w_ple_gate"][0]); shared["w_ple_proj"] = np.ascontiguousarray(g["w_ple_proj"][0])
    shared["post_b"] = _rep(g["ple_post_norm"][0])
    maps = []
    for b in range(8):
        m = dict(shared)
        m["x"] = np.ascontiguousarray(g["x"][b]); m["p"] = np.ascontiguousarray(g["p"][0, b])
        m["posb"] = _rep(g["positions"][b].astype(np.int32))
        maps.append(m)
    return maps


def kernel(**inputs):
    if "nc" not in _NC_CACHE:
        _NC_CACHE["nc"] = build()
    nc = _NC_CACHE["nc"]
    maps = make_in_maps(inputs)
    res = run_bass_kernel_spmd(nc, maps, core_ids=list(range(8)))
    return np.stack([np.asarray(r["out"]) for r in res.results], axis=0).astype(np.float32)
```
